# Optimizing a Trainium2 kernel written in Bass

```python
import jax
import jax.numpy as jnp
from jax import lax
import numpy as np

D_MODEL = 4096
BATCH = 4
SEQ = 2048
DEPTH = 4

CHUNK = 64
HEAD_DIM = D_MODEL // 32
GROUP_HEADS = 16
GROUP_WIDTH = GROUP_HEADS * HEAD_DIM
MIX_WIDTH = 2 * GROUP_WIDTH
AB_IN = 7 * GROUP_WIDTH + 2 * GROUP_HEADS
CD_IN = 6 * GROUP_WIDTH
CONV_K = 4
F_BIAS_LO = 3.0
F_BIAS_HI = 6.0
PAST_CHUNKS = 8
BAND = (PAST_CHUNKS + 1) * CHUNK
REL_MAX = 2 * CHUNK
REL_SIZE = CHUNK + REL_MAX
SB_BLOCK = 128
ROPE_BASE = 10000.0
N_MEM = 256
XA_HEADS = 4
XA_HEAD_DIM = HEAD_DIM
XA_WIDTH = XA_HEADS * XA_HEAD_DIM
N_EXPERTS = 32
TOP_K = 4
EXPERT_FF = D_MODEL // 16
SWIGLU_LIMIT = 7.0
SWIGLU_ALPHA = 1.702
LN_EPS = 1e-5
HN_EPS = 1e-6
DN_ALPHA = (2.0 * DEPTH) ** 0.25
DN_BETA = (8.0 * DEPTH) ** -0.25
N_EVEN = (DEPTH + 1) // 2
N_ODD = DEPTH // 2

kernel_name = 'hybrid_streaming_retention_mlstm_band_stickbreak_moe'

F32 = jnp.float32


def layer_norm(x, g, b):
    xf = x.astype(F32)
    mu = jnp.mean(xf, axis=-1, keepdims=True)
    var = jnp.mean(jnp.square(xf - mu), axis=-1, keepdims=True)
    return ((xf - mu) * lax.rsqrt(var + LN_EPS) * g.astype(F32) + b.astype(F32)).astype(x.dtype)


def head_norm(h, g):
    mu = jnp.mean(h, axis=-1, keepdims=True)
    var = jnp.mean(jnp.square(h - mu), axis=-1, keepdims=True)
    hn = (h - mu) * lax.rsqrt(var + HN_EPS)
    return hn.reshape(h.shape[:-2] + (-1,)) * g.astype(F32)


def to_heads(a):
    return a.reshape(a.shape[:-1] + (GROUP_HEADS, HEAD_DIM)).astype(F32)


def rope_tables(seq_len):
    pos = jnp.arange(seq_len, dtype=F32)
    inv_freq = ROPE_BASE ** (-jnp.arange(0, HEAD_DIM, 2, dtype=F32) / HEAD_DIM)
    ang = pos[:, None] * inv_freq[None, :]
    return jnp.cos(ang), jnp.sin(ang)


def rope(a, cos, sin):
    half = a.shape[-1] // 2
    a1, a2 = a[..., :half], a[..., half:]
    c = cos[None, :, None, :]
    s = sin[None, :, None, :]
    return jnp.concatenate([a1 * c - a2 * s, a1 * s + a2 * c], axis=-1)


def seq_to_chunks(a):
    b_, s_, h_ = a.shape[:3]
    a = a.reshape((b_, s_ // CHUNK, CHUNK, h_) + a.shape[3:])
    return a.transpose((1, 0, 3, 2) + tuple(range(4, a.ndim)))


def chunks_to_seq(o):
    nc, b_, h_, l_, d_ = o.shape
    return o.transpose(1, 0, 3, 2, 4).reshape(b_, nc * l_, h_, d_)


def causal_depthwise_conv(u, w, b):
    y = lax.conv_general_dilated(
        u, w[:, None, :].astype(u.dtype), window_strides=(1,), padding=[(CONV_K - 1, 0)],
        dimension_numbers=('NWC', 'WIO', 'NWC'), feature_group_count=u.shape[-1])
    return y + b.astype(u.dtype)


def retention(q, k, v):
    b_, _, h_, d_ = q.shape
    log_g = jnp.log1p(-jnp.exp2(-(5.0 + jnp.arange(h_, dtype=F32))))
    j = jnp.arange(CHUNK, dtype=F32)
    diff = j[:, None] - j[None, :]
    intra = jnp.where(diff >= 0, jnp.exp(log_g[:, None, None] * jnp.maximum(diff, 0.0)), 0.0)
    q_dec = jnp.exp(log_g[:, None] * (j + 1.0))[..., None]
    k_dec = jnp.exp(log_g[:, None] * (CHUNK - 1.0 - j))[..., None]
    c_dec = jnp.exp(log_g * CHUNK)[:, None, None]

    def step(state, inp):
        qc, kc, vc = inp
        att = jnp.einsum('bhid,bhjd->bhij', qc, kc) * intra
        o = (jnp.einsum('bhij,bhje->bhie', att, vc)
             + jnp.einsum('bhid,bhde->bhie', qc * q_dec, state))
        state = state * c_dec + jnp.einsum('bhjd,bhje->bhde', kc * k_dec, vc)
        return state, o

    state0 = jnp.zeros((b_, h_, d_, v.shape[-1]), F32)
    _, o = lax.scan(step, state0, (seq_to_chunks(q), seq_to_chunks(k), seq_to_chunks(v)))
    return chunks_to_seq(o)


def mlstm(q, k, v, ig, lf):
    b_, _, h_, d_ = q.shape
    causal = jnp.tril(jnp.ones((CHUNK, CHUNK), dtype=bool))

    def step(carry, inp):
        c_st, n_st, m_st = carry
        qc, kc, vc, ic, fc = inp
        bcum = jnp.cumsum(fc, axis=-1)
        log_intra = jnp.where(causal, bcum[..., :, None] - bcum[..., None, :] + ic[..., None, :], -jnp.inf)
        log_cross = bcum + m_st[..., None]
        m_row = jnp.maximum(log_cross, jnp.max(log_intra, axis=-1))
        w_intra = jnp.exp(log_intra - m_row[..., None])
        w_cross = jnp.exp(log_cross - m_row)
        qk = jnp.einsum('bhid,bhjd->bhij', qc, kc) * w_intra
        num = (jnp.einsum('bhij,bhje->bhie', qk, vc)
               + w_cross[..., None] * jnp.einsum('bhid,bhde->bhie', qc, c_st))
        den = jnp.sum(qk, axis=-1) + w_cross * jnp.einsum('bhid,bhd->bhi', qc, n_st)
        h = num / jnp.maximum(jnp.abs(den), jnp.exp(-m_row))[..., None]
        b_last = bcum[..., -1]
        log_state = b_last[..., None] - bcum + ic
        m_new = jnp.maximum(b_last + m_st, jnp.max(log_state, axis=-1))
        decay = jnp.exp(b_last + m_st - m_new)
        kw = kc * jnp.exp(log_state - m_new[..., None])[..., None]
        c_st = decay[..., None, None] * c_st + jnp.einsum('bhjd,bhje->bhde', kw, vc)
        n_st = decay[..., None] * n_st + jnp.sum(kw, axis=2)
        return (c_st, n_st, m_new), h

    carry0 = (jnp.zeros((b_, h_, d_, v.shape[-1]), F32), jnp.zeros((b_, h_, d_), F32),
              jnp.zeros((b_, h_), F32))
    _, h = lax.scan(step, carry0, (seq_to_chunks(q), seq_to_chunks(k), seq_to_chunks(v),
                                   seq_to_chunks(ig), seq_to_chunks(lf)))
    return chunks_to_seq(h)


def chunk_band_attention(q, k, v, rel_bias):
    b_, s_, h_, d_ = q.shape
    past = PAST_CHUNKS * CHUNK
    kp = jnp.pad(k, ((0, 0), (past, 0), (0, 0), (0, 0)))
    vp = jnp.pad(v, ((0, 0), (past, 0), (0, 0), (0, 0)))
    r = jnp.arange(CHUNK)
    jk = jnp.arange(BAND)
    dist = r[:, None] + past - jk[None, :]
    bias = rel_bias.astype(F32)[:, jnp.clip(dist, -(CHUNK - 1), REL_MAX) + (CHUNK - 1)]
    scale = d_ ** -0.5

    def one_chunk(c):
        start = c * CHUNK
        qc = lax.dynamic_slice_in_dim(q, start, CHUNK, axis=1)
        kb = lax.dynamic_slice_in_dim(kp, start, BAND, axis=1)
        vb = lax.dynamic_slice_in_dim(vp, start, BAND, axis=1)
        s = jnp.einsum('bqhd,bkhd->bhqk', qc, kb) * scale + bias
        valid = (start - past + jk) >= 0
        p = jax.nn.softmax(jnp.where(valid, s, -jnp.inf), axis=-1)
        return jnp.einsum('bhqk,bkhd->bqhd', p, vb)

    out = lax.map(one_chunk, jnp.arange(s_ // CHUNK))
    return out.transpose(1, 0, 2, 3, 4).reshape(b_, s_, h_, d_)


def stick_breaking_attention(q, k, v):
    b_, s_, h_, d_ = q.shape
    scale = d_ ** -0.5
    key_pos = jnp.arange(s_)

    def one_block(blk):
        start = blk * SB_BLOCK
        qb = lax.dynamic_slice_in_dim(q, start, SB_BLOCK, axis=1)
        z = jnp.einsum('bqhd,bkhd->bhqk', qb, k) * scale
        strict = key_pos[None, :] < (start + jnp.arange(SB_BLOCK))[:, None]
        log_rest = jnp.where(strict, jax.nn.log_sigmoid(-z), 0.0)
        between = lax.cumsum(log_rest, axis=3, reverse=True) - log_rest
        log_a = jnp.where(strict, jax.nn.log_sigmoid(z) + between, -jnp.inf)
        return jnp.einsum('bhqk,bkhd->bqhd', jnp.exp(log_a), v)

    out = lax.map(one_block, jnp.arange(s_ // SB_BLOCK))
    return out.transpose(1, 0, 2, 3, 4).reshape(b_, s_, h_, d_)


def mixer_ab(x, w_in, gate_b, conv_w, conv_b, wq_m, wk_m, ret_g, mlstm_g, w_out, cos, sin):
    wd = GROUP_WIDTH
    scale = HEAD_DIM ** -0.5
    z = x @ w_in
    rq, rk, rv, rg, mu, mv, mo = [z[..., i * wd:(i + 1) * wd] for i in range(7)]
    gates = z[..., 7 * wd:].astype(F32) + gate_b.astype(F32)
    ret = retention(rope(to_heads(rq), cos, sin), rope(to_heads(rk), cos, sin) * scale, to_heads(rv))
    out_a = head_norm(ret, ret_g) * jax.nn.silu(rg.astype(F32))
    u = to_heads(jax.nn.silu(causal_depthwise_conv(mu, conv_w, conv_b)))
    qm = jnp.einsum('bshd,hde->bshe', u, wq_m.astype(F32))
    km = jnp.einsum('bshd,hde->bshe', u, wk_m.astype(F32)) * scale
    ig = gates[..., :GROUP_HEADS]
    lf = jax.nn.log_sigmoid(gates[..., GROUP_HEADS:])
    h = mlstm(qm, km, to_heads(mv), ig, lf) * jax.nn.sigmoid(to_heads(mo))
    out_b = head_norm(h, mlstm_g)
    y = jnp.concatenate([out_a, out_b], axis=-1).astype(x.dtype)
    return y @ w_out


def mixer_cd(x, w_in, rel_bias, w_out):
    z = x @ w_in
    cq, ck, cv, sq, sk, sv = jnp.split(z, 6, axis=-1)
    out_c = chunk_band_attention(to_heads(cq), to_heads(ck), to_heads(cv), rel_bias)
    out_d = stick_breaking_attention(to_heads(sq), to_heads(sk), to_heads(sv))
    y = jnp.concatenate([out_c, out_d], axis=-2)
    y = y.reshape(x.shape[:2] + (MIX_WIDTH,)).astype(x.dtype)
    return y @ w_out


def memory_cross_attention(x, mem, wq, wkv, wo):
    b_, s_, _ = x.shape
    q = (x @ wq).reshape(b_, s_, XA_HEADS, XA_HEAD_DIM).astype(F32)
    kv = (mem @ wkv).reshape(b_, mem.shape[1], 2, XA_HEADS, XA_HEAD_DIM).astype(F32)
    s = jnp.einsum('bqhd,bkhd->bhqk', q, kv[:, :, 0]) * (XA_HEAD_DIM ** -0.5)
    p = jax.nn.softmax(s, axis=-1)
    o = jnp.einsum('bhqk,bkhd->bqhd', p, kv[:, :, 1]).reshape(b_, s_, XA_WIDTH)
    return o.astype(x.dtype) @ wo


def moe(x, router_w, router_b, w_gate, b_gate, w_up, b_up, w_down, b_down):
    b_, s_, d_ = x.shape
    xt = x.reshape(-1, d_)
    logits = (xt @ router_w).astype(F32) + router_b.astype(F32)
    top_v, top_i = lax.top_k(logits, TOP_K)
    top_w = jax.nn.softmax(top_v, axis=-1)
    gate = jnp.einsum('tk,tke->te', top_w, jax.nn.one_hot(top_i, N_EXPERTS, dtype=F32))
    g = (jnp.einsum('td,edf->tef', xt, w_gate) + b_gate).astype(F32)
    u = (jnp.einsum('td,edf->tef', xt, w_up) + b_up).astype(F32)
    g = jnp.minimum(g, SWIGLU_LIMIT)
    u = jnp.clip(u, -SWIGLU_LIMIT, SWIGLU_LIMIT)
    act = g * jax.nn.sigmoid(SWIGLU_ALPHA * g) * (u + 1.0)
    h = (act * gate[..., None]).astype(x.dtype)
    y = jnp.einsum('tef,efd->td', h, w_down) + gate.astype(x.dtype) @ b_down
    return y.reshape(b_, s_, d_)


def setup_inputs(seed: int = 0) -> dict:
    key = jax.random.key(seed)
    ks = iter(jax.random.split(key, 48))
    d, w, h, hd = D_MODEL, GROUP_WIDTH, GROUP_HEADS, HEAD_DIM

    def nrm(shape, scale):
        return jax.random.normal(next(ks), shape, F32) * scale

    def gain(shape):
        return 1.0 + nrm(shape, 0.02)

    x = nrm((BATCH, SEQ, d), 1.0)
    mem = nrm((BATCH, N_MEM, d), 1.0)
    ab_w_in = nrm((N_EVEN, d, AB_IN), d ** -0.5)
    f_bias = jnp.linspace(F_BIAS_LO, F_BIAS_HI, h, dtype=F32)[None, :] + nrm((N_EVEN, h), 0.01)
    ab_gate_b = jnp.concatenate([nrm((N_EVEN, h), 0.1), f_bias], axis=-1)
    ab_conv_w = nrm((N_EVEN, CONV_K, w), CONV_K ** -0.5)
    ab_conv_b = nrm((N_EVEN, w), 0.02)
    ab_wq = nrm((N_EVEN, h, hd, hd), hd ** -0.5)
    ab_wk = nrm((N_EVEN, h, hd, hd), hd ** -0.5)
    ab_ret_norm_g = gain((N_EVEN, w))
    ab_mlstm_norm_g = gain((N_EVEN, w))
    ab_w_out = nrm((N_EVEN, MIX_WIDTH, d), MIX_WIDTH ** -0.5 * DN_BETA)
    cd_w_in = nrm((N_ODD, d, CD_IN), d ** -0.5)
    cd_rel_bias = nrm((N_ODD, h, REL_SIZE), 0.2)
    cd_w_out = nrm((N_ODD, MIX_WIDTH, d), MIX_WIDTH ** -0.5 * DN_BETA)
    mix_ln_g = gain((DEPTH, d))
    mix_ln_b = nrm((DEPTH, d), 0.02)
    xa_wq = nrm((DEPTH, d, XA_WIDTH), d ** -0.5)
    xa_wkv = nrm((DEPTH, d, 2 * XA_WIDTH), d ** -0.5)
    xa_wo = nrm((DEPTH, XA_WIDTH, d), XA_WIDTH ** -0.5 * DN_BETA)
    xa_ln_g = gain((DEPTH, d))
    xa_ln_b = nrm((DEPTH, d), 0.02)
    moe_router_w = nrm((DEPTH, d, N_EXPERTS), d ** -0.5)
    moe_router_b = nrm((DEPTH, N_EXPERTS), 0.01)
    moe_w_gate = nrm((DEPTH, N_EXPERTS, d, EXPERT_FF), d ** -0.5)
    moe_b_gate = nrm((DEPTH, N_EXPERTS, EXPERT_FF), 0.02)
    moe_w_up = nrm((DEPTH, N_EXPERTS, d, EXPERT_FF), d ** -0.5)
    moe_b_up = nrm((DEPTH, N_EXPERTS, EXPERT_FF), 0.02)
    moe_w_down = nrm((DEPTH, N_EXPERTS, EXPERT_FF, d), EXPERT_FF ** -0.5 * DN_BETA)
    moe_b_down = nrm((DEPTH, N_EXPERTS, d), 0.02)
    moe_ln_g = gain((DEPTH, d))
    moe_ln_b = nrm((DEPTH, d), 0.02)
    return {
        'x': x, 'mem': mem,
        'ab_w_in': ab_w_in, 'ab_gate_b': ab_gate_b, 'ab_conv_w': ab_conv_w, 'ab_conv_b': ab_conv_b,
        'ab_wq': ab_wq, 'ab_wk': ab_wk, 'ab_ret_norm_g': ab_ret_norm_g,
        'ab_mlstm_norm_g': ab_mlstm_norm_g, 'ab_w_out': ab_w_out,
        'cd_w_in': cd_w_in, 'cd_rel_bias': cd_rel_bias, 'cd_w_out': cd_w_out,
        'mix_ln_g': mix_ln_g, 'mix_ln_b': mix_ln_b,
        'xa_wq': xa_wq, 'xa_wkv': xa_wkv, 'xa_wo': xa_wo, 'xa_ln_g': xa_ln_g, 'xa_ln_b': xa_ln_b,
        'moe_router_w': moe_router_w, 'moe_router_b': moe_router_b,
        'moe_w_gate': moe_w_gate, 'moe_b_gate': moe_b_gate, 'moe_w_up': moe_w_up, 'moe_b_up': moe_b_up,
        'moe_w_down': moe_w_down, 'moe_b_down': moe_b_down, 'moe_ln_g': moe_ln_g, 'moe_ln_b': moe_ln_b,
    }


def reference(x, mem, ab_w_in, ab_gate_b, ab_conv_w, ab_conv_b, ab_wq, ab_wk, ab_ret_norm_g,
              ab_mlstm_norm_g, ab_w_out, cd_w_in, cd_rel_bias, cd_w_out, mix_ln_g, mix_ln_b,
              xa_wq, xa_wkv, xa_wo, xa_ln_g, xa_ln_b, moe_router_w, moe_router_b,
              moe_w_gate, moe_b_gate, moe_w_up, moe_b_up, moe_w_down, moe_b_down,
              moe_ln_g, moe_ln_b):
    cos, sin = rope_tables(x.shape[1])
    h = x
    for layer in range(DEPTH):
        i = layer // 2
        if layer % 2 == 0:
            y = mixer_ab(h, ab_w_in[i], ab_gate_b[i], ab_conv_w[i], ab_conv_b[i], ab_wq[i], ab_wk[i],
                         ab_ret_norm_g[i], ab_mlstm_norm_g[i], ab_w_out[i], cos, sin)
        else:
            y = mixer_cd(h, cd_w_in[i], cd_rel_bias[i], cd_w_out[i])
        h = layer_norm(DN_ALPHA * h + y, mix_ln_g[layer], mix_ln_b[layer])
        y = memory_cross_attention(h, mem, xa_wq[layer], xa_wkv[layer], xa_wo[layer])
        h = layer_norm(DN_ALPHA * h + y, xa_ln_g[layer], xa_ln_b[layer])
        y = moe(h, moe_router_w[layer], moe_router_b[layer], moe_w_gate[layer], moe_b_gate[layer],
                moe_w_up[layer], moe_b_up[layer], moe_w_down[layer], moe_b_down[layer])
        h = layer_norm(DN_ALPHA * h + y, moe_ln_g[layer], moe_ln_b[layer])
    return h.astype(x.dtype)
```

```python
import numpy as np
import ml_dtypes
from contextlib import ExitStack
import concourse.bass as bass
import concourse.mybir as mybir
from concourse.bass_utils import run_bass_kernel_spmd

F32 = mybir.dt.float32
BF16 = mybir.dt.bfloat16
AF = mybir.ActivationFunctionType
ALU = mybir.AluOpType
AX = mybir.AxisListType

D = 4096
KC = 32
DEPTH = 4
DN_ALPHA = (2.0 * DEPTH) ** 0.25
LN_EPS = 1e-5
HN_EPS = 1e-6
NE = 32
NCORES = 8


class DSem:
    __slots__ = ("h", "cnt", "key")

    def __init__(self, h, key):
        self.h = h
        self.cnt = 0
        self.key = key


class _BState:
    __slots__ = ("writer", "readers", "dsem", "dlast", "dwait")

    def __init__(self):
        self.writer = None
        self.readers = {}
        self.dsem = {}
        self.dlast = None
        self.dwait = []


class Buf:
    __slots__ = ("t", "name", "s", "is_dram")

    def __init__(self, t, name, is_dram=False):
        self.t = t
        self.name = name
        self.s = _BState()
        self.is_dram = is_dram

    def view(self, ap):
        b = Buf(ap, self.name, self.is_dram)
        b.s = self.s
        return b

    def __getitem__(self, idx):
        return self.t[idx]

    writer = property(lambda self: self.s.writer, lambda self, v: setattr(self.s, "writer", v))
    readers = property(lambda self: self.s.readers, lambda self, v: setattr(self.s, "readers", v))
    dsem = property(lambda self: self.s.dsem, lambda self, v: setattr(self.s, "dsem", v))
    dlast = property(lambda self: self.s.dlast, lambda self, v: setattr(self.s, "dlast", v))
    dwait = property(lambda self: self.s.dwait, lambda self, v: setattr(self.s, "dwait", v))


class Prog:
    ENGS = ("pe", "act", "dve", "pool", "sp")

    def __init__(self):
        self.nc = bass.Bass("TRN2", target_bir_lowering=False)
        nc = self.nc
        self.st = ExitStack()
        self.top = self.st
        self.e = {"pe": nc.tensor, "act": nc.scalar, "dve": nc.vector, "pool": nc.gpsimd, "sp": nc.sync}
        self.sem = {k: self.top.enter_context(nc.semaphore("s_" + k)) for k in self.ENGS}
        self.cnt = {k: 0 for k in self.ENGS}
        self.waited = {}
        self.nbuf = 0
        self.ninst = 0
        self.dsems = []
        self.dfree = {"sw": [], "hw": []}
        self.phase_bufs = None
        self.dram_bufs = []

    def sb(self, shape, dt=F32, name=None):
        self.nbuf += 1
        name = (name or "sb") + f"_{self.nbuf}"
        t = self.st.enter_context(self.nc.sbuf_tensor(name, list(shape), dt))
        b = Buf(t, name)
        if self.phase_bufs is not None:
            self.phase_bufs.append(b)
        return b

    def ps(self, shape, dt=F32, name=None):
        self.nbuf += 1
        name = (name or "ps") + f"_{self.nbuf}"
        t = self.st.enter_context(self.nc.psum_tensor(name, list(shape), dt))
        b = Buf(t, name)
        if self.phase_bufs is not None:
            self.phase_bufs.append(b)
        return b

    def dram(self, name, shape, dt, kind):
        t = self.nc.dram_tensor(name, list(shape), dt, kind=kind)
        b = Buf(t.ap(), name, is_dram=True)
        self.dram_bufs.append(b)
        return b

    def _get_dsem(self, qt):
        if self.dfree[qt]:
            return self.dfree[qt].pop()
        d = DSem(self.top.enter_context(self.nc.semaphore(f"d{len(self.dsems)}")), f"d{len(self.dsems)}")
        self.dsems.append(d)
        return d

    def begin_phase(self):
        self.st = ExitStack()
        self.phase_bufs = []

    def barrier(self):
        for x in self.ENGS:
            for e in self.ENGS:
                if e != x:
                    self._wait(x, e, self.sem[e], self.cnt[e])
            for d in self.dsems:
                self._wait(x, d.key, d.h, 16 * d.cnt)

    def end_phase(self):
        self.barrier()
        for b in self.phase_bufs:
            for qt, d in b.dsem.items():
                self.dfree[qt].append(d)
            b.dsem = {}
        for b in self.dram_bufs:
            b.dwait = []
            b.writer = None
            b.readers = {}
        self.st.close()
        self.st = self.top
        self.phase_bufs = None

    def _wait(self, x, key, sem, val):
        if val <= 0:
            return
        k2 = (x, key)
        if self.waited.get(k2, 0) >= val:
            return
        self.waited[k2] = val
        self.e[x].wait_ge(sem, val)

    def _wait_dma(self, x, b, skip_qt=None):
        for qt, d in b.dsem.items():
            if qt != skip_qt:
                self._wait(x, d.key, d.h, 16 * d.cnt)
        for d in b.dwait:
            self._wait(x, d.key, d.h, 16 * d.cnt)

    def _deps(self, x, R, W, dma_skip=None, skip_qt=None):
        for b in R:
            if b.writer is not None:
                e, k = b.writer
                if not (x == "pe" and e == "pe"):
                    self._wait(x, e, self.sem[e], k)
            self._wait_dma(x, b, skip_qt if b is dma_skip else None)
        for b in W:
            if b.writer is not None:
                e, k = b.writer
                if not (x == "pe" and e == "pe"):
                    self._wait(x, e, self.sem[e], k)
            for e, k in b.readers.items():
                if not (x == "pe" and e == "pe"):
                    self._wait(x, e, self.sem[e], k)
            self._wait_dma(x, b, skip_qt if b is dma_skip else None)

    def I(self, x, fname, *args, R=(), W=(), **kw):
        self._deps(x, R, W)
        inst = getattr(self.e[x], fname)(*args, **kw)
        self.cnt[x] += 1
        self.ninst += 1
        k = self.cnt[x]
        inst.then_inc(self.sem[x], 1)
        for b in R:
            b.readers[x] = k
        for b in W:
            b.writer = (x, k)
            b.readers = {}
        return inst

    def dma(self, q, out, in_, R, W, **kw):
        if not W.is_dram:
            trk, kind = W, "w"
        elif not R.is_dram:
            trk, kind = R, "r"
        else:
            trk, kind = W, "w"
        qt = "sw" if q == "pool" else "hw"
        skip = trk if (trk.dlast == kind or trk.dlast is None) else None
        self._deps(q, [R], [W], dma_skip=skip, skip_qt=qt)
        if qt not in trk.dsem:
            trk.dsem[qt] = self._get_dsem(qt)
        ds = trk.dsem[qt]
        inst = self.e[q].dma_start(out=out, in_=in_, **kw)
        ds.cnt += 1
        trk.dlast = kind
        inst.then_inc(ds.h, 16)
        self.ninst += 1
        other = R if trk is W else W
        if other is not trk and ds not in other.dwait:
            other.dwait.append(ds)
        return inst

    def finish(self, bufs, x="sp"):
        for b in bufs:
            self._wait_dma(x, b)

    def close(self):
        self.top.close()


class Common:
    def __init__(self, P, ident_bf_d, ident_f_d):
        self.P = P
        self.ident_bf = P.sb([128, 128], BF16, "identb")
        self.ident_f = P.sb([128, 128], F32, "identf")
        P.dma("sp", self.ident_bf[:], ident_bf_d[:], R=ident_bf_d, W=self.ident_bf)
        P.dma("sp", self.ident_f[:], ident_f_d[:], R=ident_f_d, W=self.ident_f)
        self.eps_ln = P.sb([128, 1], F32, "epsln")
        P.I("dve", "memset", self.eps_ln[:], LN_EPS, W=[self.eps_ln])
        self.eps_hn = P.sb([128, 1], F32, "epshn")
        P.I("dve", "memset", self.eps_hn[:], HN_EPS, W=[self.eps_hn])
        self.rr = 0

    def evac_engine(self):
        self.rr += 1
        return "act" if self.rr % 2 else "dve"

    def copy(self, eng, out, in_, R, W):
        P = self.P
        if eng == "act":
            P.I("act", "activation", out=out, in_=in_, func=AF.Copy, R=R, W=W)
        else:
            P.I(eng, "tensor_copy", out=out, in_=in_, R=R, W=W)


def emit_layernorm(P, C, x, G, Bt, stats, mv, hb=None):
    xs = x[:].rearrange("p (c f) -> p c f", f=512)
    for c in range(8):
        P.I("dve", "bn_stats", out=stats[:, c, :], in_=xs[:, c, :], R=[x], W=[stats])
    P.I("dve", "bn_aggr", out=mv[:, 0:2], in_=stats[:], R=[stats], W=[mv])
    P.I("act", "activation", out=mv[:, 2:3], in_=mv[:, 1:2], func=AF.Sqrt, bias=C.eps_ln[:], scale=1.0,
        R=[mv, C.eps_ln], W=[mv])
    P.I("dve", "reciprocal", out=mv[:, 3:4], in_=mv[:, 2:3], R=[mv], W=[mv])
    P.I("dve", "tensor_scalar", out=x[:], in0=x[:], scalar1=mv[:, 0:1], scalar2=mv[:, 3:4],
        op0=ALU.subtract, op1=ALU.mult, R=[x, mv], W=[x])
    P.I("dve", "tensor_tensor", out=x[:], in0=x[:], in1=G[:], op=ALU.mult, R=[x, G], W=[x])
    P.I("dve", "tensor_tensor", out=x[:], in0=x[:], in1=Bt[:], op=ALU.add, R=[x, Bt], W=[x])
    if hb is not None:
        P.I("act", "activation", out=hb[:], in_=x[:], func=AF.Copy, R=[x], W=[hb])


def emit_transpose_to_hT(P, C, hb, hT, tt, tps):
    for g in range(4):
        tp = tps[g % len(tps)]
        for j in range(8):
            c = g * 8 + j
            P.I("pe", "transpose", out=tp[:, j, :], in_=hb[:, c:D:KC], identity=C.ident_bf[:],
                R=[hb, C.ident_bf], W=[tp])
        C.copy(C.evac_engine(), hT[:, g * 8:(g + 1) * 8, tt * 128:(tt + 1) * 128], tp[:], R=[tp], W=[hT])


def build_B(ntok=1024, n_exp=NE, P=None, io=None, pre=""):
    emb = P is not None
    if not emb:
        P = Prog()
    io = io or {}
    TB = 512
    nblk = ntok // TB
    dr = {}

    def din(name, shape, dt=F32):
        dr[name] = io[name] if name in io else P.dram(pre + name, shape, dt, "ExternalInput")
        return dr[name]

    yT_d = din("yT", [D, ntok], BF16)
    hin_d = din("h_in", [ntok, D])
    memT_d = din("memT", [D, 256])
    wout_d = din("w_out", [D, D])
    lnp_d = din("lnp", [6, D])
    wq_d = din("xa_wq", [D, 512])
    wkv_d = din("xa_wkv", [D, 1024])
    wo_d = din("xa_wo", [512, D])
    rw_d = din("rw", [D, NE])
    rb_d = din("rb", [1, NE])
    wg_d = din("wg", [NE, D, 256])
    wu_d = din("wu", [NE, D, 256])
    wd_d = din("wd", [NE, 256, D])
    bgu_d = din("bgu", [128, NE * 4])
    bd_d = din("bd", [NE, D])
    idb_d = din("ident_bf", [128, 128], BF16)
    idf_d = din("ident_f", [128, 128])
    hout_d = io["h_out"] if "h_out" in io else P.dram("h_out", [ntok, D], F32, "ExternalOutput")
    hTout_d = io["hT_out"] if "hT_out" in io else P.dram("hT_out", [ntok // 512, 128, KC * 512], BF16, "ExternalOutput")
    if emb:
        P.begin_phase()

    C = Common(P, idb_d, idf_d)

    hres = [P.sb([128, D], F32, f"hres{i}") for i in range(4)]
    hT = P.sb([128, KC, TB], BF16, "hT")
    hb = P.sb([128, D], BF16, "hb")
    lnact = P.sb([128, 2 * D], F32, "lnact")
    lnG = lnact.view(lnact.t[:, 0:D])
    lnB = lnact.view(lnact.t[:, D:2 * D])
    actH = lnact.view(lnact.t[:].bitcast(BF16).rearrange("p (e c t) -> p e c t", e=16, c=2))
    NRING = 4
    ring = [P.sb([128, 8, 512], BF16, f"ring{i}") for i in range(NRING)]
    ring_i = [0]

    def next_piece():
        r = ring[ring_i[0] % NRING]
        ring_i[0] += 1
        return r

    stats = P.sb([128, 8, 6], F32, "stats")
    mv = P.sb([128, 4], F32, "mv")
    kT = P.sb([128, 4, 256], BF16, "kT")
    vv = P.sb([128, 2, 512], BF16, "vv")
    qT = P.sb([128, 4, TB], BF16, "qT")
    oT = P.sb([128, 4, TB], BF16, "oT")
    PT = P.sb([128, 2, TB], BF16, "PT")
    sc = P.sb([128, 256], F32, "sc")
    pb = P.sb([128, 256], BF16, "pb")
    red = P.sb([128, 8], F32, "red")
    rws = P.sb([128, KC, NE], BF16, "rws")
    rbt = P.sb([128, NE], F32, "rbt")
    lg = P.sb([128, NE], F32, "lg")
    top8 = P.sb([128, 8], F32, "top8")
    ex = P.sb([128, NE], F32, "ex")
    msk = P.sb([128, NE], F32, "msk")
    gate = [P.sb([128, NE], F32, f"gate{i}") for i in range(4)]
    gateT = P.sb([NE, TB], BF16, "gateT")
    bgu = P.sb([128, NE * 4], F32, "bgu")
    tgs = [P.sb([128, TB], F32, f"tg{i}") for i in range(2)]
    tsg = P.sb([128, TB], F32, "tsg")
    tus = [P.sb([128, TB], F32, f"tu{i}") for i in range(2)]
    sel = P.sb([NE, NE, 128], BF16, "sel")

    A = [P.ps([128, 512], F32, f"A{i}") for i in range(8)]
    T = [A[6 + i].view(A[6 + i].t[:].bitcast(BF16).rearrange("p (j n) -> p j n", n=128)) for i in range(2)]
    for e in range(NE):
        P.I("dve", "tensor_copy", out=sel[:, e, :], in_=C.ident_bf[0:NE, e:e + 1].to_broadcast([NE, 128]), R=[C.ident_bf], W=[sel])

    P.dma("sp", bgu[:], bgu_d[:], R=bgu_d, W=bgu)
    P.dma("sp", rbt[:], rb_d.t[0, :].partition_broadcast(128), R=rb_d, W=rbt)
    P.dma("pool", rws[:], rw_d.t.rearrange("(p c) n -> p c n", c=KC), R=rw_d, W=rws)

    def wview(wd2):
        return wd2.rearrange("(c p) n -> p c n", p=128)

    def fview(wd2):
        return wd2.rearrange("(p c) n -> p c n", c=KC)

    P.dma("pool", hT[:, :, 0:256], fview(memT_d.t), R=memT_d, W=hT)
    wkv_v = fview(wkv_d.t)
    for hd in range(4):
        pieces = []
        for g in range(4):
            r = next_piece()
            P.dma("pool", r[:, :, 0:128], wkv_v[:, g * 8:(g + 1) * 8, hd * 128:(hd + 1) * 128], R=wkv_d, W=r)
            pieces.append(r)
        for c in range(KC):
            r = pieces[c // 8]
            P.I("pe", "matmul", A[0][:, 0:256], r[:, c % 8, 0:128], hT[:, c, 0:256], start=(c == 0), stop=(c == KC - 1),
                R=[r, hT], W=[A[0]])
        C.copy("act", kT[:, hd, :], A[0][:, 0:256], R=[A[0]], W=[kT])
    for mt in range(2):
        for g in range(4):
            r = next_piece()
            P.dma("pool", r[:], wkv_v[:, g * 8:(g + 1) * 8, 512:1024], R=wkv_d, W=r)
            for j in range(8):
                c = g * 8 + j
                P.I("pe", "matmul", A[1][:], hT[:, c, mt * 128:(mt + 1) * 128], r[:, j, :], start=(c == 0), stop=(c == KC - 1),
                    R=[r, hT], W=[A[1]])
        C.copy("act", vv[:, mt, :], A[1][:], R=[A[1]], W=[vv])

    def load_ln(i):
        P.dma("sp", lnG[:], lnp_d.t[2 * i, :].partition_broadcast(128), R=lnp_d, W=lnG)
        P.dma("sp", lnB[:], lnp_d.t[2 * i + 1, :].partition_broadcast(128), R=lnp_d, W=lnB)

    def proj_residual(w_v, nk, src_T):
        for db in range(8):
            pieces = []
            if nk >= 8:
                for g in range(nk // 8):
                    r = next_piece()
                    P.dma("pool", r[:], w_v[:, g * 8:(g + 1) * 8, db * 512:(db + 1) * 512], R=dr["w_out"], W=r)
                    pieces.append(r)
            else:
                r = next_piece()
                P.dma("pool", r[:, 0:nk, :], w_v[:, 0:nk, db * 512:(db + 1) * 512], R=dr["xa_wo"], W=r)
                pieces.append(r)
            for tt in range(4):
                for c in range(nk):
                    r = pieces[c // 8]
                    P.I("pe", "matmul", A[tt][:], src_T[:, c, tt * 128:(tt + 1) * 128], r[:, c % 8, :],
                        start=(c == 0), stop=(c == nk - 1), R=[r, src_T], W=[A[tt]])
            for tt in range(4):
                P.I("dve", "scalar_tensor_tensor", out=hres[tt][:, db * 512:(db + 1) * 512],
                    in0=hres[tt][:, db * 512:(db + 1) * 512], scalar=DN_ALPHA, in1=A[tt][:],
                    op0=ALU.mult, op1=ALU.add, R=[hres[tt], A[tt]], W=[hres[tt]])

    def ln_and_transpose(i):
        load_ln(i)
        for tt in range(4):
            emit_layernorm(P, C, hres[tt], lnG, lnB, stats, mv, hb)
            emit_transpose_to_hT(P, C, hb, hT, tt, T)

    for blk in range(nblk):
        t0 = blk * TB
        P.dma("sp", hT[:], wview(yT_d.t)[:, :, t0:t0 + TB], R=yT_d, W=hT)
        for tt in range(4):
            P.dma("sp", hres[tt][:], hin_d.t[t0 + tt * 128:t0 + (tt + 1) * 128, :], R=hin_d, W=hres[tt])
        proj_residual(wview(wout_d.t), KC, hT)
        ln_and_transpose(0)
        wq_v = fview(wq_d.t)
        pieces = []
        for g in range(4):
            r = next_piece()
            P.dma("pool", r[:], wq_v[:, g * 8:(g + 1) * 8, :], R=wq_d, W=r)
            pieces.append(r)
        for hd in range(4):
            for c in range(KC):
                r = pieces[c // 8]
                P.I("pe", "matmul", A[hd][:], r[:, c % 8, hd * 128:(hd + 1) * 128], hT[:, c, :],
                    start=(c == 0), stop=(c == KC - 1), R=[r, hT], W=[A[hd]])
        for hd in range(4):
            C.copy(C.evac_engine(), qT[:, hd, :], A[hd][:], R=[A[hd]], W=[qT])
        for hd in range(4):
            for tt in range(4):
                sp_ = A[4 + (tt % 2)]
                P.I("pe", "matmul", sp_[:, 0:256], qT[:, hd, tt * 128:(tt + 1) * 128], kT[:, hd, :], start=True, stop=True,
                    R=[qT, kT], W=[sp_])
                P.I("dve", "reduce_max", out=red[:, 0:1], in_=sp_[:, 0:256], axis=AX.X, R=[sp_], W=[red])
                P.I("dve", "tensor_scalar", out=red[:, 1:2], in0=red[:, 0:1], scalar1=-(128.0 ** -0.5), scalar2=None,
                    op0=ALU.mult, R=[red], W=[red])
                P.I("act", "activation", out=sc[:], in_=sp_[:, 0:256], func=AF.Exp, bias=red[:, 1:2], scale=128.0 ** -0.5,
                    accum_out=red[:, 2:3], R=[sp_, red], W=[sc, red])
                P.I("dve", "reciprocal", out=red[:, 3:4], in_=red[:, 2:3], R=[red], W=[red])
                P.I("dve", "tensor_scalar", out=pb[:], in0=sc[:], scalar1=red[:, 3:4], scalar2=None, op0=ALU.mult,
                    R=[sc, red], W=[pb])
                tp = T[tt % 2]
                for mc in range(2):
                    P.I("pe", "transpose", out=tp[:, mc, :], in_=pb[:, mc * 128:(mc + 1) * 128], identity=C.ident_bf[:],
                        R=[pb, C.ident_bf], W=[tp])
                C.copy("act", PT[:, :, tt * 128:(tt + 1) * 128], tp[:, 0:2, :], R=[tp], W=[PT])
            for mc in range(2):
                P.I("pe", "matmul", A[hd][:], vv[:, mc, hd * 128:(hd + 1) * 128], PT[:, mc, :], start=(mc == 0), stop=(mc == 1),
                    R=[vv, PT], W=[A[hd]])
            C.copy("act", oT[:, hd, :], A[hd][:], R=[A[hd]], W=[oT])
        proj_residual(wview(wo_d.t), 4, oT)
        ln_and_transpose(1)
        for tt in range(4):
            lp = A[4 + (tt % 2)]
            for c in range(KC):
                P.I("pe", "matmul", lp[:, 0:NE], hT[:, c, tt * 128:(tt + 1) * 128], rws[:, c, :], start=(c == 0), stop=(c == KC - 1),
                    R=[hT, rws], W=[lp])
            P.I("dve", "tensor_tensor", out=lg[:], in0=lp[:, 0:NE], in1=rbt[:], op=ALU.add, R=[lp, rbt], W=[lg])
            P.I("dve", "max", out=top8[:], in_=lg[:], R=[lg], W=[top8])
            P.I("dve", "tensor_scalar", out=red[:, 4:5], in0=top8[:, 0:1], scalar1=-1.0, scalar2=None, op0=ALU.mult,
                R=[top8], W=[red])
            P.I("act", "activation", out=ex[:], in_=lg[:], func=AF.Exp, bias=red[:, 4:5], scale=1.0, R=[lg, red], W=[ex])
            P.I("dve", "tensor_scalar", out=msk[:], in0=lg[:], scalar1=top8[:, 3:4], scalar2=None, op0=ALU.is_ge,
                R=[lg, top8], W=[msk])
            P.I("dve", "tensor_tensor", out=ex[:], in0=ex[:], in1=msk[:], op=ALU.mult, R=[ex, msk], W=[ex])
            P.I("dve", "reduce_sum", out=red[:, 5:6], in_=ex[:], axis=AX.X, R=[ex], W=[red])
            P.I("dve", "reciprocal", out=red[:, 6:7], in_=red[:, 5:6], R=[red], W=[red])
            P.I("dve", "tensor_scalar", out=gate[tt][:], in0=ex[:], scalar1=red[:, 6:7], scalar2=None, op0=ALU.mult,
                R=[ex, red], W=[gate[tt]])
            P.I("pe", "transpose", out=lp[0:NE, 128:256], in_=gate[tt][:], identity=C.ident_f[:], R=[gate[tt], C.ident_f], W=[lp])
            C.copy("act", gateT[:, tt * 128:(tt + 1) * 128], lp[0:NE, 128:256], R=[lp], W=[gateT])
        for db in range(8):
            r = next_piece()
            P.dma("pool", r[0:NE, 0, :], bd_d.t[:, db * 512:(db + 1) * 512], R=bd_d, W=r)
            for tt in range(4):
                yp = A[4 + (tt % 2)]
                P.I("pe", "matmul", yp[:], gateT[:, tt * 128:(tt + 1) * 128], r[0:NE, 0, :], start=True, stop=True,
                    R=[gateT, r], W=[yp])
                P.I("dve", "scalar_tensor_tensor", out=hres[tt][:, db * 512:(db + 1) * 512],
                    in0=hres[tt][:, db * 512:(db + 1) * 512], scalar=DN_ALPHA, in1=yp[:],
                    op0=ALU.mult, op1=ALU.add, R=[hres[tt], yp], W=[hres[tt]])
        for half in range(2):
            e0 = half * 16
            nE = min(16, max(0, n_exp - e0))
            if nE == 0:
                continue
            for el in range(nE):
                e = e0 + el
                wg_v = fview(wg_d.t[e])
                wu_v = fview(wu_d.t[e])
                for g in range(4):
                    r = next_piece()
                    P.dma("pool", r[:, :, 0:256], wg_v[:, g * 8:(g + 1) * 8, :], R=wg_d, W=r)
                    P.dma("pool", r[:, :, 256:512], wu_v[:, g * 8:(g + 1) * 8, :], R=wu_d, W=r)
                    for j in range(8):
                        c = g * 8 + j
                        for m in range(4):
                            P.I("pe", "matmul", A[m][:], r[:, j, m * 128:(m + 1) * 128], hT[:, c, :],
                                start=(c == 0), stop=(c == KC - 1), R=[r, hT], W=[A[m]])
                gb = A[4 + e % 2]
                P.I("pe", "matmul", gb[:], sel[:, e, :], gateT[:], start=True, stop=True, R=[sel, gateT], W=[gb])
                for fc in range(2):
                    P.I("dve", "tensor_scalar", out=tgs[fc][:], in0=A[fc][:], scalar1=bgu[:, e * 4 + fc:e * 4 + fc + 1], scalar2=7.0,
                        op0=ALU.add, op1=ALU.min, R=[A[fc], bgu], W=[tgs[fc]])
                    P.I("act", "activation", out=tus[fc][:], in_=A[2 + fc][:], func=AF.Identity, bias=bgu[:, e * 4 + 2 + fc:e * 4 + 3 + fc],
                        scale=1.0, R=[A[2 + fc], bgu], W=[tus[fc]])
                for fc in range(2):
                    tg, tu = tgs[fc], tus[fc]
                    P.I("act", "activation", out=tsg[:], in_=tg[:], func=AF.Sigmoid, scale=1.702, R=[tg], W=[tsg])
                    P.I("dve", "tensor_scalar", out=tu[:], in0=tu[:], scalar1=7.0, scalar2=-7.0, op0=ALU.min, op1=ALU.max,
                        R=[tu], W=[tu])
                    P.I("dve", "tensor_tensor", out=tg[:], in0=tg[:], in1=tsg[:], op=ALU.mult, R=[tg, tsg], W=[tg])
                    P.I("dve", "tensor_tensor", out=tg[:], in0=tg[:], in1=gb[:], op=ALU.mult, R=[tg, gb], W=[tg])
                    P.I("dve", "scalar_tensor_tensor", out=actH[:, el, fc, :], in0=tu[:], scalar=1.0, in1=tg[:], op0=ALU.add, op1=ALU.mult,
                        R=[tu, tg], W=[actH])
            npc = (nE + 3) // 4
            for db in range(8):
                pcs = []
                for q in range(npc):
                    r = next_piece()
                    ne_q = min(4, nE - q * 4)
                    src = wd_d.t[e0 + q * 4:e0 + q * 4 + ne_q].rearrange("e (c p) n -> p (e c) n", p=128)[:, :, db * 512:(db + 1) * 512]
                    P.dma("pool", r[:, 0:2 * ne_q, :], src, R=wd_d, W=r)
                    pcs.append((r, ne_q))
                for q, (r, ne_q) in enumerate(pcs):
                    for tt in range(4):
                        yp = A[(db % 2) * 4 + tt]
                        for i in range(2 * ne_q):
                            el, fc = q * 4 + i // 2, i % 2
                            first = (q == 0 and i == 0)
                            last = (q == npc - 1 and i == 2 * ne_q - 1)
                            P.I("pe", "matmul", yp[:], actH[:, el, fc, tt * 128:(tt + 1) * 128], r[:, i, :], start=first, stop=last,
                                R=[actH, r], W=[yp])
                for tt in range(4):
                    yp = A[(db % 2) * 4 + tt]
                    P.I("dve", "tensor_tensor", out=hres[tt][:, db * 512:(db + 1) * 512], in0=yp[:],
                        in1=hres[tt][:, db * 512:(db + 1) * 512], op=ALU.add, R=[yp, hres[tt]], W=[hres[tt]])
        load_ln(2)
        for tt in range(4):
            emit_layernorm(P, C, hres[tt], lnG, lnB, stats, mv, hb)
            P.dma("sp", hout_d.t[t0 + tt * 128:t0 + (tt + 1) * 128, :], hres[tt][:], R=hres[tt], W=hout_d)
            emit_transpose_to_hT(P, C, hb, hT, tt, T)
        P.dma("sp", hTout_d.t[blk], hT[:].rearrange("p c t -> p (c t)"), R=hT, W=hTout_d)
    if emb:
        P.end_phase()
        return P
    P.finish([hout_d, hTout_d])
    P.close()
    return P


T_SEQ = 2048


class ACtx:
    pass


def a_setup(P, n_units, ncols, extra_in, io=None, pre="", emb=False):
    io = io or {}
    X = ACtx()
    X.P = P

    def din(name, shape, dt):
        return io[name] if name in io else P.dram(pre + name, shape, dt, "ExternalInput")
    X.hT_d = din("hT", [T_SEQ // 512, 128, KC * 512], BF16)
    X.w_d = din("w_units", [n_units, D, ncols], F32)
    idb_d = din("ident_bf", [128, 128], BF16)
    idf_d = din("ident_f", [128, 128], F32)
    X.ex = {}
    for name, shape, dt in extra_in:
        X.ex[name] = din(name, shape, dt)
    X.yT_d = io["yT_out"] if "yT_out" in io else P.dram("yT_out", [n_units * 128, T_SEQ], BF16, "ExternalOutput")
    if emb:
        P.begin_phase()
    X.C = Common(P, idb_d, idf_d)
    X.hTb = [P.sb([128, KC, 512], BF16, f"hTb{i}") for i in range(2)]
    X.W = P.sb([128, KC, ncols], BF16, "Wunit")
    X.nload = 0
    return X


def a_inproj(X, u, ncols, sinks, A):
    P = X.P
    wv = X.w_d.t[u].rearrange("(p c) n -> p c n", c=KC)
    for g in range(4):
        P.dma("pool", X.W[:, g * 8:(g + 1) * 8, :], wv[:, g * 8:(g + 1) * 8, :], R=X.w_d, W=X.W)
    nb = ncols // 128
    for blk in range(4):
        hb_ = X.hTb[X.nload % 2]
        X.nload += 1
        P.dma("sp", hb_[:].rearrange("p c t -> p (c t)"), X.hT_d.t[blk], R=X.hT_d, W=hb_)
        for j in range(nb):
            ps = A[j % len(A)]
            for c in range(KC):
                P.I("pe", "matmul", ps[:], X.W[:, c, j * 128:(j + 1) * 128], hb_[:, c, :], start=(c == 0), stop=(c == KC - 1),
                    R=[X.W, hb_], W=[ps])
            sinks[j](blk, ps)


def a_to_tokmajor(X, srcT, dst, T):
    P = X.P
    for g in range(2):
        tp = T[g % len(T)]
        for j in range(8):
            t = g * 8 + j
            P.I("pe", "transpose", out=tp[:, j, :], in_=srcT[:, t * 128:(t + 1) * 128], identity=X.C.ident_bf[:],
                R=[srcT, X.C.ident_bf], W=[tp])
        X.C.copy(X.C.evac_engine(), dst[:, g * 8:(g + 1) * 8, :], tp[:], R=[tp], W=[dst])


def build_A_odd(nh=8, P=None, io=None, pre=""):
    emb = P is not None
    if not emb:
        P = Prog()
    n_units = 2 * nh
    X = a_setup(P, n_units, 384, [("bias_ext", [nh, 64, 704], F32), ("tril", [128, 128], F32)], io, pre, emb)
    C = X.C
    scale = 128.0 ** -0.5
    A = [P.ps([128, 512], F32, f"A{i}") for i in range(4)]
    S2 = P.ps([128, 1024], F32, "S2")
    T = [P.ps([128, 8, 128], BF16, f"T{i}") for i in range(2)]

    qT = P.sb([128, T_SEQ], BF16, "qT")
    kT = P.sb([128, T_SEQ], BF16, "kT")
    vT = P.sb([128, T_SEQ], BF16, "vT")
    v = P.sb([128, 16, 128], BF16, "v")
    yT = [P.sb([128, T_SEQ], BF16, f"yT{i}") for i in range(2)]
    tril = P.sb([128, 128], F32, "tril")
    P.dma("sp", tril[:], X.ex["tril"][:], R=X.ex["tril"], W=tril)
    one = P.sb([128, 1], F32, "one")
    P.I("dve", "memset", one[:], 1.0, W=[one])
    red = P.sb([128, 8], F32, "red")

    def mk_sinks():
        def sq(blk, ps):
            P.I("act", "activation", out=qT[:, blk * 512:(blk + 1) * 512], in_=ps[:], func=AF.Copy, scale=scale, R=[ps], W=[qT])

        def sk(blk, ps):
            P.I("dve", "tensor_copy", out=kT[:, blk * 512:(blk + 1) * 512], in_=ps[:], R=[ps], W=[kT])

        def sv(blk, ps):
            P.I("act", "activation", out=vT[:, blk * 512:(blk + 1) * 512], in_=ps[:], func=AF.Copy, R=[ps], W=[vT])
        return [sq, sk, sv]

    bias = P.sb([64, 704], F32, "bias")
    s_sb = P.sb([64, 640], F32, "s_sb")
    Pb = [P.sb([64, 640], BF16, f"Pb{i}") for i in range(2)]
    pe_ = P.sb([64, 640], F32, "pexp")
    PTb = P.sb([128, 5, 64], BF16, "PTb")
    for i in range(2):
        P.I("dve", "memset", Pb[i][:], 0.0, W=[Pb[i]])
    for hl in range(nh):
        u = hl
        a_inproj(X, u, 384, mk_sinks(), A[0:3])
        a_to_tokmajor(X, vT, v, T)
        P.dma("sp", bias[:], X.ex["bias_ext"].t[hl], R=X.ex["bias_ext"], W=bias)
        yo = yT[u % 2]
        for c in range(32):
            par = c % 2
            wb0 = (c - 8) // 2
            lo_band, hi_band = (0, 576) if par == 0 else (64, 640)
            lo = max(lo_band, max(0, -wb0) * 128)
            hi = hi_band
            k0 = wb0 * 128
            for (a, b_) in ((lo, min(hi, 512)), (max(lo, 512), hi)):
                if b_ > a:
                    P.I("pe", "matmul", S2[0:64, a:b_], qT[:, c * 64:(c + 1) * 64], kT[:, k0 + a:k0 + b_], start=True, stop=True,
                        R=[qT, kT], W=[S2])
            boff = 64 if par == 0 else 0
            P.I("dve", "tensor_tensor", out=s_sb[:, lo:hi], in0=S2[0:64, lo:hi], in1=bias[:, boff + lo:boff + hi], op=ALU.add,
                R=[S2, bias], W=[s_sb])
            P.I("dve", "reduce_max", out=red[0:64, 0:1], in_=s_sb[:, lo:hi], axis=AX.X, R=[s_sb], W=[red])
            P.I("dve", "tensor_scalar", out=red[0:64, 1:2], in0=red[0:64, 0:1], scalar1=-1.0, scalar2=None, op0=ALU.mult, R=[red], W=[red])
            P.I("act", "activation", out=pe_[:, lo:hi], in_=s_sb[:, lo:hi], func=AF.Exp, bias=red[0:64, 1:2], scale=1.0,
                accum_out=red[0:64, 2:3], R=[s_sb, red], W=[pe_, red])
            P.I("dve", "reciprocal", out=red[0:64, 3:4], in_=red[0:64, 2:3], R=[red], W=[red])
            pbuf = Pb[par]
            P.I("dve", "tensor_scalar", out=pbuf[:, lo:hi], in0=pe_[:, lo:hi], scalar1=red[0:64, 3:4], scalar2=None, op0=ALU.mult,
                R=[pe_, red], W=[pbuf])
            wbs = [w for w in range(5) if wb0 + w >= 0]
            tp = T[c % 2]
            for w in wbs:
                P.I("pe", "transpose", out=tp[:, w, 0:64], in_=pbuf[:, w * 128:(w + 1) * 128], identity=C.ident_bf[0:64, 0:64],
                    R=[pbuf, C.ident_bf], W=[tp])
            C.copy("act", PTb[:, wbs[0]:5, :], tp[:, wbs[0]:5, 0:64], R=[tp], W=[PTb])
            op_ = A[3]
            for i, w in enumerate(wbs):
                P.I("pe", "matmul", op_[:, 0:64], v[:, wb0 + w, :], PTb[:, w, :], start=(i == 0), stop=(i == len(wbs) - 1),
                    R=[v, PTb], W=[op_])
            C.copy("dve", yo[:, c * 64:(c + 1) * 64], op_[:, 0:64], R=[op_], W=[yo])
        P.dma("sp", X.yT_d.t[u * 128:(u + 1) * 128, :], yo[:], R=yo, W=X.yT_d)

    E = P.sb([128, 512], F32, "E")
    SP = P.sb([128, 512], F32, "SP")
    Cc = P.sb([128, 512], F32, "Cc")
    tt_ = P.sb([128, T_SEQ], F32, "tt")
    Ab = P.sb([128, 512], BF16, "Ab")
    AT = P.sb([128, 4, 128], BF16, "AT")
    carry = P.sb([128, 2], F32, "carry")
    onesrow = P.sb([128, 512], F32, "onesrow")
    P.I("dve", "memset", onesrow[:], 1.0, W=[onesrow])
    for hl in range(nh):
        u = nh + hl
        a_inproj(X, u, 384, mk_sinks(), A[0:3])
        a_to_tokmajor(X, vT, v, T)
        yo = yT[u % 2]
        for qi in range(16):
            nkb = qi + 1
            nch = (nkb + 3) // 4
            for ch in range(nch):
                kb0 = ch * 4
                nb_ = min(4, nkb - kb0)
                w_ = nb_ * 128
                Z = A[ch % 2]
                P.I("pe", "matmul", Z[:, 0:w_], qT[:, qi * 128:(qi + 1) * 128], kT[:, kb0 * 128:kb0 * 128 + w_], start=True, stop=True,
                    R=[qT, kT], W=[Z])
                P.I("act", "activation", out=E[:, 0:w_], in_=Z[:, 0:w_], func=AF.Exp, R=[Z], W=[E])
                P.I("act", "activation", out=SP[:, 0:w_], in_=E[:, 0:w_], func=AF.Ln, bias=one[:], scale=1.0, R=[E, one], W=[SP])
                last = (ch == nch - 1)
                if last:
                    d0 = (nb_ - 1) * 128
                    P.I("dve", "tensor_tensor", out=SP[:, d0:d0 + 128], in0=SP[:, d0:d0 + 128], in1=tril[:], op=ALU.mult,
                        R=[SP, tril], W=[SP])
                init = 0.0 if ch == 0 else carry[:, 0:1]
                rd_ = [onesrow, SP] + ([] if ch == 0 else [carry])
                P.I("dve", "tensor_tensor_scan", out=Cc[:, 0:w_], data0=onesrow[:, 0:w_], data1=SP[:, 0:w_], initial=init,
                    op0=ALU.mult, op1=ALU.add, R=rd_, W=[Cc])
                P.I("dve", "tensor_copy", out=carry[:, 0:1], in_=Cc[:, w_ - 1:w_], R=[Cc], W=[carry])
                P.I("dve", "tensor_tensor", out=E[:, 0:w_], in0=Z[:, 0:w_], in1=SP[:, 0:w_], op=ALU.subtract, R=[Z, SP], W=[E])
                P.I("dve", "tensor_tensor", out=tt_[:, kb0 * 128:kb0 * 128 + w_], in0=E[:, 0:w_], in1=Cc[:, 0:w_], op=ALU.add,
                    R=[E, Cc], W=[tt_])
            P.I("dve", "tensor_scalar", out=carry[:, 1:2], in0=carry[:, 0:1], scalar1=-1.0, scalar2=None, op0=ALU.mult, R=[carry], W=[carry])
            op_ = A[2 + (qi % 2)]
            for ch in range(nch):
                kb0 = ch * 4
                nb_ = min(4, nkb - kb0)
                w_ = nb_ * 128
                P.I("act", "activation", out=Ab[:, 0:w_], in_=tt_[:, kb0 * 128:kb0 * 128 + w_], func=AF.Exp, bias=carry[:, 1:2], scale=1.0,
                    R=[tt_, carry], W=[Ab])
                if ch == nch - 1:
                    d0 = (nb_ - 1) * 128
                    P.I("dve", "tensor_tensor", out=Ab[:, d0:d0 + 128], in0=Ab[:, d0:d0 + 128], in1=tril[:], op=ALU.mult,
                        R=[Ab, tril], W=[Ab])
                tp = T[ch % 2]
                for j in range(nb_):
                    P.I("pe", "transpose", out=tp[:, j, :], in_=Ab[:, j * 128:(j + 1) * 128], identity=C.ident_bf[:],
                        R=[Ab, C.ident_bf], W=[tp])
                C.copy("act", AT[:, 0:nb_, :], tp[:, 0:nb_, :], R=[tp], W=[AT])
                for j in range(nb_):
                    kb = kb0 + j
                    P.I("pe", "matmul", op_[:, 0:128], v[:, kb, :], AT[:, j, :], start=(kb == 0), stop=(kb == nkb - 1),
                        R=[v, AT], W=[op_])
            C.copy("dve", yo[:, qi * 128:(qi + 1) * 128], op_[:, 0:128], R=[op_], W=[yo])
        P.dma("sp", X.yT_d.t[u * 128:(u + 1) * 128, :], yo[:], R=yo, W=X.yT_d)
    if emb:
        P.end_phase()
        return P
    P.finish([X.yT_d])
    P.close()
    return P


def build_A_even(nh=8, P=None, io=None, pre=""):
    emb = P is not None
    if not emb:
        P = Prog()
    n_units = 2 * nh
    extra = [("ropec", [128, T_SEQ], F32), ("ropes", [128, T_SEQ], F32), ("perm", [128, 128], BF16),
             ("DT", [nh, 128, 128], F32), ("qdec4", [nh, 512], F32), ("kdec", [128, nh], F32), ("cdec", [128, nh], F32),
             ("retg", [128, nh], F32), ("mlg", [128, nh], F32), ("convw", [128, nh * 5], F32),
             ("wqk", [nh, 2, 128, 128], F32), ("wgate", [2, D, nh], F32), ("gateb", [nh, 2], F32),
             ("triuT", [128, 128], F32), ("e127", [128, 128], F32)]
    X = a_setup(P, n_units, 512, extra, io, pre, emb)
    C = X.C
    scale = 128.0 ** -0.5
    A = [P.ps([128, 512], F32, f"A{i}") for i in range(6)]
    T = [P.ps([128, 8, 128], BF16, f"T{i}") for i in range(2)]

    def ld(name, shape, dt=F32, src=None, q="sp"):
        b = P.sb(shape, dt, name)
        P.dma(q, b[:], (src if src is not None else X.ex[name][:]), R=X.ex[name], W=b)
        return b

    ropec = ld("ropec", [128, T_SEQ])
    ropes = ld("ropes", [128, T_SEQ])
    perm = ld("perm", [128, 128], BF16)
    kdec = ld("kdec", [128, nh])
    cdec = ld("cdec", [128, nh])
    retg = ld("retg", [128, nh])
    mlg = ld("mlg", [128, nh])
    convw = ld("convw", [128, nh * 5])
    triuT = ld("triuT", [128, 128])
    e127 = ld("e127", [128, 128])
    gateb = ld("gateb", [nh, 2])
    DT = P.sb([128, 128], F32, "DT")
    qdec = P.sb([128, 512], F32, "qdec")

    z0 = P.sb([128, T_SEQ], BF16, "z0")
    z1 = P.sb([128, T_SEQ], BF16, "z1")
    z2 = P.sb([128, T_SEQ], BF16, "z2")
    z3 = P.sb([128, T_SEQ], BF16, "z3")
    qd = P.sb([128, T_SEQ], BF16, "qd")
    xf = P.sb([128, T_SEQ + 4], F32, "xf")
    acc = P.sb([128, T_SEQ], F32, "acc")
    vtok = P.sb([128, 16, 132], BF16, "vtok")
    ktok = P.sb([128, 16, 128], BF16, "ktok")
    gtok = P.sb([128, 16, 128], BF16, "gtok")
    yT = [P.sb([128, T_SEQ], BF16, f"yT{i}") for i in range(2)]
    S = P.sb([128, 132], F32, "S")
    Sb = P.sb([128, 132], BF16, "Sb")
    PTt = P.sb([128, 128], BF16, "PTt")
    ot = P.sb([128, 128], F32, "ot")
    onb = P.sb([128, 128], BF16, "onb")
    stats = P.sb([128, 6], F32, "stats")
    mv = P.sb([128, 8], F32, "mv")
    t1 = P.sb([128, 512], F32, "t1")
    t2 = P.sb([128, 512], F32, "t2")
    P.I("dve", "memset", vtok[:], 1.0, W=[vtok])
    P.I("dve", "memset", xf[:], 0.0, W=[xf])

    def rope_sink(dst, sc_):
        def f(blk, ps):
            sl = slice(blk * 512, (blk + 1) * 512)
            P.I("act", "activation", out=z3[:, sl], in_=ps[:], func=AF.Copy, scale=sc_, R=[ps], W=[z3])
            sw = A[4 + blk % 2]
            P.I("pe", "matmul", sw[:], perm[:], z3[:, sl], start=True, stop=True, R=[perm, z3], W=[sw])
            P.I("dve", "tensor_tensor", out=t1[:], in0=z3[:, sl], in1=ropec[:, sl], op=ALU.mult, R=[z3, ropec], W=[t1])
            P.I("dve", "tensor_tensor", out=t2[:], in0=sw[:], in1=ropes[:, sl], op=ALU.mult, R=[sw, ropes], W=[t2])
            P.I("dve", "tensor_tensor", out=dst[:, sl], in0=t1[:], in1=t2[:], op=ALU.add, R=[t1, t2], W=[dst])
        return f

    def plain_sink(dst, func=AF.Copy):
        def f(blk, ps):
            P.I("act", "activation", out=dst[:, blk * 512:(blk + 1) * 512], in_=ps[:], func=func, R=[ps], W=[dst])
        return f

    def head_norm_out(src_ps_or_sb, R_, gcol, yo, tok0):
        P.I("dve", "bn_stats", out=stats[:], in_=src_ps_or_sb, R=R_, W=[stats])
        P.I("dve", "bn_aggr", out=mv[:, 0:2], in_=stats[:], R=[stats], W=[mv])
        P.I("act", "activation", out=mv[:, 2:3], in_=mv[:, 1:2], func=AF.Sqrt, bias=C.eps_hn[:], scale=1.0, R=[mv, C.eps_hn], W=[mv])
        P.I("dve", "reciprocal", out=mv[:, 3:4], in_=mv[:, 2:3], R=[mv], W=[mv])
        P.I("dve", "tensor_scalar", out=onb[:], in0=src_ps_or_sb, scalar1=mv[:, 0:1], scalar2=mv[:, 3:4], op0=ALU.subtract, op1=ALU.mult,
            R=list(R_) + [mv], W=[onb])
        tp = T[0]
        P.I("pe", "transpose", out=tp[:, 0, :], in_=onb[:], identity=C.ident_bf[:], R=[onb, C.ident_bf], W=[tp])
        return tp

    for hl in range(nh):
        u = hl
        a_inproj(X, u, 512, [rope_sink(z0, scale), rope_sink(z1, 1.0), plain_sink(z2), plain_sink(acc, AF.Silu)], A[0:4])
        qT_, kT_, vT_ = z0, z1, z2
        P.dma("sp", DT[:], X.ex["DT"].t[hl], R=X.ex["DT"], W=DT)
        P.dma("sp", qdec[:], X.ex["qdec4"].t[hl, :].partition_broadcast(128), R=X.ex["qdec4"], W=qdec)
        for blk in range(4):
            sl = slice(blk * 512, (blk + 1) * 512)
            P.I("dve", "tensor_tensor", out=qd[:, sl], in0=qT_[:, sl], in1=qdec[:], op=ALU.mult, R=[qT_, qdec], W=[qd])
        for g in range(2):
            tp = T[g % 2]
            for j in range(8):
                t = g * 8 + j
                P.I("pe", "transpose", out=tp[:, j, :], in_=vT_[:, t * 128:(t + 1) * 128], identity=C.ident_bf[:], R=[vT_, C.ident_bf], W=[tp])
            C.copy("dve", vtok[:, g * 8:(g + 1) * 8, 0:128], tp[:], R=[tp], W=[vtok])
        for g in range(2):
            tp = T[g % 2]
            for j in range(8):
                t = g * 8 + j
                P.I("pe", "transpose", out=tp[:, j, :], in_=kT_[:, t * 128:(t + 1) * 128], identity=C.ident_bf[:], R=[kT_, C.ident_bf], W=[tp])
            P.I("act", "activation", out=ktok[:, g * 8:(g + 1) * 8, :], in_=tp[:], func=AF.Copy, scale=kdec[:, hl:hl + 1], R=[tp, kdec], W=[ktok])
        P.I("dve", "memset", S[:], 0.0, W=[S])
        P.I("dve", "memset", Sb[:], 0.0, W=[Sb])
        yo = yT[u % 2]
        for c in range(16):
            cs = slice(c * 128, (c + 1) * 128)
            sp_ = A[c % 2]
            P.I("pe", "matmul", sp_[:, 0:128], kT_[:, cs], qT_[:, cs], start=True, stop=True, R=[kT_, qT_], W=[sp_])
            P.I("dve", "tensor_tensor", out=PTt[:], in0=sp_[:, 0:128], in1=DT[:], op=ALU.mult, R=[sp_, DT], W=[PTt])
            op_ = A[2 + c % 2]
            P.I("pe", "matmul", op_[:, 0:128], PTt[:], vtok[:, c, 0:128], start=True, stop=False, R=[PTt, vtok], W=[op_])
            P.I("pe", "matmul", op_[:, 0:128], qd[:, cs], Sb[:, 0:128], start=False, stop=True, R=[qd, Sb], W=[op_])
            stp = A[4 + c % 2]
            P.I("pe", "matmul", stp[:, 0:128], ktok[:, c, :], vtok[:, c, 0:128], start=True, stop=True, R=[ktok, vtok], W=[stp])
            P.I("dve", "scalar_tensor_tensor", out=S[:, 0:128], in0=S[:, 0:128], scalar=cdec[:, hl:hl + 1], in1=stp[:, 0:128],
                op0=ALU.mult, op1=ALU.add, R=[S, cdec, stp], W=[S])
            P.I("act", "activation", out=Sb[:, 0:128], in_=S[:, 0:128], func=AF.Copy, R=[S], W=[Sb])
            tp = head_norm_out(op_[:, 0:128], [op_], None, yo, c * 128)
            P.I("dve", "scalar_tensor_tensor", out=yo[:, cs], in0=tp[:, 0, :], scalar=retg[:, hl:hl + 1], in1=acc[:, cs],
                op0=ALU.mult, op1=ALU.mult, R=[tp, retg, acc], W=[yo])
        P.dma("sp", X.yT_d.t[u * 128:(u + 1) * 128, :], yo[:], R=yo, W=X.yT_d)

    class V:
        def __init__(self, buf):
            self.buf = buf

        def __getitem__(self, idx):
            if not isinstance(idx, tuple):
                idx = (idx,)
            return self.buf.t[(slice(0, nh),) + tuple(idx[1:])]
    IG, FG, Rf, ones8 = V(ropec), V(ropes), V(xf), V(acc)
    Gm = P.sb([nh, T_SEQ], F32, "Gm")
    one8 = P.sb([nh, 1], F32, "one8")
    ngb = P.sb([nh, 2], F32, "ngb")
    wgs = P.sb([128, 2, KC, nh], BF16, "wgs")
    AL = slice(0, T_SEQ)
    P.I("dve", "memset", ones8[:, AL], 1.0, W=[acc])
    P.I("dve", "memset", one8[:], 1.0, W=[one8])
    P.I("dve", "tensor_scalar", out=ngb[:], in0=gateb[:], scalar1=-1.0, scalar2=None, op0=ALU.mult, R=[gateb], W=[ngb])
    for gi in range(2):
        P.dma("pool", wgs[:, gi, :, :], X.ex["wgate"].t[gi].rearrange("(p c) n -> p c n", c=KC), R=X.ex["wgate"], W=wgs)
    for blk in range(4):
        hb_ = X.hTb[X.nload % 2]
        X.nload += 1
        P.dma("sp", hb_[:].rearrange("p c t -> p (c t)"), X.hT_d.t[blk], R=X.hT_d, W=hb_)
        sl = slice(blk * 512, (blk + 1) * 512)
        for gi in range(2):
            ps = A[gi]
            for c in range(KC):
                P.I("pe", "matmul", ps[0:nh, :], wgs[:, gi, c, :], hb_[:, c, :], start=(c == 0), stop=(c == KC - 1), R=[wgs, hb_], W=[ps])
        P.I("act", "activation", out=IG[:, sl], in_=A[0][0:nh, :], func=AF.Identity, bias=gateb[:, 0:1], scale=1.0, R=[A[0], gateb], W=[ropec])
        P.I("act", "activation", out=FG[:, sl], in_=A[1][0:nh, :], func=AF.Exp, bias=ngb[:, 1:2], scale=-1.0, R=[A[1], ngb], W=[ropes])
    P.I("act", "activation", out=FG[:, AL], in_=FG[:, AL], func=AF.Ln, bias=one8[:], scale=1.0, R=[ropes, one8], W=[ropes])
    P.I("dve", "tensor_tensor_scan", out=FG[:, AL], data0=ones8[:, AL], data1=FG[:, AL], initial=0.0, op0=ALU.mult, op1=ALU.add,
        R=[acc, ropes], W=[ropes])
    P.I("dve", "tensor_tensor", out=IG[:, AL], in0=IG[:, AL], in1=FG[:, AL], op=ALU.add, R=[ropec, ropes], W=[ropec])
    P.I("dve", "tensor_tensor_scan", out=Gm[:], data0=IG[:, AL], data1=IG[:, AL], initial=0.0, op0=ALU.max, op1=ALU.max, R=[ropec], W=[Gm])
    G3 = Gm[:].rearrange("p (c l) -> p c l", l=128)
    R3 = Rf[:, AL].rearrange("p (c l) -> p c l", l=128)
    P.I("dve", "memset", Rf[:, 0:128], 0.0, W=[xf])
    P.I("dve", "tensor_copy", out=R3[:, 1:16, :], in_=G3[:, 0:15, 127:128].to_broadcast([nh, 15, 128]), R=[Gm], W=[xf])
    P.I("dve", "tensor_tensor", out=IG[:, AL], in0=IG[:, AL], in1=Rf[:, AL], op=ALU.subtract, R=[ropec, xf], W=[ropec])
    P.I("act", "activation", out=IG[:, AL], in_=IG[:, AL], func=AF.Exp, R=[ropec], W=[ropec])
    P.I("dve", "tensor_tensor", out=Rf[:, AL], in0=Rf[:, AL], in1=Gm[:], op=ALU.subtract, R=[xf, Gm], W=[xf])
    P.I("act", "activation", out=Rf[:, AL], in_=Rf[:, AL], func=AF.Exp, R=[xf], W=[xf])
    P.I("dve", "tensor_tensor", out=FG[:, AL], in0=FG[:, AL], in1=Gm[:], op=ALU.subtract, R=[ropes, Gm], W=[ropes])
    P.I("act", "activation", out=FG[:, AL], in_=FG[:, AL], func=AF.Exp, R=[ropes], W=[ropes])
    qa, qr, qe = IG, Rf, FG
    qpar = {id(IG): ropec, id(Rf): xf, id(FG): ropes}
    qtok = [P.sb([128, 16, nh], F32, f"qtok{i}") for i in range(3)]
    for qi_, src in enumerate((qa, qr, qe)):
        ps = A[2 + qi_ % 2]
        for c in range(16):
            P.I("pe", "transpose", out=ps[:, c * nh:(c + 1) * nh], in_=src[:, c * 128:(c + 1) * 128], identity=C.ident_f[0:nh, 0:nh],
                R=[qpar[id(src)], C.ident_f], W=[ps])
        C.copy("dve", qtok[qi_][:], ps[:, 0:16 * nh].rearrange("p (c h) -> p c h", h=nh), R=[ps], W=[qtok[qi_]])
    a_tok, r_tok, e_tok = qtok
    decb = P.sb([128, 16, nh], F32, "decb")
    P.I("dve", "memset", xf[:], 0.0, R=[], W=[xf])
    P.I("pe", "matmul", A[4][:, 0:16 * nh], e127[:], r_tok[:].rearrange("p c h -> p (c h)"), start=True, stop=True, R=[e127, r_tok], W=[A[4]])
    C.copy("dve", decb[:], A[4][:, 0:16 * nh].rearrange("p (c h) -> p c h", h=nh), R=[A[4]], W=[decb])

    wq_s = P.sb([128, 128], BF16, "wq_s")
    wk_s = P.sb([128, 128], BF16, "wk_s")
    ka = ktok
    den = P.sb([128, 8], F32, "den")
    for hl in range(nh):
        u = nh + hl
        cw = convw[:, hl * 5:(hl + 1) * 5]

        def mu_sink(blk, ps):
            P.I("act", "activation", out=xf[:, 3 + blk * 512:3 + (blk + 1) * 512], in_=ps[:], func=AF.Copy, R=[ps], W=[xf])
        a_inproj(X, u, 384, [mu_sink, plain_sink(z2), plain_sink(z3, AF.Sigmoid)], A[0:3])
        P.dma("pool", wq_s[:], X.ex["wqk"].t[hl, 0], R=X.ex["wqk"], W=wq_s)
        P.dma("pool", wk_s[:], X.ex["wqk"].t[hl, 1], R=X.ex["wqk"], W=wk_s)
        P.I("dve", "tensor_scalar", out=acc[:], in0=xf[:, 3:3 + T_SEQ], scalar1=cw[:, 3:4], scalar2=cw[:, 4:5], op0=ALU.mult, op1=ALU.add,
            R=[xf, convw], W=[acc])
        for i in range(3):
            P.I("dve", "scalar_tensor_tensor", out=acc[:], in0=xf[:, i:i + T_SEQ], scalar=cw[:, i:i + 1], in1=acc[:], op0=ALU.mult, op1=ALU.add,
                R=[xf, convw, acc], W=[acc])
        P.I("act", "activation", out=z0[:], in_=acc[:], func=AF.Silu, R=[acc], W=[z0])
        uT = z0
        for blk in range(4):
            sl = slice(blk * 512, (blk + 1) * 512)
            P.I("pe", "matmul", A[0][:], wq_s[:], uT[:, sl], start=True, stop=True, R=[wq_s, uT], W=[A[0]])
            C.copy("act", z1[:, sl], A[0][:], R=[A[0]], W=[z1])
            P.I("pe", "matmul", A[1][:], wk_s[:], uT[:, sl], start=True, stop=True, R=[wk_s, uT], W=[A[1]])
            P.I("act", "activation", out=qd[:, sl], in_=A[1][:], func=AF.Copy, scale=scale, R=[A[1]], W=[qd])
        qmT, kmT = z1, qd
        for t in range(16):
            ps = A[2 + t % 2]
            P.I("pe", "matmul", ps[:, 0:128], uT[:, t * 128:(t + 1) * 128], wk_s[:], start=True, stop=True, R=[uT, wk_s], W=[ps])
            P.I("dve", "tensor_scalar", out=ka[:, t, :], in0=ps[:, 0:128], scalar1=a_tok[:, t, hl:hl + 1], scalar2=scale, op0=ALU.mult, op1=ALU.mult,
                R=[ps, a_tok], W=[ka])
        for src, dst in ((z2, vtok), (z3, gtok)):
            for g in range(2):
                tp = T[g % 2]
                for j in range(8):
                    t = g * 8 + j
                    P.I("pe", "transpose", out=tp[:, j, :], in_=src[:, t * 128:(t + 1) * 128], identity=C.ident_bf[:], R=[src, C.ident_bf], W=[tp])
                C.copy("dve" if dst is vtok else "act", dst[:, g * 8:(g + 1) * 8, 0:128], tp[:], R=[tp], W=[dst])
        P.I("dve", "memset", S[:], 0.0, W=[S])
        P.I("dve", "memset", Sb[:], 0.0, W=[Sb])
        yo = yT[u % 2]
        for c in range(16):
            cs = slice(c * 128, (c + 1) * 128)
            sp_ = A[c % 2]
            P.I("pe", "matmul", sp_[:, 0:128], kmT[:, cs], qmT[:, cs], start=True, stop=True, R=[kmT, qmT], W=[sp_])
            P.I("dve", "scalar_tensor_tensor", out=PTt[:], in0=sp_[:, 0:128], scalar=a_tok[:, c, hl:hl + 1], in1=triuT[:], op0=ALU.mult, op1=ALU.mult,
                R=[sp_, a_tok, triuT], W=[PTt])
            op_ = A[2 + c % 2]
            P.I("pe", "matmul", op_[:, 0:129], PTt[:], vtok[:, c, 0:129], start=True, stop=False, R=[PTt, vtok], W=[op_])
            P.I("pe", "matmul", op_[:, 0:129], qmT[:, cs], Sb[:, 0:129], start=False, stop=True, R=[qmT, Sb], W=[op_])
            stp = A[4 + c % 2]
            P.I("pe", "matmul", stp[:, 0:129], ka[:, c, :], vtok[:, c, 0:129], start=True, stop=True, R=[ka, vtok], W=[stp])
            P.I("dve", "tensor_tensor", out=S[:, 0:129], in0=S[:, 0:129], in1=stp[:, 0:129], op=ALU.add, R=[S, stp], W=[S])
            P.I("dve", "tensor_scalar", out=S[:, 0:129], in0=S[:, 0:129], scalar1=decb[:, c, hl:hl + 1], scalar2=None, op0=ALU.mult, R=[S, decb], W=[S])
            P.I("act", "activation", out=Sb[:, 0:129], in_=S[:, 0:129], func=AF.Copy, R=[S], W=[Sb])
            P.I("dve", "tensor_scalar", out=den[:, 0:1], in0=op_[:, 128:129], scalar1=r_tok[:, c, hl:hl + 1], scalar2=None, op0=ALU.mult,
                R=[op_, r_tok], W=[den])
            P.I("dve", "tensor_scalar", out=den[:, 4:5], in0=den[:, 0:1], scalar1=-1.0, scalar2=None, op0=ALU.mult, R=[den], W=[den])
            P.I("dve", "tensor_tensor", out=den[:, 0:1], in0=den[:, 0:1], in1=den[:, 4:5], op=ALU.max, R=[den], W=[den])
            P.I("dve", "tensor_tensor", out=den[:, 1:2], in0=den[:, 0:1], in1=e_tok[:, c, hl:hl + 1], op=ALU.max, R=[den, e_tok], W=[den])
            P.I("dve", "reciprocal", out=den[:, 2:3], in_=den[:, 1:2], R=[den], W=[den])
            P.I("dve", "tensor_tensor", out=den[:, 3:4], in0=den[:, 2:3], in1=r_tok[:, c, hl:hl + 1], op=ALU.mult, R=[den, r_tok], W=[den])
            P.I("dve", "scalar_tensor_tensor", out=ot[:], in0=op_[:, 0:128], scalar=den[:, 3:4], in1=gtok[:, c, :], op0=ALU.mult, op1=ALU.mult,
                R=[op_, den, gtok], W=[ot])
            tp = head_norm_out(ot[:], [ot], None, yo, c * 128)
            P.I("act", "activation", out=yo[:, cs], in_=tp[:, 0, :], func=AF.Copy, scale=mlg[:, hl:hl + 1], R=[tp, mlg], W=[yo])
        P.dma("sp", X.yT_d.t[u * 128:(u + 1) * 128, :], yo[:], R=yo, W=X.yT_d)
    if emb:
        P.end_phase()
        return P
    P.finish([X.yT_d])
    P.close()
    return P


def build_P0(ntok=1024, P=None, io=None):
    emb = P is not None
    if not emb:
        P = Prog()
    io = io or {}
    x_d = io["x"] if "x" in io else P.dram("x", [ntok, D], F32, "ExternalInput")
    idb_d = io["ident_bf"] if "ident_bf" in io else P.dram("ident_bf", [128, 128], BF16, "ExternalInput")
    idf_d = io["ident_f"] if "ident_f" in io else P.dram("ident_f", [128, 128], F32, "ExternalInput")
    hT_d = io["hT_out"] if "hT_out" in io else P.dram("hT_out", [ntok // 512, 128, KC * 512], BF16, "ExternalOutput")
    if emb:
        P.begin_phase()
    C = Common(P, idb_d, idf_d)
    xs = [P.sb([128, D], F32, f"xs{i}") for i in range(2)]
    hb = [P.sb([128, D], BF16, f"hb{i}") for i in range(2)]
    hT = P.sb([128, KC, 512], BF16, "hT")
    T = [P.ps([128, 8, 128], BF16, f"T{i}") for i in range(2)]
    for blk in range(ntok // 512):
        for tt in range(4):
            t = blk * 4 + tt
            P.dma("sp", xs[t % 2][:], x_d.t[t * 128:(t + 1) * 128, :], R=x_d, W=xs[t % 2])
            P.I("act", "activation", out=hb[t % 2][:], in_=xs[t % 2][:], func=AF.Copy, R=[xs[t % 2]], W=[hb[t % 2]])
            emit_transpose_to_hT(P, C, hb[t % 2], hT, tt, T)
        P.dma("sp", hT_d.t[blk], hT[:].rearrange("p c t -> p (c t)"), R=hT, W=hT_d)
    if emb:
        P.end_phase()
        return P
    P.finish([hT_d])
    P.close()
    return P


def build_fused(depth=DEPTH, n_exp=NE):
    P = Prog()
    x_d = P.dram("x", [T_SEQ, D], F32, "ExternalInput")
    memT_d = P.dram("memT", [D, 256], F32, "ExternalInput")
    idb_d = P.dram("ident_bf", [128, 128], BF16, "ExternalInput")
    idf_d = P.dram("ident_f", [128, 128], F32, "ExternalInput")
    out_d = P.dram("out", [T_SEQ, D], F32, "ExternalOutput")
    hT_s = P.dram("hT_s", [T_SEQ // 512, 128, KC * 512], BF16, "Internal")
    yT_s = P.dram("yT_s", [D, T_SEQ], BF16, "Internal")
    h_s = P.dram("h_s", [T_SEQ, D], F32, "Internal")
    ids = dict(ident_bf=idb_d, ident_f=idf_d)
    build_P0(T_SEQ, P=P, io=dict(x=x_d, hT_out=hT_s, **ids))
    for layer in range(depth):
        ioA = dict(hT=hT_s, yT_out=yT_s, **ids)
        if layer % 2 == 0:
            build_A_even(16, P=P, io=ioA, pre=f"L{layer}_")
        else:
            build_A_odd(16, P=P, io=ioA, pre=f"L{layer}_")
        ioB = dict(yT=yT_s, h_in=(x_d if layer == 0 else h_s), memT=memT_d,
                   h_out=(out_d if layer == depth - 1 else h_s), hT_out=hT_s, **ids)
        build_B(T_SEQ, n_exp, P=P, io=ioB, pre=f"L{layer}_")
    P.finish([out_d])
    P.close()
    return P


_bf = ml_dtypes.bfloat16
GW = 2048


def _consts():
    return dict(ident_bf=np.eye(128, dtype=np.float32).astype(_bf), ident_f=np.eye(128, dtype=np.float32))


def prep_A_even(inp, i, heads):
    nh = len(heads)
    w_in = inp["ab_w_in"][i]
    units = []
    for h in heads:
        units.append(np.concatenate([w_in[:, j * GW + h * 128:j * GW + (h + 1) * 128] for j in range(4)], axis=1))
    for h in heads:
        u = np.zeros((D, 512), np.float32)
        for jj, j in enumerate((4, 5, 6)):
            u[:, jj * 128:(jj + 1) * 128] = w_in[:, j * GW + h * 128:j * GW + (h + 1) * 128]
        units.append(u)
    pos = np.arange(T_SEQ, dtype=np.float64)
    inv = 10000.0 ** (-np.arange(0, 128, 2, dtype=np.float64) / 128)
    ang = (pos[:, None] * inv[None, :]).astype(np.float32).astype(np.float64)
    cos, sin = np.cos(ang).T, np.sin(ang).T
    ropec = np.concatenate([cos, cos], 0).astype(np.float32)
    ropes = np.concatenate([-sin, sin], 0).astype(np.float32)
    perm = np.zeros((128, 128), np.float32)
    for m in range(128):
        perm[(m + 64) % 128, m] = 1.0
    gam = np.array([1.0 - 2.0 ** (-(5.0 + h)) for h in heads], np.float64)
    ii = np.arange(128)
    DTm = np.zeros((nh, 128, 128), np.float32)
    for a, g in enumerate(gam):
        d = ii[None, :] - ii[:, None]
        DTm[a] = np.where(d >= 0, g ** np.maximum(d, 0), 0.0)
    qdec4 = np.stack([np.tile(g ** (ii + 1.0), 4) for g in gam]).astype(np.float32)
    kdec = np.stack([g ** (127.0 - ii) for g in gam], 1).astype(np.float32)
    cdec = np.tile((gam ** 128.0)[None, :], (128, 1)).astype(np.float32)
    retg = np.stack([inp["ab_ret_norm_g"][i][h * 128:(h + 1) * 128] for h in heads], 1)
    mlg = np.stack([inp["ab_mlstm_norm_g"][i][h * 128:(h + 1) * 128] for h in heads], 1)
    convw = np.zeros((128, nh * 5), np.float32)
    for a, h in enumerate(heads):
        convw[:, a * 5:a * 5 + 4] = inp["ab_conv_w"][i][:, h * 128:(h + 1) * 128].T
        convw[:, a * 5 + 4] = inp["ab_conv_b"][i][h * 128:(h + 1) * 128]
    wqk = np.stack([np.stack([inp["ab_wq"][i][h], inp["ab_wk"][i][h]]) for h in heads])
    wgate = np.stack([np.stack([w_in[:, 7 * GW + h] for h in heads], 1), np.stack([w_in[:, 7 * GW + 16 + h] for h in heads], 1)])
    gateb = np.stack([np.array([inp["ab_gate_b"][i][h], inp["ab_gate_b"][i][16 + h]]) for h in heads]).astype(np.float32)
    triuT = np.triu(np.ones((128, 128), np.float32))
    e127 = np.zeros((128, 128), np.float32)
    e127[127, :] = 1.0
    d = dict(w_units=np.ascontiguousarray(np.stack(units)), ropec=ropec, ropes=ropes, perm=perm.astype(_bf), DT=DTm, qdec4=qdec4,
             kdec=kdec, cdec=cdec, retg=np.ascontiguousarray(retg), mlg=np.ascontiguousarray(mlg), convw=convw,
             wqk=np.ascontiguousarray(wqk), wgate=np.ascontiguousarray(wgate), gateb=gateb, triuT=triuT, e127=e127)
    d.update(_consts())
    return d


def prep_A_odd(inp, i, heads):
    w_in = inp["cd_w_in"][i]
    rel = inp["cd_rel_bias"][i]

    def unit(g, h):
        return np.concatenate([w_in[:, (3 * g + j) * GW + h * 128:(3 * g + j) * GW + (h + 1) * 128] for j in range(3)], axis=1)
    w_units = np.stack([unit(0, h) for h in heads] + [unit(1, h) for h in heads])
    r = np.arange(64)[:, None]
    m = np.arange(704)[None, :]
    idx = np.clip(r + 512 - (m - 64), -63, 128) + 63
    bias_ext = np.stack([rel[h][idx] for h in heads]).astype(np.float32)
    d = dict(w_units=np.ascontiguousarray(w_units), bias_ext=bias_ext, tril=np.tril(np.ones((128, 128), np.float32), -1))
    d.update(_consts())
    return d


def prep_B(inp, layer):
    i = layer // 2
    w_out = inp["ab_w_out"][i] if layer % 2 == 0 else inp["cd_w_out"][i]
    lnp = np.stack([inp["mix_ln_g"][layer], inp["mix_ln_b"][layer], inp["xa_ln_g"][layer], inp["xa_ln_b"][layer],
                    inp["moe_ln_g"][layer], inp["moe_ln_b"][layer]])
    bg, bu = inp["moe_b_gate"][layer], inp["moe_b_up"][layer]
    bgu = np.zeros((128, NE * 4), np.float32)
    for c in range(2):
        bgu[:, c::4] = bg[:, c * 128:(c + 1) * 128].T
        bgu[:, 2 + c::4] = bu[:, c * 128:(c + 1) * 128].T
    d = dict(w_out=np.ascontiguousarray(w_out), lnp=lnp, xa_wq=np.ascontiguousarray(inp["xa_wq"][layer]),
             xa_wkv=np.ascontiguousarray(inp["xa_wkv"][layer]), xa_wo=np.ascontiguousarray(inp["xa_wo"][layer]),
             rw=np.ascontiguousarray(inp["moe_router_w"][layer]), rb=np.ascontiguousarray(inp["moe_router_b"][layer][None, :]),
             wg=np.ascontiguousarray(inp["moe_w_gate"][layer]), wu=np.ascontiguousarray(inp["moe_w_up"][layer]),
             wd=np.ascontiguousarray(inp["moe_w_down"][layer]), bgu=bgu, bd=np.ascontiguousarray(inp["moe_b_down"][layer]))
    d.update(_consts())
    return d


_PROGS = {}


def _prog(name):
    if name not in _PROGS:
        _PROGS[name] = {"P0": build_P0, "A_even": lambda: build_A_even(8), "A_odd": lambda: build_A_odd(8), "B": build_B,
                        "fused": build_fused}[name]()
    return _PROGS[name]


def fused_weights(inp, depth=DEPTH):
    w = {}
    heads = list(range(16))
    for layer in range(depth):
        i = layer // 2
        d = prep_A_even(inp, i, heads) if layer % 2 == 0 else prep_A_odd(inp, i, heads)
        d.update(prep_B(inp, layer))
        for k, v in d.items():
            if k in ("ident_bf", "ident_f"):
                continue
            w[f"L{layer}_{k}"] = v
    return w


def kernel(**inp):
    inp = {k: np.asarray(v) for k, v in inp.items()}
    x = inp["x"]
    mem = inp["mem"]
    w = fused_weights(inp)
    cst = _consts()
    maps = []
    for c in range(NCORES):
        b = c % 4
        d = dict(w)
        d.update(cst)
        d["x"] = np.ascontiguousarray(x[b])
        d["memT"] = np.ascontiguousarray(mem[b].T)
        maps.append(d)
    res = run_bass_kernel_spmd(_prog("fused").nc, maps, core_ids=list(range(NCORES))).results
    return np.stack([res[b]["out"] for b in range(4)]).astype(np.float32)
```

```python
import numpy as np
import ml_dtypes
from contextlib import ExitStack
import concourse.bass as bass
import concourse.mybir as mybir
from concourse.bass_utils import run_bass_kernel_spmd

F32 = mybir.dt.float32
BF16 = mybir.dt.bfloat16
AF = mybir.ActivationFunctionType
ALU = mybir.AluOpType
AX = mybir.AxisListType

D = 4096
KC = 32
DEPTH = 4
DN_ALPHA = (2.0 * DEPTH) ** 0.25
LN_EPS = 1e-5
HN_EPS = 1e-6
NE = 32
NCORES = 8


class DSem:
    __slots__ = ("h", "cnt", "key")

    def __init__(self, h, key):
        self.h = h
        self.cnt = 0
        self.key = key


class _BState:
    __slots__ = ("writer", "readers", "dsem", "dlast", "dwait_w", "dwait_r")

    def __init__(self):
        self.writer = None
        self.readers = {}
        self.dsem = {}
        self.dlast = None
        self.dwait_w = []
        self.dwait_r = []


class Buf:
    __slots__ = ("t", "name", "s", "is_dram")

    def __init__(self, t, name, is_dram=False):
        self.t = t
        self.name = name
        self.s = _BState()
        self.is_dram = is_dram

    def view(self, ap):
        b = Buf(ap, self.name, self.is_dram)
        b.s = self.s
        return b

    def __getitem__(self, idx):
        return self.t[idx]

    writer = property(lambda self: self.s.writer, lambda self, v: setattr(self.s, "writer", v))
    readers = property(lambda self: self.s.readers, lambda self, v: setattr(self.s, "readers", v))
    dsem = property(lambda self: self.s.dsem, lambda self, v: setattr(self.s, "dsem", v))
    dlast = property(lambda self: self.s.dlast, lambda self, v: setattr(self.s, "dlast", v))
    dwait_w = property(lambda self: self.s.dwait_w, lambda self, v: setattr(self.s, "dwait_w", v))
    dwait_r = property(lambda self: self.s.dwait_r, lambda self, v: setattr(self.s, "dwait_r", v))


class Prog:
    ENGS = ("pe", "act", "dve", "pool", "sp")

    def __init__(self):
        self.nc = bass.Bass("TRN2", target_bir_lowering=False)
        nc = self.nc
        self.st = ExitStack()
        self.top = self.st
        self.e = {"pe": nc.tensor, "act": nc.scalar, "dve": nc.vector, "pool": nc.gpsimd, "sp": nc.sync}
        self.sem = {k: self.top.enter_context(nc.semaphore("s_" + k)) for k in self.ENGS}
        self.cnt = {k: 0 for k in self.ENGS}
        self.waited = {}
        self.nbuf = 0
        self.ninst = 0
        self.dsems = []
        self.dfree = {"sw": [], "hw": []}
        self.phase_bufs = None
        self.dram_bufs = []

    def sb(self, shape, dt=F32, name=None):
        self.nbuf += 1
        name = (name or "sb") + f"_{self.nbuf}"
        t = self.st.enter_context(self.nc.sbuf_tensor(name, list(shape), dt))
        b = Buf(t, name)
        if self.phase_bufs is not None:
            self.phase_bufs.append(b)
        return b

    def ps(self, shape, dt=F32, name=None):
        self.nbuf += 1
        name = (name or "ps") + f"_{self.nbuf}"
        t = self.st.enter_context(self.nc.psum_tensor(name, list(shape), dt))
        b = Buf(t, name)
        if self.phase_bufs is not None:
            self.phase_bufs.append(b)
        return b

    def dram(self, name, shape, dt, kind):
        t = self.nc.dram_tensor(name, list(shape), dt, kind=kind)
        b = Buf(t.ap(), name, is_dram=True)
        self.dram_bufs.append(b)
        return b

    def _get_dsem(self, qt):
        if self.dfree[qt]:
            return self.dfree[qt].pop()
        d = DSem(self.top.enter_context(self.nc.semaphore(f"d{len(self.dsems)}")), f"d{len(self.dsems)}")
        self.dsems.append(d)
        return d

    def begin_phase(self):
        self.st = ExitStack()
        self.phase_bufs = []

    def barrier(self):
        for x in self.ENGS:
            for e in self.ENGS:
                if e != x:
                    self._wait(x, e, self.sem[e], self.cnt[e])
            for d in self.dsems:
                self._wait(x, d.key, d.h, 16 * d.cnt)

    def end_phase(self):
        self.barrier()
        for b in self.phase_bufs:
            for qt, d in b.dsem.items():
                self.dfree[qt].append(d)
            b.dsem = {}
        for b in self.dram_bufs:
            b.dwait_w = []
            b.dwait_r = []
            b.writer = None
            b.readers = {}
        self.st.close()
        self.st = self.top
        self.phase_bufs = None

    def _wait(self, x, key, sem, val):
        if val <= 0:
            return
        k2 = (x, key)
        if self.waited.get(k2, 0) >= val:
            return
        self.waited[k2] = val
        self.e[x].wait_ge(sem, val)

    def _wait_dma(self, x, b, skip_qt=None, for_write=True):
        for qt, d in b.dsem.items():
            if qt != skip_qt:
                self._wait(x, d.key, d.h, 16 * d.cnt)
        for d in b.dwait_w:
            self._wait(x, d.key, d.h, 16 * d.cnt)
        if for_write:
            for d in b.dwait_r:
                self._wait(x, d.key, d.h, 16 * d.cnt)

    def _deps(self, x, R, W, dma_skip=None, skip_qt=None):
        for b in R:
            if b.writer is not None:
                e, k = b.writer
                if not (x == "pe" and e == "pe"):
                    self._wait(x, e, self.sem[e], k)
            self._wait_dma(x, b, skip_qt if b is dma_skip else None, for_write=False)
        for b in W:
            if b.writer is not None:
                e, k = b.writer
                if not (x == "pe" and e == "pe"):
                    self._wait(x, e, self.sem[e], k)
            for e, k in b.readers.items():
                if not (x == "pe" and e == "pe"):
                    self._wait(x, e, self.sem[e], k)
            self._wait_dma(x, b, skip_qt if b is dma_skip else None)

    def I(self, x, fname, *args, R=(), W=(), **kw):
        self._deps(x, R, W)
        inst = getattr(self.e[x], fname)(*args, **kw)
        self.cnt[x] += 1
        self.ninst += 1
        k = self.cnt[x]
        inst.then_inc(self.sem[x], 1)
        for b in R:
            b.readers[x] = k
        for b in W:
            b.writer = (x, k)
            b.readers = {}
        return inst

    def dma(self, q, out, in_, R, W, **kw):
        if not W.is_dram:
            trk, kind = W, "w"
        elif not R.is_dram:
            trk, kind = R, "r"
        else:
            trk, kind = W, "w"
        qt = "sw" if q == "pool" else "hw"
        skip = trk if (trk.dlast == kind or trk.dlast is None) else None
        self._deps(q, [R], [W], dma_skip=skip, skip_qt=qt)
        if qt not in trk.dsem:
            trk.dsem[qt] = self._get_dsem(qt)
        ds = trk.dsem[qt]
        inst = self.e[q].dma_start(out=out, in_=in_, **kw)
        ds.cnt += 1
        trk.dlast = kind
        inst.then_inc(ds.h, 16)
        self.ninst += 1
        other = R if trk is W else W
        if other is not trk:
            lst = other.dwait_r if other is R else other.dwait_w
            if ds not in lst:
                lst.append(ds)
        return inst

    def finish(self, bufs, x="sp"):
        for b in bufs:
            self._wait_dma(x, b)

    def close(self):
        self.top.close()


class Common:
    def __init__(self, P, ident_bf_d, ident_f_d):
        self.P = P
        self.ident_bf = P.sb([128, 128], BF16, "identb")
        self.ident_f = P.sb([128, 128], F32, "identf")
        P.dma("sp", self.ident_bf[:], ident_bf_d[:], R=ident_bf_d, W=self.ident_bf)
        P.dma("sp", self.ident_f[:], ident_f_d[:], R=ident_f_d, W=self.ident_f)
        self.eps_ln = P.sb([128, 1], F32, "epsln")
        P.I("dve", "memset", self.eps_ln[:], LN_EPS, W=[self.eps_ln])
        self.eps_hn = P.sb([128, 1], F32, "epshn")
        P.I("dve", "memset", self.eps_hn[:], HN_EPS, W=[self.eps_hn])
        self.rr = 0

    def evac_engine(self):
        self.rr += 1
        return "act" if self.rr % 2 else "dve"

    def copy(self, eng, out, in_, R, W):
        P = self.P
        if eng == "act":
            P.I("act", "activation", out=out, in_=in_, func=AF.Copy, R=R, W=W)
        else:
            P.I(eng, "tensor_copy", out=out, in_=in_, R=R, W=W)


def emit_layernorm(P, C, x, G, Bt, stats, mv, hb=None):
    xs = x[:].rearrange("p (c f) -> p c f", f=512)
    for c in range(8):
        P.I("dve", "bn_stats", out=stats[:, c, :], in_=xs[:, c, :], R=[x], W=[stats])
    P.I("dve", "bn_aggr", out=mv[:, 0:2], in_=stats[:], R=[stats], W=[mv])
    P.I("act", "activation", out=mv[:, 2:3], in_=mv[:, 1:2], func=AF.Sqrt, bias=C.eps_ln[:], scale=1.0,
        R=[mv, C.eps_ln], W=[mv])
    P.I("dve", "reciprocal", out=mv[:, 3:4], in_=mv[:, 2:3], R=[mv], W=[mv])
    P.I("dve", "tensor_scalar", out=x[:], in0=x[:], scalar1=mv[:, 0:1], scalar2=mv[:, 3:4],
        op0=ALU.subtract, op1=ALU.mult, R=[x, mv], W=[x])
    P.I("dve", "tensor_tensor", out=x[:], in0=x[:], in1=G[:], op=ALU.mult, R=[x, G], W=[x])
    P.I("dve", "tensor_tensor", out=x[:], in0=x[:], in1=Bt[:], op=ALU.add, R=[x, Bt], W=[x])
    if hb is not None:
        P.I("act", "activation", out=hb[:], in_=x[:], func=AF.Copy, R=[x], W=[hb])


def emit_transpose_to_hT(P, C, hb, hT, tt, tps):
    for g in range(4):
        tp = tps[g % len(tps)]
        for j in range(8):
            c = g * 8 + j
            P.I("pe", "transpose", out=tp[:, j, :], in_=hb[:, c:D:KC], identity=C.ident_bf[:],
                R=[hb, C.ident_bf], W=[tp])
        C.copy(C.evac_engine(), hT[:, g * 8:(g + 1) * 8, tt * 128:(tt + 1) * 128], tp[:], R=[tp], W=[hT])


def build_B(ntok=1024, n_exp=NE, P=None, io=None, pre=""):
    emb = P is not None
    if not emb:
        P = Prog()
    io = io or {}
    TB = 512
    nblk = ntok // TB
    dr = {}

    def din(name, shape, dt=F32):
        dr[name] = io[name] if name in io else P.dram(pre + name, shape, dt, "ExternalInput")
        return dr[name]

    yT_d = din("yT", [D, ntok], BF16)
    hin_d = din("h_in", [ntok, D])
    memT_d = din("memT", [D, 256])
    wout_d = din("w_out", [D, D])
    lnp_d = din("lnp", [6, D])
    wq_d = din("xa_wq", [D, 512])
    wkv_d = din("xa_wkv", [D, 1024])
    wo_d = din("xa_wo", [512, D])
    rw_d = din("rw", [D, NE])
    rb_d = din("rb", [1, NE])
    wg_d = din("wg", [NE, D, 256])
    wu_d = din("wu", [NE, D, 256])
    wd_d = din("wd", [NE, 256, D])
    bgu_d = din("bgu", [128, NE * 4])
    bd_d = din("bd", [NE, D])
    idb_d = din("ident_bf", [128, 128], BF16)
    idf_d = din("ident_f", [128, 128])
    hout_d = io["h_out"] if "h_out" in io else P.dram("h_out", [ntok, D], F32, "ExternalOutput")
    hTout_d = io["hT_out"] if "hT_out" in io else P.dram("hT_out", [ntok // 512, 128, KC * 512], BF16, "ExternalOutput")
    if emb:
        P.begin_phase()

    C = Common(P, idb_d, idf_d)

    hres = [P.sb([128, D], F32, f"hres{i}") for i in range(4)]
    hT = P.sb([128, KC, TB], BF16, "hT")
    hb = P.sb([128, D], BF16, "hb")
    lnact = P.sb([128, 2 * D], F32, "lnact")
    lnG = lnact.view(lnact.t[:, 0:D])
    lnB = lnact.view(lnact.t[:, D:2 * D])
    actH = lnact.view(lnact.t[:].bitcast(BF16).rearrange("p (e c t) -> p e c t", e=16, c=2))
    NRING = 4
    ring = [P.sb([128, 8, 512], BF16, f"ring{i}") for i in range(NRING)]
    ring_i = [0]

    def next_piece():
        r = ring[ring_i[0] % NRING]
        ring_i[0] += 1
        return r

    stats = P.sb([128, 8, 6], F32, "stats")
    mv = P.sb([128, 4], F32, "mv")
    kT = P.sb([128, 4, 256], BF16, "kT")
    vv = P.sb([128, 2, 512], BF16, "vv")
    qT = P.sb([128, 4, TB], BF16, "qT")
    oT = P.sb([128, 4, TB], BF16, "oT")
    PT = P.sb([128, 2, TB], BF16, "PT")
    sc = P.sb([128, 256], F32, "sc")
    pb = P.sb([128, 256], BF16, "pb")
    red = P.sb([128, 8], F32, "red")
    rws = P.sb([128, KC, NE], BF16, "rws")
    rbt = P.sb([128, NE], F32, "rbt")
    lg = P.sb([128, NE], F32, "lg")
    top8 = P.sb([128, 8], F32, "top8")
    ex = P.sb([128, NE], F32, "ex")
    msk = P.sb([128, NE], F32, "msk")
    gate = [P.sb([128, NE], F32, f"gate{i}") for i in range(4)]
    gateT = P.sb([NE, TB], BF16, "gateT")
    bgu = P.sb([128, NE * 4], F32, "bgu")
    tgs = [P.sb([128, TB], F32, f"tg{i}") for i in range(2)]
    tsg = P.sb([128, TB], F32, "tsg")
    tus = [P.sb([128, TB], F32, f"tu{i}") for i in range(2)]
    sel = P.sb([NE, NE, 128], BF16, "sel")

    A = [P.ps([128, 512], F32, f"A{i}") for i in range(8)]
    T = [A[6 + i].view(A[6 + i].t[:].bitcast(BF16).rearrange("p (j n) -> p j n", n=128)) for i in range(2)]
    for e in range(NE):
        P.I("dve", "tensor_copy", out=sel[:, e, :], in_=C.ident_bf[0:NE, e:e + 1].to_broadcast([NE, 128]), R=[C.ident_bf], W=[sel])

    P.dma("sp", bgu[:], bgu_d[:], R=bgu_d, W=bgu)
    P.dma("sp", rbt[:], rb_d.t[0, :].partition_broadcast(128), R=rb_d, W=rbt)
    P.dma("pool", rws[:], rw_d.t.rearrange("(p c) n -> p c n", c=KC), R=rw_d, W=rws)

    def wview(wd2):
        return wd2.rearrange("(c p) n -> p c n", p=128)

    def fview(wd2):
        return wd2.rearrange("(p c) n -> p c n", c=KC)

    P.dma("pool", hT[:, :, 0:256], fview(memT_d.t), R=memT_d, W=hT)
    wkv_v = fview(wkv_d.t)
    for hd in range(4):
        pieces = []
        for g in range(4):
            r = next_piece()
            P.dma("pool", r[:, :, 0:128], wkv_v[:, g * 8:(g + 1) * 8, hd * 128:(hd + 1) * 128], R=wkv_d, W=r)
            pieces.append(r)
        for c in range(KC):
            r = pieces[c // 8]
            P.I("pe", "matmul", A[0][:, 0:256], r[:, c % 8, 0:128], hT[:, c, 0:256], start=(c == 0), stop=(c == KC - 1),
                R=[r, hT], W=[A[0]])
        C.copy("act", kT[:, hd, :], A[0][:, 0:256], R=[A[0]], W=[kT])
    for mt in range(2):
        for g in range(4):
            r = next_piece()
            P.dma("pool", r[:], wkv_v[:, g * 8:(g + 1) * 8, 512:1024], R=wkv_d, W=r)
            for j in range(8):
                c = g * 8 + j
                P.I("pe", "matmul", A[1][:], hT[:, c, mt * 128:(mt + 1) * 128], r[:, j, :], start=(c == 0), stop=(c == KC - 1),
                    R=[r, hT], W=[A[1]])
        C.copy("act", vv[:, mt, :], A[1][:], R=[A[1]], W=[vv])

    def load_ln(i):
        P.dma("sp", lnG[:], lnp_d.t[2 * i, :].partition_broadcast(128), R=lnp_d, W=lnG)
        P.dma("sp", lnB[:], lnp_d.t[2 * i + 1, :].partition_broadcast(128), R=lnp_d, W=lnB)

    def proj_residual(w_v, nk, src_T):
        for db in range(8):
            pieces = []
            if nk >= 8:
                for g in range(nk // 8):
                    r = next_piece()
                    P.dma("pool", r[:], w_v[:, g * 8:(g + 1) * 8, db * 512:(db + 1) * 512], R=dr["w_out"], W=r)
                    pieces.append(r)
            else:
                r = next_piece()
                P.dma("pool", r[:, 0:nk, :], w_v[:, 0:nk, db * 512:(db + 1) * 512], R=dr["xa_wo"], W=r)
                pieces.append(r)
            for tt in range(4):
                for c in range(nk):
                    r = pieces[c // 8]
                    P.I("pe", "matmul", A[tt][:], src_T[:, c, tt * 128:(tt + 1) * 128], r[:, c % 8, :],
                        start=(c == 0), stop=(c == nk - 1), R=[r, src_T], W=[A[tt]])
            for tt in range(4):
                P.I("dve", "scalar_tensor_tensor", out=hres[tt][:, db * 512:(db + 1) * 512],
                    in0=hres[tt][:, db * 512:(db + 1) * 512], scalar=DN_ALPHA, in1=A[tt][:],
                    op0=ALU.mult, op1=ALU.add, R=[hres[tt], A[tt]], W=[hres[tt]])

    def ln_and_transpose(i):
        load_ln(i)
        for tt in range(4):
            emit_layernorm(P, C, hres[tt], lnG, lnB, stats, mv, hb)
            emit_transpose_to_hT(P, C, hb, hT, tt, T)

    for blk in range(nblk):
        t0 = blk * TB
        P.dma("sp", hT[:], wview(yT_d.t)[:, :, t0:t0 + TB], R=yT_d, W=hT)
        for tt in range(4):
            P.dma("sp", hres[tt][:], hin_d.t[t0 + tt * 128:t0 + (tt + 1) * 128, :], R=hin_d, W=hres[tt])
        proj_residual(wview(wout_d.t), KC, hT)
        ln_and_transpose(0)
        wq_v = fview(wq_d.t)
        pieces = []
        for g in range(4):
            r = next_piece()
            P.dma("pool", r[:], wq_v[:, g * 8:(g + 1) * 8, :], R=wq_d, W=r)
            pieces.append(r)
        for hd in range(4):
            for c in range(KC):
                r = pieces[c // 8]
                P.I("pe", "matmul", A[hd][:], r[:, c % 8, hd * 128:(hd + 1) * 128], hT[:, c, :],
                    start=(c == 0), stop=(c == KC - 1), R=[r, hT], W=[A[hd]])
        for hd in range(4):
            C.copy(C.evac_engine(), qT[:, hd, :], A[hd][:], R=[A[hd]], W=[qT])
        for hd in range(4):
            for tt in range(4):
                sp_ = A[4 + (tt % 2)]
                P.I("pe", "matmul", sp_[:, 0:256], qT[:, hd, tt * 128:(tt + 1) * 128], kT[:, hd, :], start=True, stop=True,
                    R=[qT, kT], W=[sp_])
                P.I("dve", "reduce_max", out=red[:, 0:1], in_=sp_[:, 0:256], axis=AX.X, R=[sp_], W=[red])
                P.I("dve", "tensor_scalar", out=red[:, 1:2], in0=red[:, 0:1], scalar1=-(128.0 ** -0.5), scalar2=None,
                    op0=ALU.mult, R=[red], W=[red])
                P.I("act", "activation", out=sc[:], in_=sp_[:, 0:256], func=AF.Exp, bias=red[:, 1:2], scale=128.0 ** -0.5,
                    accum_out=red[:, 2:3], R=[sp_, red], W=[sc, red])
                P.I("dve", "reciprocal", out=red[:, 3:4], in_=red[:, 2:3], R=[red], W=[red])
                P.I("dve", "tensor_scalar", out=pb[:], in0=sc[:], scalar1=red[:, 3:4], scalar2=None, op0=ALU.mult,
                    R=[sc, red], W=[pb])
                tp = T[tt % 2]
                for mc in range(2):
                    P.I("pe", "transpose", out=tp[:, mc, :], in_=pb[:, mc * 128:(mc + 1) * 128], identity=C.ident_bf[:],
                        R=[pb, C.ident_bf], W=[tp])
                C.copy("act", PT[:, :, tt * 128:(tt + 1) * 128], tp[:, 0:2, :], R=[tp], W=[PT])
            for mc in range(2):
                P.I("pe", "matmul", A[hd][:], vv[:, mc, hd * 128:(hd + 1) * 128], PT[:, mc, :], start=(mc == 0), stop=(mc == 1),
                    R=[vv, PT], W=[A[hd]])
            C.copy("act", oT[:, hd, :], A[hd][:], R=[A[hd]], W=[oT])
        proj_residual(wview(wo_d.t), 4, oT)
        ln_and_transpose(1)
        for tt in range(4):
            lp = A[4 + (tt % 2)]
            for c in range(KC):
                P.I("pe", "matmul", lp[:, 0:NE], hT[:, c, tt * 128:(tt + 1) * 128], rws[:, c, :], start=(c == 0), stop=(c == KC - 1),
                    R=[hT, rws], W=[lp])
            P.I("dve", "tensor_tensor", out=lg[:], in0=lp[:, 0:NE], in1=rbt[:], op=ALU.add, R=[lp, rbt], W=[lg])
            P.I("dve", "max", out=top8[:], in_=lg[:], R=[lg], W=[top8])
            P.I("dve", "tensor_scalar", out=red[:, 4:5], in0=top8[:, 0:1], scalar1=-1.0, scalar2=None, op0=ALU.mult,
                R=[top8], W=[red])
            P.I("act", "activation", out=ex[:], in_=lg[:], func=AF.Exp, bias=red[:, 4:5], scale=1.0, R=[lg, red], W=[ex])
            P.I("dve", "tensor_scalar", out=msk[:], in0=lg[:], scalar1=top8[:, 3:4], scalar2=None, op0=ALU.is_ge,
                R=[lg, top8], W=[msk])
            P.I("dve", "tensor_tensor", out=ex[:], in0=ex[:], in1=msk[:], op=ALU.mult, R=[ex, msk], W=[ex])
            P.I("dve", "reduce_sum", out=red[:, 5:6], in_=ex[:], axis=AX.X, R=[ex], W=[red])
            P.I("dve", "reciprocal", out=red[:, 6:7], in_=red[:, 5:6], R=[red], W=[red])
            P.I("dve", "tensor_scalar", out=gate[tt][:], in0=ex[:], scalar1=red[:, 6:7], scalar2=None, op0=ALU.mult,
                R=[ex, red], W=[gate[tt]])
            P.I("pe", "transpose", out=lp[0:NE, 128:256], in_=gate[tt][:], identity=C.ident_f[:], R=[gate[tt], C.ident_f], W=[lp])
            C.copy("act", gateT[:, tt * 128:(tt + 1) * 128], lp[0:NE, 128:256], R=[lp], W=[gateT])
        for db in range(8):
            r = next_piece()
            P.dma("pool", r[0:NE, 0, :], bd_d.t[:, db * 512:(db + 1) * 512], R=bd_d, W=r)
            for tt in range(4):
                yp = A[4 + (tt % 2)]
                P.I("pe", "matmul", yp[:], gateT[:, tt * 128:(tt + 1) * 128], r[0:NE, 0, :], start=True, stop=True,
                    R=[gateT, r], W=[yp])
                P.I("dve", "scalar_tensor_tensor", out=hres[tt][:, db * 512:(db + 1) * 512],
                    in0=hres[tt][:, db * 512:(db + 1) * 512], scalar=DN_ALPHA, in1=yp[:],
                    op0=ALU.mult, op1=ALU.add, R=[hres[tt], yp], W=[hres[tt]])
        for half in range(2):
            e0 = half * 16
            nE = min(16, max(0, n_exp - e0))
            if nE == 0:
                continue
            for el in range(nE):
                e = e0 + el
                wg_v = fview(wg_d.t[e])
                wu_v = fview(wu_d.t[e])
                for g in range(4):
                    r = next_piece()
                    rv = r[:].rearrange("p j n -> p (j n)").rearrange("p (h j n) -> p h j n", h=2, j=8)
                    P.dma("pool", rv[:, 0], wg_v[:, g * 8:(g + 1) * 8, :], R=wg_d, W=r)
                    P.dma("pool", rv[:, 1], wu_v[:, g * 8:(g + 1) * 8, :], R=wu_d, W=r)
                    for j in range(8):
                        c = g * 8 + j
                        for m in range(4):
                            P.I("pe", "matmul", A[m][:], rv[:, m // 2, j, (m % 2) * 128:(m % 2 + 1) * 128], hT[:, c, :],
                                start=(c == 0), stop=(c == KC - 1), R=[r, hT], W=[A[m]])
                gb = A[4 + e % 2]
                P.I("pe", "matmul", gb[:], sel[:, e, :], gateT[:], start=True, stop=True, R=[sel, gateT], W=[gb])
                for fc in range(2):
                    P.I("dve", "tensor_scalar", out=tgs[fc][:], in0=A[fc][:], scalar1=bgu[:, e * 4 + fc:e * 4 + fc + 1], scalar2=7.0,
                        op0=ALU.add, op1=ALU.min, R=[A[fc], bgu], W=[tgs[fc]])
                    P.I("act", "activation", out=tus[fc][:], in_=A[2 + fc][:], func=AF.Identity, bias=bgu[:, e * 4 + 2 + fc:e * 4 + 3 + fc],
                        scale=1.0, R=[A[2 + fc], bgu], W=[tus[fc]])
                for fc in range(2):
                    tg, tu = tgs[fc], tus[fc]
                    P.I("act", "activation", out=tsg[:], in_=tg[:], func=AF.Sigmoid, scale=1.702, R=[tg], W=[tsg])
                    P.I("dve", "tensor_scalar", out=tu[:], in0=tu[:], scalar1=7.0, scalar2=-7.0, op0=ALU.min, op1=ALU.max,
                        R=[tu], W=[tu])
                    P.I("dve", "tensor_tensor", out=tg[:], in0=tg[:], in1=tsg[:], op=ALU.mult, R=[tg, tsg], W=[tg])
                    P.I("dve", "tensor_tensor", out=tg[:], in0=tg[:], in1=gb[:], op=ALU.mult, R=[tg, gb], W=[tg])
                    P.I("dve", "scalar_tensor_tensor", out=actH[:, el, fc, :], in0=tu[:], scalar=1.0, in1=tg[:], op0=ALU.add, op1=ALU.mult,
                        R=[tu, tg], W=[actH])
            npc = (nE + 3) // 4
            for db in range(8):
                pcs = []
                for q in range(npc):
                    r = next_piece()
                    ne_q = min(4, nE - q * 4)
                    src = wd_d.t[e0 + q * 4:e0 + q * 4 + ne_q].rearrange("e (c p) n -> p (e c) n", p=128)[:, :, db * 512:(db + 1) * 512]
                    P.dma("pool", r[:, 0:2 * ne_q, :], src, R=wd_d, W=r)
                    pcs.append((r, ne_q))
                for q, (r, ne_q) in enumerate(pcs):
                    for tt in range(4):
                        yp = A[(db % 2) * 4 + tt]
                        for i in range(2 * ne_q):
                            el, fc = q * 4 + i // 2, i % 2
                            first = (q == 0 and i == 0)
                            last = (q == npc - 1 and i == 2 * ne_q - 1)
                            P.I("pe", "matmul", yp[:], actH[:, el, fc, tt * 128:(tt + 1) * 128], r[:, i, :], start=first, stop=last,
                                R=[actH, r], W=[yp])
                for tt in range(4):
                    yp = A[(db % 2) * 4 + tt]
                    P.I("dve", "tensor_tensor", out=hres[tt][:, db * 512:(db + 1) * 512], in0=yp[:],
                        in1=hres[tt][:, db * 512:(db + 1) * 512], op=ALU.add, R=[yp, hres[tt]], W=[hres[tt]])
        load_ln(2)
        for tt in range(4):
            emit_layernorm(P, C, hres[tt], lnG, lnB, stats, mv, hb)
            P.dma("sp", hout_d.t[t0 + tt * 128:t0 + (tt + 1) * 128, :], hres[tt][:], R=hres[tt], W=hout_d)
            emit_transpose_to_hT(P, C, hb, hT, tt, T)
        P.dma("sp", hTout_d.t[blk], hT[:].rearrange("p c t -> p (c t)"), R=hT, W=hTout_d)
    if emb:
        P.end_phase()
        return P
    P.finish([hout_d, hTout_d])
    P.close()
    return P


T_SEQ = 2048


class ACtx:
    pass


def a_setup(P, n_units, ncols, extra_in, io=None, pre="", emb=False):
    io = io or {}
    X = ACtx()
    X.P = P

    def din(name, shape, dt):
        return io[name] if name in io else P.dram(pre + name, shape, dt, "ExternalInput")
    X.hT_d = din("hT", [T_SEQ // 512, 128, KC * 512], BF16)
    X.w_d = din("w_units", [n_units, D, ncols], F32)
    idb_d = din("ident_bf", [128, 128], BF16)
    idf_d = din("ident_f", [128, 128], F32)
    X.ex = {}
    for name, shape, dt in extra_in:
        X.ex[name] = din(name, shape, dt)
    X.yT_d = io["yT_out"] if "yT_out" in io else P.dram("yT_out", [n_units * 128, T_SEQ], BF16, "ExternalOutput")
    if emb:
        P.begin_phase()
    X.C = Common(P, idb_d, idf_d)
    X.hTb = [P.sb([128, KC, 512], BF16, f"hTb{i}") for i in range(2)]
    X.W = P.sb([128, KC, ncols], BF16, "Wunit")
    X.nload = 0
    return X


def a_inproj(X, u, ncols, sinks, A):
    P = X.P
    wv = X.w_d.t[u].rearrange("(p c) n -> p c n", c=KC)
    for g in range(4):
        P.dma("pool", X.W[:, g * 8:(g + 1) * 8, :], wv[:, g * 8:(g + 1) * 8, :], R=X.w_d, W=X.W)
    nb = ncols // 128
    for blk in range(4):
        hb_ = X.hTb[X.nload % 2]
        X.nload += 1
        P.dma("sp", hb_[:].rearrange("p c t -> p (c t)"), X.hT_d.t[blk], R=X.hT_d, W=hb_)
        for j in range(nb):
            ps = A[j % len(A)]
            for c in range(KC):
                P.I("pe", "matmul", ps[:], X.W[:, c, j * 128:(j + 1) * 128], hb_[:, c, :], start=(c == 0), stop=(c == KC - 1),
                    R=[X.W, hb_], W=[ps])
            sinks[j](blk, ps)


def a_to_tokmajor(X, srcT, dst, T):
    P = X.P
    for g in range(2):
        tp = T[g % len(T)]
        for j in range(8):
            t = g * 8 + j
            P.I("pe", "transpose", out=tp[:, j, :], in_=srcT[:, t * 128:(t + 1) * 128], identity=X.C.ident_bf[:],
                R=[srcT, X.C.ident_bf], W=[tp])
        X.C.copy(X.C.evac_engine(), dst[:, g * 8:(g + 1) * 8, :], tp[:], R=[tp], W=[dst])


def build_A_odd(nh=8, P=None, io=None, pre=""):
    emb = P is not None
    if not emb:
        P = Prog()
    n_units = 2 * nh
    X = a_setup(P, n_units, 384, [("bias_ext", [nh, 64, 704], F32), ("tril", [128, 128], F32)], io, pre, emb)
    C = X.C
    scale = 128.0 ** -0.5
    A = [P.ps([128, 512], F32, f"A{i}") for i in range(4)]
    S2 = P.ps([128, 1024], F32, "S2")
    T = [P.ps([128, 8, 128], BF16, f"T{i}") for i in range(2)]

    qT = P.sb([128, T_SEQ], BF16, "qT")
    kT = P.sb([128, T_SEQ], BF16, "kT")
    vT = P.sb([128, T_SEQ], BF16, "vT")
    v = P.sb([128, 16, 128], BF16, "v")
    yT = [P.sb([128, T_SEQ], BF16, f"yT{i}") for i in range(2)]
    tril = P.sb([128, 128], F32, "tril")
    P.dma("sp", tril[:], X.ex["tril"][:], R=X.ex["tril"], W=tril)
    one = P.sb([128, 1], F32, "one")
    P.I("dve", "memset", one[:], 1.0, W=[one])
    red = P.sb([128, 8], F32, "red")

    def mk_sinks():
        def sq(blk, ps):
            P.I("act", "activation", out=qT[:, blk * 512:(blk + 1) * 512], in_=ps[:], func=AF.Copy, scale=scale, R=[ps], W=[qT])

        def sk(blk, ps):
            P.I("dve", "tensor_copy", out=kT[:, blk * 512:(blk + 1) * 512], in_=ps[:], R=[ps], W=[kT])

        def sv(blk, ps):
            P.I("act", "activation", out=vT[:, blk * 512:(blk + 1) * 512], in_=ps[:], func=AF.Copy, R=[ps], W=[vT])
        return [sq, sk, sv]

    bias = P.sb([64, 704], F32, "bias")
    s_sb = P.sb([64, 640], F32, "s_sb")
    Pb = [P.sb([64, 640], BF16, f"Pb{i}") for i in range(2)]
    pe_ = P.sb([64, 640], F32, "pexp")
    PTb = P.sb([128, 5, 64], BF16, "PTb")
    for i in range(2):
        P.I("dve", "memset", Pb[i][:], 0.0, W=[Pb[i]])
    for hl in range(nh):
        u = hl
        a_inproj(X, u, 384, mk_sinks(), A[0:3])
        a_to_tokmajor(X, vT, v, T)
        P.dma("sp", bias[:], X.ex["bias_ext"].t[hl], R=X.ex["bias_ext"], W=bias)
        yo = yT[u % 2]
        for c in range(32):
            par = c % 2
            wb0 = (c - 8) // 2
            lo_band, hi_band = (0, 576) if par == 0 else (64, 640)
            lo = max(lo_band, max(0, -wb0) * 128)
            hi = hi_band
            k0 = wb0 * 128
            for (a, b_) in ((lo, min(hi, 512)), (max(lo, 512), hi)):
                if b_ > a:
                    P.I("pe", "matmul", S2[0:64, a:b_], qT[:, c * 64:(c + 1) * 64], kT[:, k0 + a:k0 + b_], start=True, stop=True,
                        R=[qT, kT], W=[S2])
            boff = 64 if par == 0 else 0
            P.I("dve", "tensor_tensor", out=s_sb[:, lo:hi], in0=S2[0:64, lo:hi], in1=bias[:, boff + lo:boff + hi], op=ALU.add,
                R=[S2, bias], W=[s_sb])
            P.I("dve", "reduce_max", out=red[0:64, 0:1], in_=s_sb[:, lo:hi], axis=AX.X, R=[s_sb], W=[red])
            P.I("dve", "tensor_scalar", out=red[0:64, 1:2], in0=red[0:64, 0:1], scalar1=-1.0, scalar2=None, op0=ALU.mult, R=[red], W=[red])
            P.I("act", "activation", out=pe_[:, lo:hi], in_=s_sb[:, lo:hi], func=AF.Exp, bias=red[0:64, 1:2], scale=1.0,
                accum_out=red[0:64, 2:3], R=[s_sb, red], W=[pe_, red])
            P.I("dve", "reciprocal", out=red[0:64, 3:4], in_=red[0:64, 2:3], R=[red], W=[red])
            pbuf = Pb[par]
            P.I("dve", "tensor_scalar", out=pbuf[:, lo:hi], in0=pe_[:, lo:hi], scalar1=red[0:64, 3:4], scalar2=None, op0=ALU.mult,
                R=[pe_, red], W=[pbuf])
            wbs = [w for w in range(5) if wb0 + w >= 0]
            tp = T[c % 2]
            for w in wbs:
                P.I("pe", "transpose", out=tp[:, w, 0:64], in_=pbuf[:, w * 128:(w + 1) * 128], identity=C.ident_bf[0:64, 0:64],
                    R=[pbuf, C.ident_bf], W=[tp])
            C.copy("act", PTb[:, wbs[0]:5, :], tp[:, wbs[0]:5, 0:64], R=[tp], W=[PTb])
            op_ = A[3]
            for i, w in enumerate(wbs):
                P.I("pe", "matmul", op_[:, 0:64], v[:, wb0 + w, :], PTb[:, w, :], start=(i == 0), stop=(i == len(wbs) - 1),
                    R=[v, PTb], W=[op_])
            C.copy("dve", yo[:, c * 64:(c + 1) * 64], op_[:, 0:64], R=[op_], W=[yo])
        P.dma("sp", X.yT_d.t[u * 128:(u + 1) * 128, :], yo[:], R=yo, W=X.yT_d)

    E = P.sb([128, 512], F32, "E")
    SP = P.sb([128, 512], F32, "SP")
    Cc = P.sb([128, 512], F32, "Cc")
    tt_ = P.sb([128, T_SEQ], F32, "tt")
    Ab = P.sb([128, 512], BF16, "Ab")
    AT = P.sb([128, 4, 128], BF16, "AT")
    carry = P.sb([128, 2], F32, "carry")
    onesrow = P.sb([128, 512], F32, "onesrow")
    P.I("dve", "memset", onesrow[:], 1.0, W=[onesrow])
    for hl in range(nh):
        u = nh + hl
        a_inproj(X, u, 384, mk_sinks(), A[0:3])
        a_to_tokmajor(X, vT, v, T)
        yo = yT[u % 2]
        for qi in range(16):
            nkb = qi + 1
            nch = (nkb + 3) // 4
            for ch in range(nch):
                kb0 = ch * 4
                nb_ = min(4, nkb - kb0)
                w_ = nb_ * 128
                Z = A[ch % 2]
                P.I("pe", "matmul", Z[:, 0:w_], qT[:, qi * 128:(qi + 1) * 128], kT[:, kb0 * 128:kb0 * 128 + w_], start=True, stop=True,
                    R=[qT, kT], W=[Z])
                P.I("act", "activation", out=E[:, 0:w_], in_=Z[:, 0:w_], func=AF.Exp, R=[Z], W=[E])
                P.I("act", "activation", out=SP[:, 0:w_], in_=E[:, 0:w_], func=AF.Ln, bias=one[:], scale=1.0, R=[E, one], W=[SP])
                last = (ch == nch - 1)
                if last:
                    d0 = (nb_ - 1) * 128
                    P.I("dve", "tensor_tensor", out=SP[:, d0:d0 + 128], in0=SP[:, d0:d0 + 128], in1=tril[:], op=ALU.mult,
                        R=[SP, tril], W=[SP])
                init = 0.0 if ch == 0 else carry[:, 0:1]
                rd_ = [onesrow, SP] + ([] if ch == 0 else [carry])
                P.I("dve", "tensor_tensor_scan", out=Cc[:, 0:w_], data0=onesrow[:, 0:w_], data1=SP[:, 0:w_], initial=init,
                    op0=ALU.mult, op1=ALU.add, R=rd_, W=[Cc])
                P.I("dve", "tensor_copy", out=carry[:, 0:1], in_=Cc[:, w_ - 1:w_], R=[Cc], W=[carry])
                P.I("dve", "tensor_tensor", out=E[:, 0:w_], in0=Z[:, 0:w_], in1=SP[:, 0:w_], op=ALU.subtract, R=[Z, SP], W=[E])
                P.I("dve", "tensor_tensor", out=tt_[:, kb0 * 128:kb0 * 128 + w_], in0=E[:, 0:w_], in1=Cc[:, 0:w_], op=ALU.add,
                    R=[E, Cc], W=[tt_])
            P.I("dve", "tensor_scalar", out=carry[:, 1:2], in0=carry[:, 0:1], scalar1=-1.0, scalar2=None, op0=ALU.mult, R=[carry], W=[carry])
            op_ = A[2 + (qi % 2)]
            for ch in range(nch):
                kb0 = ch * 4
                nb_ = min(4, nkb - kb0)
                w_ = nb_ * 128
                P.I("act", "activation", out=Ab[:, 0:w_], in_=tt_[:, kb0 * 128:kb0 * 128 + w_], func=AF.Exp, bias=carry[:, 1:2], scale=1.0,
                    R=[tt_, carry], W=[Ab])
                if ch == nch - 1:
                    d0 = (nb_ - 1) * 128
                    P.I("dve", "tensor_tensor", out=Ab[:, d0:d0 + 128], in0=Ab[:, d0:d0 + 128], in1=tril[:], op=ALU.mult,
                        R=[Ab, tril], W=[Ab])
                tp = T[ch % 2]
                for j in range(nb_):
                    P.I("pe", "transpose", out=tp[:, j, :], in_=Ab[:, j * 128:(j + 1) * 128], identity=C.ident_bf[:],
                        R=[Ab, C.ident_bf], W=[tp])
                C.copy("act", AT[:, 0:nb_, :], tp[:, 0:nb_, :], R=[tp], W=[AT])
                for j in range(nb_):
                    kb = kb0 + j
                    P.I("pe", "matmul", op_[:, 0:128], v[:, kb, :], AT[:, j, :], start=(kb == 0), stop=(kb == nkb - 1),
                        R=[v, AT], W=[op_])
            C.copy("dve", yo[:, qi * 128:(qi + 1) * 128], op_[:, 0:128], R=[op_], W=[yo])
        P.dma("sp", X.yT_d.t[u * 128:(u + 1) * 128, :], yo[:], R=yo, W=X.yT_d)
    if emb:
        P.end_phase()
        return P
    P.finish([X.yT_d])
    P.close()
    return P


def build_A_even(nh=8, P=None, io=None, pre=""):
    emb = P is not None
    if not emb:
        P = Prog()
    n_units = 2 * nh
    extra = [("ropec", [128, T_SEQ], F32), ("ropes", [128, T_SEQ], F32), ("perm", [128, 128], BF16),
             ("DT", [nh, 128, 128], F32), ("qdec4", [nh, 512], F32), ("kdec", [128, nh], F32), ("cdec", [128, nh], F32),
             ("retg", [128, nh], F32), ("mlg", [128, nh], F32), ("convw", [128, nh * 5], F32),
             ("wqk", [nh, 2, 128, 128], F32), ("wgate", [2, D, nh], F32), ("gateb", [nh, 2], F32),
             ("triuT", [128, 128], F32), ("e127", [128, 128], F32)]
    X = a_setup(P, n_units, 512, extra, io, pre, emb)
    C = X.C
    scale = 128.0 ** -0.5
    A = [P.ps([128, 512], F32, f"A{i}") for i in range(6)]
    T = [P.ps([128, 8, 128], BF16, f"T{i}") for i in range(2)]

    def ld(name, shape, dt=F32, src=None, q="sp"):
        b = P.sb(shape, dt, name)
        P.dma(q, b[:], (src if src is not None else X.ex[name][:]), R=X.ex[name], W=b)
        return b

    ropec = ld("ropec", [128, T_SEQ])
    ropes = ld("ropes", [128, T_SEQ])
    perm = ld("perm", [128, 128], BF16)
    kdec = ld("kdec", [128, nh])
    cdec = ld("cdec", [128, nh])
    retg = ld("retg", [128, nh])
    mlg = ld("mlg", [128, nh])
    convw = ld("convw", [128, nh * 5])
    triuT = ld("triuT", [128, 128])
    e127 = ld("e127", [128, 128])
    gateb = ld("gateb", [nh, 2])
    DT = P.sb([128, 128], F32, "DT")
    qdec = P.sb([128, 512], F32, "qdec")

    z0 = P.sb([128, T_SEQ], BF16, "z0")
    z1 = P.sb([128, T_SEQ], BF16, "z1")
    z2 = P.sb([128, T_SEQ], BF16, "z2")
    z3 = P.sb([128, T_SEQ], BF16, "z3")
    qd = P.sb([128, T_SEQ], BF16, "qd")
    xf = P.sb([128, T_SEQ + 4], F32, "xf")
    acc = P.sb([128, T_SEQ], F32, "acc")
    vtok = P.sb([128, 16, 132], BF16, "vtok")
    ktok = P.sb([128, 16, 128], BF16, "ktok")
    gtok = P.sb([128, 16, 128], BF16, "gtok")
    yT = [P.sb([128, T_SEQ], BF16, f"yT{i}") for i in range(2)]
    S = P.sb([128, 132], F32, "S")
    Sb = P.sb([128, 132], BF16, "Sb")
    PTt = P.sb([128, 128], BF16, "PTt")
    ot = P.sb([128, 128], F32, "ot")
    onb = P.sb([128, 128], BF16, "onb")
    stats = P.sb([128, 6], F32, "stats")
    mv = P.sb([128, 8], F32, "mv")
    t1 = P.sb([128, 512], F32, "t1")
    t2 = P.sb([128, 512], F32, "t2")
    P.I("dve", "memset", vtok[:], 1.0, W=[vtok])
    P.I("dve", "memset", xf[:], 0.0, W=[xf])

    def rope_sink(dst, sc_):
        def f(blk, ps):
            sl = slice(blk * 512, (blk + 1) * 512)
            P.I("act", "activation", out=z3[:, sl], in_=ps[:], func=AF.Copy, scale=sc_, R=[ps], W=[z3])
            sw = A[4 + blk % 2]
            P.I("pe", "matmul", sw[:], perm[:], z3[:, sl], start=True, stop=True, R=[perm, z3], W=[sw])
            P.I("dve", "tensor_tensor", out=t1[:], in0=z3[:, sl], in1=ropec[:, sl], op=ALU.mult, R=[z3, ropec], W=[t1])
            P.I("dve", "tensor_tensor", out=t2[:], in0=sw[:], in1=ropes[:, sl], op=ALU.mult, R=[sw, ropes], W=[t2])
            P.I("dve", "tensor_tensor", out=dst[:, sl], in0=t1[:], in1=t2[:], op=ALU.add, R=[t1, t2], W=[dst])
        return f

    def plain_sink(dst, func=AF.Copy):
        def f(blk, ps):
            P.I("act", "activation", out=dst[:, blk * 512:(blk + 1) * 512], in_=ps[:], func=func, R=[ps], W=[dst])
        return f

    def head_norm_out(src_ps_or_sb, R_, gcol, yo, tok0):
        P.I("dve", "bn_stats", out=stats[:], in_=src_ps_or_sb, R=R_, W=[stats])
        P.I("dve", "bn_aggr", out=mv[:, 0:2], in_=stats[:], R=[stats], W=[mv])
        P.I("act", "activation", out=mv[:, 2:3], in_=mv[:, 1:2], func=AF.Sqrt, bias=C.eps_hn[:], scale=1.0, R=[mv, C.eps_hn], W=[mv])
        P.I("dve", "reciprocal", out=mv[:, 3:4], in_=mv[:, 2:3], R=[mv], W=[mv])
        P.I("dve", "tensor_scalar", out=onb[:], in0=src_ps_or_sb, scalar1=mv[:, 0:1], scalar2=mv[:, 3:4], op0=ALU.subtract, op1=ALU.mult,
            R=list(R_) + [mv], W=[onb])
        tp = T[0]
        P.I("pe", "transpose", out=tp[:, 0, :], in_=onb[:], identity=C.ident_bf[:], R=[onb, C.ident_bf], W=[tp])
        return tp

    for hl in range(nh):
        u = hl
        a_inproj(X, u, 512, [rope_sink(z0, scale), rope_sink(z1, 1.0), plain_sink(z2), plain_sink(acc, AF.Silu)], A[0:4])
        qT_, kT_, vT_ = z0, z1, z2
        P.dma("sp", DT[:], X.ex["DT"].t[hl], R=X.ex["DT"], W=DT)
        P.dma("sp", qdec[:], X.ex["qdec4"].t[hl, :].partition_broadcast(128), R=X.ex["qdec4"], W=qdec)
        for blk in range(4):
            sl = slice(blk * 512, (blk + 1) * 512)
            P.I("dve", "tensor_tensor", out=qd[:, sl], in0=qT_[:, sl], in1=qdec[:], op=ALU.mult, R=[qT_, qdec], W=[qd])
        for g in range(2):
            tp = T[g % 2]
            for j in range(8):
                t = g * 8 + j
                P.I("pe", "transpose", out=tp[:, j, :], in_=vT_[:, t * 128:(t + 1) * 128], identity=C.ident_bf[:], R=[vT_, C.ident_bf], W=[tp])
            C.copy("dve", vtok[:, g * 8:(g + 1) * 8, 0:128], tp[:], R=[tp], W=[vtok])
        for g in range(2):
            tp = T[g % 2]
            for j in range(8):
                t = g * 8 + j
                P.I("pe", "transpose", out=tp[:, j, :], in_=kT_[:, t * 128:(t + 1) * 128], identity=C.ident_bf[:], R=[kT_, C.ident_bf], W=[tp])
            P.I("act", "activation", out=ktok[:, g * 8:(g + 1) * 8, :], in_=tp[:], func=AF.Copy, scale=kdec[:, hl:hl + 1], R=[tp, kdec], W=[ktok])
        P.I("dve", "memset", S[:], 0.0, W=[S])
        P.I("dve", "memset", Sb[:], 0.0, W=[Sb])
        yo = yT[u % 2]
        for c in range(16):
            cs = slice(c * 128, (c + 1) * 128)
            sp_ = A[c % 2]
            P.I("pe", "matmul", sp_[:, 0:128], kT_[:, cs], qT_[:, cs], start=True, stop=True, R=[kT_, qT_], W=[sp_])
            P.I("dve", "tensor_tensor", out=PTt[:], in0=sp_[:, 0:128], in1=DT[:], op=ALU.mult, R=[sp_, DT], W=[PTt])
            op_ = A[2 + c % 2]
            P.I("pe", "matmul", op_[:, 0:128], PTt[:], vtok[:, c, 0:128], start=True, stop=False, R=[PTt, vtok], W=[op_])
            P.I("pe", "matmul", op_[:, 0:128], qd[:, cs], Sb[:, 0:128], start=False, stop=True, R=[qd, Sb], W=[op_])
            stp = A[4 + c % 2]
            P.I("pe", "matmul", stp[:, 0:128], ktok[:, c, :], vtok[:, c, 0:128], start=True, stop=True, R=[ktok, vtok], W=[stp])
            P.I("dve", "scalar_tensor_tensor", out=S[:, 0:128], in0=S[:, 0:128], scalar=cdec[:, hl:hl + 1], in1=stp[:, 0:128],
                op0=ALU.mult, op1=ALU.add, R=[S, cdec, stp], W=[S])
            P.I("act", "activation", out=Sb[:, 0:128], in_=S[:, 0:128], func=AF.Copy, R=[S], W=[Sb])
            tp = head_norm_out(op_[:, 0:128], [op_], None, yo, c * 128)
            P.I("dve", "scalar_tensor_tensor", out=yo[:, cs], in0=tp[:, 0, :], scalar=retg[:, hl:hl + 1], in1=acc[:, cs],
                op0=ALU.mult, op1=ALU.mult, R=[tp, retg, acc], W=[yo])
        P.dma("sp", X.yT_d.t[u * 128:(u + 1) * 128, :], yo[:], R=yo, W=X.yT_d)

    class V:
        def __init__(self, buf):
            self.buf = buf

        def __getitem__(self, idx):
            if not isinstance(idx, tuple):
                idx = (idx,)
            return self.buf.t[(slice(0, nh),) + tuple(idx[1:])]
    IG, FG, Rf, ones8 = V(ropec), V(ropes), V(xf), V(acc)
    Gm = P.sb([nh, T_SEQ], F32, "Gm")
    one8 = P.sb([nh, 1], F32, "one8")
    ngb = P.sb([nh, 2], F32, "ngb")
    wgs = P.sb([128, 2, KC, nh], BF16, "wgs")
    AL = slice(0, T_SEQ)
    P.I("dve", "memset", ones8[:, AL], 1.0, W=[acc])
    P.I("dve", "memset", one8[:], 1.0, W=[one8])
    P.I("dve", "tensor_scalar", out=ngb[:], in0=gateb[:], scalar1=-1.0, scalar2=None, op0=ALU.mult, R=[gateb], W=[ngb])
    for gi in range(2):
        P.dma("pool", wgs[:, gi, :, :], X.ex["wgate"].t[gi].rearrange("(p c) n -> p c n", c=KC), R=X.ex["wgate"], W=wgs)
    for blk in range(4):
        hb_ = X.hTb[X.nload % 2]
        X.nload += 1
        P.dma("sp", hb_[:].rearrange("p c t -> p (c t)"), X.hT_d.t[blk], R=X.hT_d, W=hb_)
        sl = slice(blk * 512, (blk + 1) * 512)
        for gi in range(2):
            ps = A[gi]
            for c in range(KC):
                P.I("pe", "matmul", ps[0:nh, :], wgs[:, gi, c, :], hb_[:, c, :], start=(c == 0), stop=(c == KC - 1), R=[wgs, hb_], W=[ps])
        P.I("act", "activation", out=IG[:, sl], in_=A[0][0:nh, :], func=AF.Identity, bias=gateb[:, 0:1], scale=1.0, R=[A[0], gateb], W=[ropec])
        P.I("act", "activation", out=FG[:, sl], in_=A[1][0:nh, :], func=AF.Exp, bias=ngb[:, 1:2], scale=-1.0, R=[A[1], ngb], W=[ropes])
    P.I("act", "activation", out=FG[:, AL], in_=FG[:, AL], func=AF.Ln, bias=one8[:], scale=1.0, R=[ropes, one8], W=[ropes])
    P.I("dve", "tensor_tensor_scan", out=FG[:, AL], data0=ones8[:, AL], data1=FG[:, AL], initial=0.0, op0=ALU.mult, op1=ALU.add,
        R=[acc, ropes], W=[ropes])
    P.I("dve", "tensor_tensor", out=IG[:, AL], in0=IG[:, AL], in1=FG[:, AL], op=ALU.add, R=[ropec, ropes], W=[ropec])
    P.I("dve", "tensor_tensor_scan", out=Gm[:], data0=IG[:, AL], data1=IG[:, AL], initial=0.0, op0=ALU.max, op1=ALU.max, R=[ropec], W=[Gm])
    G3 = Gm[:].rearrange("p (c l) -> p c l", l=128)
    R3 = Rf[:, AL].rearrange("p (c l) -> p c l", l=128)
    P.I("dve", "memset", Rf[:, 0:128], 0.0, W=[xf])
    P.I("dve", "tensor_copy", out=R3[:, 1:16, :], in_=G3[:, 0:15, 127:128].to_broadcast([nh, 15, 128]), R=[Gm], W=[xf])
    P.I("dve", "tensor_tensor", out=IG[:, AL], in0=IG[:, AL], in1=Rf[:, AL], op=ALU.subtract, R=[ropec, xf], W=[ropec])
    P.I("act", "activation", out=IG[:, AL], in_=IG[:, AL], func=AF.Exp, R=[ropec], W=[ropec])
    P.I("dve", "tensor_tensor", out=Rf[:, AL], in0=Rf[:, AL], in1=Gm[:], op=ALU.subtract, R=[xf, Gm], W=[xf])
    P.I("act", "activation", out=Rf[:, AL], in_=Rf[:, AL], func=AF.Exp, R=[xf], W=[xf])
    P.I("dve", "tensor_tensor", out=FG[:, AL], in0=FG[:, AL], in1=Gm[:], op=ALU.subtract, R=[ropes, Gm], W=[ropes])
    P.I("act", "activation", out=FG[:, AL], in_=FG[:, AL], func=AF.Exp, R=[ropes], W=[ropes])
    qa, qr, qe = IG, Rf, FG
    qpar = {id(IG): ropec, id(Rf): xf, id(FG): ropes}
    qtok = [P.sb([128, 16, nh], F32, f"qtok{i}") for i in range(3)]
    for qi_, src in enumerate((qa, qr, qe)):
        ps = A[2 + qi_ % 2]
        for c in range(16):
            P.I("pe", "transpose", out=ps[:, c * nh:(c + 1) * nh], in_=src[:, c * 128:(c + 1) * 128], identity=C.ident_f[0:nh, 0:nh],
                R=[qpar[id(src)], C.ident_f], W=[ps])
        C.copy("dve", qtok[qi_][:], ps[:, 0:16 * nh].rearrange("p (c h) -> p c h", h=nh), R=[ps], W=[qtok[qi_]])
    a_tok, r_tok, e_tok = qtok
    decb = P.sb([128, 16, nh], F32, "decb")
    P.I("dve", "memset", xf[:], 0.0, R=[], W=[xf])
    P.I("pe", "matmul", A[4][:, 0:16 * nh], e127[:], r_tok[:].rearrange("p c h -> p (c h)"), start=True, stop=True, R=[e127, r_tok], W=[A[4]])
    C.copy("dve", decb[:], A[4][:, 0:16 * nh].rearrange("p (c h) -> p c h", h=nh), R=[A[4]], W=[decb])

    wq_s = P.sb([128, 128], BF16, "wq_s")
    wk_s = P.sb([128, 128], BF16, "wk_s")
    ka = ktok
    den = P.sb([128, 8], F32, "den")
    for hl in range(nh):
        u = nh + hl
        cw = convw[:, hl * 5:(hl + 1) * 5]

        def mu_sink(blk, ps):
            P.I("act", "activation", out=xf[:, 3 + blk * 512:3 + (blk + 1) * 512], in_=ps[:], func=AF.Copy, R=[ps], W=[xf])
        a_inproj(X, u, 384, [mu_sink, plain_sink(z2), plain_sink(z3, AF.Sigmoid)], A[0:3])
        P.dma("pool", wq_s[:], X.ex["wqk"].t[hl, 0], R=X.ex["wqk"], W=wq_s)
        P.dma("pool", wk_s[:], X.ex["wqk"].t[hl, 1], R=X.ex["wqk"], W=wk_s)
        P.I("dve", "tensor_scalar", out=acc[:], in0=xf[:, 3:3 + T_SEQ], scalar1=cw[:, 3:4], scalar2=cw[:, 4:5], op0=ALU.mult, op1=ALU.add,
            R=[xf, convw], W=[acc])
        for i in range(3):
            P.I("dve", "scalar_tensor_tensor", out=acc[:], in0=xf[:, i:i + T_SEQ], scalar=cw[:, i:i + 1], in1=acc[:], op0=ALU.mult, op1=ALU.add,
                R=[xf, convw, acc], W=[acc])
        P.I("act", "activation", out=z0[:], in_=acc[:], func=AF.Silu, R=[acc], W=[z0])
        uT = z0
        for blk in range(4):
            sl = slice(blk * 512, (blk + 1) * 512)
            P.I("pe", "matmul", A[0][:], wq_s[:], uT[:, sl], start=True, stop=True, R=[wq_s, uT], W=[A[0]])
            C.copy("act", z1[:, sl], A[0][:], R=[A[0]], W=[z1])
            P.I("pe", "matmul", A[1][:], wk_s[:], uT[:, sl], start=True, stop=True, R=[wk_s, uT], W=[A[1]])
            P.I("act", "activation", out=qd[:, sl], in_=A[1][:], func=AF.Copy, scale=scale, R=[A[1]], W=[qd])
        qmT, kmT = z1, qd
        for t in range(16):
            ps = A[2 + t % 2]
            P.I("pe", "matmul", ps[:, 0:128], uT[:, t * 128:(t + 1) * 128], wk_s[:], start=True, stop=True, R=[uT, wk_s], W=[ps])
            P.I("dve", "tensor_scalar", out=ka[:, t, :], in0=ps[:, 0:128], scalar1=a_tok[:, t, hl:hl + 1], scalar2=scale, op0=ALU.mult, op1=ALU.mult,
                R=[ps, a_tok], W=[ka])
        for src, dst in ((z2, vtok), (z3, gtok)):
            for g in range(2):
                tp = T[g % 2]
                for j in range(8):
                    t = g * 8 + j
                    P.I("pe", "transpose", out=tp[:, j, :], in_=src[:, t * 128:(t + 1) * 128], identity=C.ident_bf[:], R=[src, C.ident_bf], W=[tp])
                C.copy("dve" if dst is vtok else "act", dst[:, g * 8:(g + 1) * 8, 0:128], tp[:], R=[tp], W=[dst])
        P.I("dve", "memset", S[:], 0.0, W=[S])
        P.I("dve", "memset", Sb[:], 0.0, W=[Sb])
        yo = yT[u % 2]
        for c in range(16):
            cs = slice(c * 128, (c + 1) * 128)
            sp_ = A[c % 2]
            P.I("pe", "matmul", sp_[:, 0:128], kmT[:, cs], qmT[:, cs], start=True, stop=True, R=[kmT, qmT], W=[sp_])
            P.I("dve", "scalar_tensor_tensor", out=PTt[:], in0=sp_[:, 0:128], scalar=a_tok[:, c, hl:hl + 1], in1=triuT[:], op0=ALU.mult, op1=ALU.mult,
                R=[sp_, a_tok, triuT], W=[PTt])
            op_ = A[2 + c % 2]
            P.I("pe", "matmul", op_[:, 0:129], PTt[:], vtok[:, c, 0:129], start=True, stop=False, R=[PTt, vtok], W=[op_])
            P.I("pe", "matmul", op_[:, 0:129], qmT[:, cs], Sb[:, 0:129], start=False, stop=True, R=[qmT, Sb], W=[op_])
            stp = A[4 + c % 2]
            P.I("pe", "matmul", stp[:, 0:129], ka[:, c, :], vtok[:, c, 0:129], start=True, stop=True, R=[ka, vtok], W=[stp])
            P.I("dve", "tensor_tensor", out=S[:, 0:129], in0=S[:, 0:129], in1=stp[:, 0:129], op=ALU.add, R=[S, stp], W=[S])
            P.I("dve", "tensor_scalar", out=S[:, 0:129], in0=S[:, 0:129], scalar1=decb[:, c, hl:hl + 1], scalar2=None, op0=ALU.mult, R=[S, decb], W=[S])
            P.I("act", "activation", out=Sb[:, 0:129], in_=S[:, 0:129], func=AF.Copy, R=[S], W=[Sb])
            P.I("dve", "tensor_scalar", out=den[:, 0:1], in0=op_[:, 128:129], scalar1=r_tok[:, c, hl:hl + 1], scalar2=None, op0=ALU.mult,
                R=[op_, r_tok], W=[den])
            P.I("dve", "tensor_scalar", out=den[:, 4:5], in0=den[:, 0:1], scalar1=-1.0, scalar2=None, op0=ALU.mult, R=[den], W=[den])
            P.I("dve", "tensor_tensor", out=den[:, 0:1], in0=den[:, 0:1], in1=den[:, 4:5], op=ALU.max, R=[den], W=[den])
            P.I("dve", "tensor_tensor", out=den[:, 1:2], in0=den[:, 0:1], in1=e_tok[:, c, hl:hl + 1], op=ALU.max, R=[den, e_tok], W=[den])
            P.I("dve", "reciprocal", out=den[:, 2:3], in_=den[:, 1:2], R=[den], W=[den])
            P.I("dve", "tensor_tensor", out=den[:, 3:4], in0=den[:, 2:3], in1=r_tok[:, c, hl:hl + 1], op=ALU.mult, R=[den, r_tok], W=[den])
            P.I("dve", "scalar_tensor_tensor", out=ot[:], in0=op_[:, 0:128], scalar=den[:, 3:4], in1=gtok[:, c, :], op0=ALU.mult, op1=ALU.mult,
                R=[op_, den, gtok], W=[ot])
            tp = head_norm_out(ot[:], [ot], None, yo, c * 128)
            P.I("act", "activation", out=yo[:, cs], in_=tp[:, 0, :], func=AF.Copy, scale=mlg[:, hl:hl + 1], R=[tp, mlg], W=[yo])
        P.dma("sp", X.yT_d.t[u * 128:(u + 1) * 128, :], yo[:], R=yo, W=X.yT_d)
    if emb:
        P.end_phase()
        return P
    P.finish([X.yT_d])
    P.close()
    return P


def build_P0(ntok=1024, P=None, io=None):
    emb = P is not None
    if not emb:
        P = Prog()
    io = io or {}
    x_d = io["x"] if "x" in io else P.dram("x", [ntok, D], F32, "ExternalInput")
    idb_d = io["ident_bf"] if "ident_bf" in io else P.dram("ident_bf", [128, 128], BF16, "ExternalInput")
    idf_d = io["ident_f"] if "ident_f" in io else P.dram("ident_f", [128, 128], F32, "ExternalInput")
    hT_d = io["hT_out"] if "hT_out" in io else P.dram("hT_out", [ntok // 512, 128, KC * 512], BF16, "ExternalOutput")
    if emb:
        P.begin_phase()
    C = Common(P, idb_d, idf_d)
    xs = [P.sb([128, D], F32, f"xs{i}") for i in range(2)]
    hb = [P.sb([128, D], BF16, f"hb{i}") for i in range(2)]
    hT = P.sb([128, KC, 512], BF16, "hT")
    T = [P.ps([128, 8, 128], BF16, f"T{i}") for i in range(2)]
    for blk in range(ntok // 512):
        for tt in range(4):
            t = blk * 4 + tt
            P.dma("sp", xs[t % 2][:], x_d.t[t * 128:(t + 1) * 128, :], R=x_d, W=xs[t % 2])
            P.I("act", "activation", out=hb[t % 2][:], in_=xs[t % 2][:], func=AF.Copy, R=[xs[t % 2]], W=[hb[t % 2]])
            emit_transpose_to_hT(P, C, hb[t % 2], hT, tt, T)
        P.dma("sp", hT_d.t[blk], hT[:].rearrange("p c t -> p (c t)"), R=hT, W=hT_d)
    if emb:
        P.end_phase()
        return P
    P.finish([hT_d])
    P.close()
    return P


def build_fused(depth=DEPTH, n_exp=NE):
    P = Prog()
    x_d = P.dram("x", [T_SEQ, D], F32, "ExternalInput")
    memT_d = P.dram("memT", [D, 256], F32, "ExternalInput")
    idb_d = P.dram("ident_bf", [128, 128], BF16, "ExternalInput")
    idf_d = P.dram("ident_f", [128, 128], F32, "ExternalInput")
    out_d = P.dram("out", [T_SEQ, D], F32, "ExternalOutput")
    hT_s = P.dram("hT_s", [T_SEQ // 512, 128, KC * 512], BF16, "Internal")
    yT_s = P.dram("yT_s", [D, T_SEQ], BF16, "Internal")
    h_s = P.dram("h_s", [T_SEQ, D], F32, "Internal")
    ids = dict(ident_bf=idb_d, ident_f=idf_d)
    build_P0(T_SEQ, P=P, io=dict(x=x_d, hT_out=hT_s, **ids))
    for layer in range(depth):
        ioA = dict(hT=hT_s, yT_out=yT_s, **ids)
        if layer % 2 == 0:
            build_A_even(16, P=P, io=ioA, pre=f"L{layer}_")
        else:
            build_A_odd(16, P=P, io=ioA, pre=f"L{layer}_")
        ioB = dict(yT=yT_s, h_in=(x_d if layer == 0 else h_s), memT=memT_d,
                   h_out=(out_d if layer == depth - 1 else h_s), hT_out=hT_s, **ids)
        build_B(T_SEQ, n_exp, P=P, io=ioB, pre=f"L{layer}_")
    P.finish([out_d])
    P.close()
    return P


_bf = ml_dtypes.bfloat16
GW = 2048


def _consts():
    return dict(ident_bf=np.eye(128, dtype=np.float32).astype(_bf), ident_f=np.eye(128, dtype=np.float32))


def prep_A_even(inp, i, heads):
    nh = len(heads)
    w_in = inp["ab_w_in"][i]
    units = []
    for h in heads:
        units.append(np.concatenate([w_in[:, j * GW + h * 128:j * GW + (h + 1) * 128] for j in range(4)], axis=1))
    for h in heads:
        u = np.zeros((D, 512), np.float32)
        for jj, j in enumerate((4, 5, 6)):
            u[:, jj * 128:(jj + 1) * 128] = w_in[:, j * GW + h * 128:j * GW + (h + 1) * 128]
        units.append(u)
    pos = np.arange(T_SEQ, dtype=np.float64)
    inv = 10000.0 ** (-np.arange(0, 128, 2, dtype=np.float64) / 128)
    ang = (pos[:, None] * inv[None, :]).astype(np.float32).astype(np.float64)
    cos, sin = np.cos(ang).T, np.sin(ang).T
    ropec = np.concatenate([cos, cos], 0).astype(np.float32)
    ropes = np.concatenate([-sin, sin], 0).astype(np.float32)
    perm = np.zeros((128, 128), np.float32)
    for m in range(128):
        perm[(m + 64) % 128, m] = 1.0
    gam = np.array([1.0 - 2.0 ** (-(5.0 + h)) for h in heads], np.float64)
    ii = np.arange(128)
    DTm = np.zeros((nh, 128, 128), np.float32)
    for a, g in enumerate(gam):
        d = ii[None, :] - ii[:, None]
        DTm[a] = np.where(d >= 0, g ** np.maximum(d, 0), 0.0)
    qdec4 = np.stack([np.tile(g ** (ii + 1.0), 4) for g in gam]).astype(np.float32)
    kdec = np.stack([g ** (127.0 - ii) for g in gam], 1).astype(np.float32)
    cdec = np.tile((gam ** 128.0)[None, :], (128, 1)).astype(np.float32)
    retg = np.stack([inp["ab_ret_norm_g"][i][h * 128:(h + 1) * 128] for h in heads], 1)
    mlg = np.stack([inp["ab_mlstm_norm_g"][i][h * 128:(h + 1) * 128] for h in heads], 1)
    convw = np.zeros((128, nh * 5), np.float32)
    for a, h in enumerate(heads):
        convw[:, a * 5:a * 5 + 4] = inp["ab_conv_w"][i][:, h * 128:(h + 1) * 128].T
        convw[:, a * 5 + 4] = inp["ab_conv_b"][i][h * 128:(h + 1) * 128]
    wqk = np.stack([np.stack([inp["ab_wq"][i][h], inp["ab_wk"][i][h]]) for h in heads])
    wgate = np.stack([np.stack([w_in[:, 7 * GW + h] for h in heads], 1), np.stack([w_in[:, 7 * GW + 16 + h] for h in heads], 1)])
    gateb = np.stack([np.array([inp["ab_gate_b"][i][h], inp["ab_gate_b"][i][16 + h]]) for h in heads]).astype(np.float32)
    triuT = np.triu(np.ones((128, 128), np.float32))
    e127 = np.zeros((128, 128), np.float32)
    e127[127, :] = 1.0
    d = dict(w_units=np.ascontiguousarray(np.stack(units)), ropec=ropec, ropes=ropes, perm=perm.astype(_bf), DT=DTm, qdec4=qdec4,
             kdec=kdec, cdec=cdec, retg=np.ascontiguousarray(retg), mlg=np.ascontiguousarray(mlg), convw=convw,
             wqk=np.ascontiguousarray(wqk), wgate=np.ascontiguousarray(wgate), gateb=gateb, triuT=triuT, e127=e127)
    d.update(_consts())
    return d


def prep_A_odd(inp, i, heads):
    w_in = inp["cd_w_in"][i]
    rel = inp["cd_rel_bias"][i]

    def unit(g, h):
        return np.concatenate([w_in[:, (3 * g + j) * GW + h * 128:(3 * g + j) * GW + (h + 1) * 128] for j in range(3)], axis=1)
    w_units = np.stack([unit(0, h) for h in heads] + [unit(1, h) for h in heads])
    r = np.arange(64)[:, None]
    m = np.arange(704)[None, :]
    idx = np.clip(r + 512 - (m - 64), -63, 128) + 63
    bias_ext = np.stack([rel[h][idx] for h in heads]).astype(np.float32)
    d = dict(w_units=np.ascontiguousarray(w_units), bias_ext=bias_ext, tril=np.tril(np.ones((128, 128), np.float32), -1))
    d.update(_consts())
    return d


def prep_B(inp, layer):
    i = layer // 2
    w_out = inp["ab_w_out"][i] if layer % 2 == 0 else inp["cd_w_out"][i]
    lnp = np.stack([inp["mix_ln_g"][layer], inp["mix_ln_b"][layer], inp["xa_ln_g"][layer], inp["xa_ln_b"][layer],
                    inp["moe_ln_g"][layer], inp["moe_ln_b"][layer]])
    bg, bu = inp["moe_b_gate"][layer], inp["moe_b_up"][layer]
    bgu = np.zeros((128, NE * 4), np.float32)
    for c in range(2):
        bgu[:, c::4] = bg[:, c * 128:(c + 1) * 128].T
        bgu[:, 2 + c::4] = bu[:, c * 128:(c + 1) * 128].T
    d = dict(w_out=np.ascontiguousarray(w_out), lnp=lnp, xa_wq=np.ascontiguousarray(inp["xa_wq"][layer]),
             xa_wkv=np.ascontiguousarray(inp["xa_wkv"][layer]), xa_wo=np.ascontiguousarray(inp["xa_wo"][layer]),
             rw=np.ascontiguousarray(inp["moe_router_w"][layer]), rb=np.ascontiguousarray(inp["moe_router_b"][layer][None, :]),
             wg=np.ascontiguousarray(inp["moe_w_gate"][layer]), wu=np.ascontiguousarray(inp["moe_w_up"][layer]),
             wd=np.ascontiguousarray(inp["moe_w_down"][layer]), bgu=bgu, bd=np.ascontiguousarray(inp["moe_b_down"][layer]))
    d.update(_consts())
    return d


_PROGS = {}


def _prog(name):
    if name not in _PROGS:
        _PROGS[name] = {"P0": build_P0, "A_even": lambda: build_A_even(8), "A_odd": lambda: build_A_odd(8), "B": build_B,
                        "fused": build_fused}[name]()
    return _PROGS[name]


def fused_weights(inp, depth=DEPTH):
    w = {}
    heads = list(range(16))
    for layer in range(depth):
        i = layer // 2
        d = prep_A_even(inp, i, heads) if layer % 2 == 0 else prep_A_odd(inp, i, heads)
        d.update(prep_B(inp, layer))
        for k, v in d.items():
            if k in ("ident_bf", "ident_f"):
                continue
            w[f"L{layer}_{k}"] = v
    return w


def kernel(**inp):
    inp = {k: np.asarray(v) for k, v in inp.items()}
    x = inp["x"]
    mem = inp["mem"]
    w = fused_weights(inp)
    cst = _consts()
    maps = []
    for c in range(NCORES):
        b = c % 4
        d = dict(w)
        d.update(cst)
        d["x"] = np.ascontiguousarray(x[b])
        d["memT"] = np.ascontiguousarray(mem[b].T)
        maps.append(d)
    res = run_bass_kernel_spmd(_prog("fused").nc, maps, core_ids=list(range(NCORES))).results
    return np.stack([res[b]["out"] for b in range(4)]).astype(np.float32)
```

```python
import numpy as np
import ml_dtypes
from contextlib import ExitStack
import concourse.bass as bass
import concourse.mybir as mybir
from concourse.bass_utils import run_bass_kernel_spmd

F32 = mybir.dt.float32
BF16 = mybir.dt.bfloat16
AF = mybir.ActivationFunctionType
ALU = mybir.AluOpType
AX = mybir.AxisListType

D = 4096
KC = 32
DEPTH = 4
DN_ALPHA = (2.0 * DEPTH) ** 0.25
LN_EPS = 1e-5
HN_EPS = 1e-6
NE = 32
NCORES = 8


class DSem:
    __slots__ = ("h", "cnt", "key")

    def __init__(self, h, key):
        self.h = h
        self.cnt = 0
        self.key = key


class _BState:
    __slots__ = ("writer", "readers", "dsem", "dlast", "dwait_w", "dwait_r")

    def __init__(self):
        self.writer = None
        self.readers = {}
        self.dsem = {}
        self.dlast = None
        self.dwait_w = []
        self.dwait_r = []


class Buf:
    __slots__ = ("t", "name", "s", "is_dram")

    def __init__(self, t, name, is_dram=False):
        self.t = t
        self.name = name
        self.s = _BState()
        self.is_dram = is_dram

    def view(self, ap):
        b = Buf(ap, self.name, self.is_dram)
        b.s = self.s
        return b

    def __getitem__(self, idx):
        return self.t[idx]

    writer = property(lambda self: self.s.writer, lambda self, v: setattr(self.s, "writer", v))
    readers = property(lambda self: self.s.readers, lambda self, v: setattr(self.s, "readers", v))
    dsem = property(lambda self: self.s.dsem, lambda self, v: setattr(self.s, "dsem", v))
    dlast = property(lambda self: self.s.dlast, lambda self, v: setattr(self.s, "dlast", v))
    dwait_w = property(lambda self: self.s.dwait_w, lambda self, v: setattr(self.s, "dwait_w", v))
    dwait_r = property(lambda self: self.s.dwait_r, lambda self, v: setattr(self.s, "dwait_r", v))


class Prog:
    ENGS = ("pe", "act", "dve", "pool", "sp")

    def __init__(self):
        self.nc = bass.Bass("TRN2", target_bir_lowering=False)
        nc = self.nc
        self.st = ExitStack()
        self.top = self.st
        self.e = {"pe": nc.tensor, "act": nc.scalar, "dve": nc.vector, "pool": nc.gpsimd, "sp": nc.sync}
        self.sem = {k: self.top.enter_context(nc.semaphore("s_" + k)) for k in self.ENGS}
        self.cnt = {k: 0 for k in self.ENGS}
        self.waited = {}
        self.nbuf = 0
        self.ninst = 0
        self.dsems = []
        self.dfree = {"sw": [], "hw": []}
        self.phase_bufs = None
        self.dram_bufs = []

    def sb(self, shape, dt=F32, name=None):
        self.nbuf += 1
        name = (name or "sb") + f"_{self.nbuf}"
        t = self.st.enter_context(self.nc.sbuf_tensor(name, list(shape), dt))
        b = Buf(t, name)
        if self.phase_bufs is not None:
            self.phase_bufs.append(b)
        return b

    def ps(self, shape, dt=F32, name=None):
        self.nbuf += 1
        name = (name or "ps") + f"_{self.nbuf}"
        t = self.st.enter_context(self.nc.psum_tensor(name, list(shape), dt))
        b = Buf(t, name)
        if self.phase_bufs is not None:
            self.phase_bufs.append(b)
        return b

    def dram(self, name, shape, dt, kind):
        t = self.nc.dram_tensor(name, list(shape), dt, kind=kind)
        b = Buf(t.ap(), name, is_dram=True)
        self.dram_bufs.append(b)
        return b

    def _get_dsem(self, qt):
        if self.dfree[qt]:
            return self.dfree[qt].pop()
        d = DSem(self.top.enter_context(self.nc.semaphore(f"d{len(self.dsems)}")), f"d{len(self.dsems)}")
        self.dsems.append(d)
        return d

    def begin_phase(self):
        self.st = ExitStack()
        self.phase_bufs = []

    def barrier(self):
        for x in self.ENGS:
            for e in self.ENGS:
                if e != x:
                    self._wait(x, e, self.sem[e], self.cnt[e])
            for d in self.dsems:
                self._wait(x, d.key, d.h, 16 * d.cnt)

    def end_phase(self):
        self.barrier()
        for b in self.phase_bufs:
            for qt, d in b.dsem.items():
                self.dfree[qt].append(d)
            b.dsem = {}
        for b in self.dram_bufs:
            b.dwait_w = []
            b.dwait_r = []
            b.writer = None
            b.readers = {}
        self.st.close()
        self.st = self.top
        self.phase_bufs = None

    def _wait(self, x, key, sem, val):
        if val <= 0:
            return
        k2 = (x, key)
        if self.waited.get(k2, 0) >= val:
            return
        self.waited[k2] = val
        self.e[x].wait_ge(sem, val)

    def _wait_dma(self, x, b, skip_qt=None, for_write=True):
        for qt, d in b.dsem.items():
            if qt != skip_qt:
                self._wait(x, d.key, d.h, 16 * d.cnt)
        for d in b.dwait_w:
            self._wait(x, d.key, d.h, 16 * d.cnt)
        if for_write:
            for d in b.dwait_r:
                self._wait(x, d.key, d.h, 16 * d.cnt)

    def _deps(self, x, R, W, dma_skip=None, skip_qt=None):
        for b in R:
            if b.writer is not None:
                e, k = b.writer
                if not (x == "pe" and e == "pe"):
                    self._wait(x, e, self.sem[e], k)
            self._wait_dma(x, b, skip_qt if b is dma_skip else None, for_write=False)
        for b in W:
            if b.writer is not None:
                e, k = b.writer
                if not (x == "pe" and e == "pe"):
                    self._wait(x, e, self.sem[e], k)
            for e, k in b.readers.items():
                if not (x == "pe" and e == "pe"):
                    self._wait(x, e, self.sem[e], k)
            self._wait_dma(x, b, skip_qt if b is dma_skip else None)

    def I(self, x, fname, *args, R=(), W=(), **kw):
        self._deps(x, R, W)
        inst = getattr(self.e[x], fname)(*args, **kw)
        self.cnt[x] += 1
        self.ninst += 1
        k = self.cnt[x]
        inst.then_inc(self.sem[x], 1)
        for b in R:
            b.readers[x] = k
        for b in W:
            b.writer = (x, k)
            b.readers = {}
        return inst

    def dma(self, q, out, in_, R, W, **kw):
        if not W.is_dram:
            trk, kind = W, "w"
        elif not R.is_dram:
            trk, kind = R, "r"
        else:
            trk, kind = W, "w"
        qt = "sw" if q == "pool" else "hw"
        skip = trk if (trk.dlast == kind or trk.dlast is None) else None
        self._deps(q, [R], [W], dma_skip=skip, skip_qt=qt)
        if qt not in trk.dsem:
            trk.dsem[qt] = self._get_dsem(qt)
        ds = trk.dsem[qt]
        inst = self.e[q].dma_start(out=out, in_=in_, **kw)
        ds.cnt += 1
        trk.dlast = kind
        inst.then_inc(ds.h, 16)
        self.ninst += 1
        other = R if trk is W else W
        if other is not trk:
            lst = other.dwait_r if other is R else other.dwait_w
            if ds not in lst:
                lst.append(ds)
        return inst

    def finish(self, bufs, x="sp"):
        for b in bufs:
            self._wait_dma(x, b)

    def close(self):
        self.top.close()


class Common:
    def __init__(self, P, ident_bf_d, ident_f_d):
        self.P = P
        self.ident_bf = P.sb([128, 128], BF16, "identb")
        self.ident_f = P.sb([128, 128], F32, "identf")
        P.dma("sp", self.ident_bf[:], ident_bf_d[:], R=ident_bf_d, W=self.ident_bf)
        P.dma("sp", self.ident_f[:], ident_f_d[:], R=ident_f_d, W=self.ident_f)
        self.eps_ln = P.sb([128, 1], F32, "epsln")
        P.I("dve", "memset", self.eps_ln[:], LN_EPS, W=[self.eps_ln])
        self.eps_hn = P.sb([128, 1], F32, "epshn")
        P.I("dve", "memset", self.eps_hn[:], HN_EPS, W=[self.eps_hn])
        self.rr = 0

    def evac_engine(self):
        self.rr += 1
        return "act" if self.rr % 2 else "dve"

    def copy(self, eng, out, in_, R, W):
        P = self.P
        if eng == "act":
            P.I("act", "activation", out=out, in_=in_, func=AF.Copy, R=R, W=W)
        else:
            P.I(eng, "tensor_copy", out=out, in_=in_, R=R, W=W)


def emit_layernorm(P, C, x, G, Bt, stats, mv, hb=None):
    xs = x[:].rearrange("p (c f) -> p c f", f=512)
    for c in range(8):
        P.I("dve", "bn_stats", out=stats[:, c, :], in_=xs[:, c, :], R=[x], W=[stats])
    P.I("dve", "bn_aggr", out=mv[:, 0:2], in_=stats[:], R=[stats], W=[mv])
    P.I("act", "activation", out=mv[:, 2:3], in_=mv[:, 1:2], func=AF.Sqrt, bias=C.eps_ln[:], scale=1.0,
        R=[mv, C.eps_ln], W=[mv])
    P.I("dve", "reciprocal", out=mv[:, 3:4], in_=mv[:, 2:3], R=[mv], W=[mv])
    P.I("dve", "tensor_scalar", out=x[:], in0=x[:], scalar1=mv[:, 0:1], scalar2=mv[:, 3:4],
        op0=ALU.subtract, op1=ALU.mult, R=[x, mv], W=[x])
    P.I("dve", "tensor_tensor", out=x[:], in0=x[:], in1=G[:], op=ALU.mult, R=[x, G], W=[x])
    P.I("dve", "tensor_tensor", out=x[:], in0=x[:], in1=Bt[:], op=ALU.add, R=[x, Bt], W=[x])
    if hb is not None:
        P.I("act", "activation", out=hb[:], in_=x[:], func=AF.Copy, R=[x], W=[hb])


def emit_transpose_to_hT(P, C, hb, hT, tt, tps):
    for g in range(4):
        tp = tps[g % len(tps)]
        for j in range(8):
            c = g * 8 + j
            P.I("pe", "transpose", out=tp[:, j, :], in_=hb[:, c:D:KC], identity=C.ident_bf[:],
                R=[hb, C.ident_bf], W=[tp])
        C.copy(C.evac_engine(), hT[:, g * 8:(g + 1) * 8, tt * 128:(tt + 1) * 128], tp[:], R=[tp], W=[hT])


def build_B(ntok=1024, n_exp=NE, P=None, io=None, pre=""):
    emb = P is not None
    if not emb:
        P = Prog()
    io = io or {}
    TB = 512
    nblk = ntok // TB
    dr = {}

    def din(name, shape, dt=F32):
        dr[name] = io[name] if name in io else P.dram(pre + name, shape, dt, "ExternalInput")
        return dr[name]

    yT_d = din("yT", [D, ntok], BF16)
    hin_d = din("h_in", [ntok, D])
    memT_d = din("memT", [D, 256])
    wout_d = din("w_out", [D, D])
    lnp_d = din("lnp", [6, D])
    wq_d = din("xa_wq", [D, 512])
    wkv_d = din("xa_wkv", [D, 1024])
    wo_d = din("xa_wo", [512, D])
    rw_d = din("rw", [D, NE])
    rb_d = din("rb", [1, NE])
    wg_d = din("wg", [NE, D, 256])
    wu_d = din("wu", [NE, D, 256])
    wd_d = din("wd", [NE, 256, D])
    bgu_d = din("bgu", [128, NE * 4])
    bd_d = din("bd", [NE, D])
    idb_d = din("ident_bf", [128, 128], BF16)
    idf_d = din("ident_f", [128, 128])
    hout_d = io["h_out"] if "h_out" in io else P.dram("h_out", [ntok, D], F32, "ExternalOutput")
    hTout_d = io["hT_out"] if "hT_out" in io else P.dram("hT_out", [ntok // 512, 128, KC * 512], BF16, "ExternalOutput")
    if emb:
        P.begin_phase()

    C = Common(P, idb_d, idf_d)

    hres = [P.sb([128, D], F32, f"hres{i}") for i in range(4)]
    hT = P.sb([128, KC, TB], BF16, "hT")
    hb = P.sb([128, D], BF16, "hb")
    lnact = P.sb([128, 2 * D], F32, "lnact")
    lnG = lnact.view(lnact.t[:, 0:D])
    lnB = lnact.view(lnact.t[:, D:2 * D])
    actH = lnact.view(lnact.t[:].bitcast(BF16).rearrange("p (e c t) -> p e c t", e=16, c=2))
    NRING = 4
    ring = [P.sb([128, 8, 512], BF16, f"ring{i}") for i in range(NRING)]
    ring_i = [0]

    def next_piece():
        r = ring[ring_i[0] % NRING]
        ring_i[0] += 1
        return r

    stats = P.sb([128, 8, 6], F32, "stats")
    mv = P.sb([128, 4], F32, "mv")
    kT = P.sb([128, 4, 256], BF16, "kT")
    vv = P.sb([128, 2, 512], BF16, "vv")
    qT = P.sb([128, 4, TB], BF16, "qT")
    oT = P.sb([128, 4, TB], BF16, "oT")
    PT = P.sb([128, 2, TB], BF16, "PT")
    sc = P.sb([128, 256], F32, "sc")
    pb = P.sb([128, 256], BF16, "pb")
    red = P.sb([128, 8], F32, "red")
    rws = P.sb([128, KC, NE], BF16, "rws")
    rbt = P.sb([128, NE], F32, "rbt")
    lg = P.sb([128, NE], F32, "lg")
    top8 = P.sb([128, 8], F32, "top8")
    ex = P.sb([128, NE], F32, "ex")
    msk = P.sb([128, NE], F32, "msk")
    gate = [P.sb([128, NE], F32, f"gate{i}") for i in range(4)]
    gateT = P.sb([NE, TB], BF16, "gateT")
    bgu = P.sb([128, NE * 4], F32, "bgu")
    tgs = [P.sb([128, TB], F32, f"tg{i}") for i in range(2)]
    tsg = P.sb([128, TB], F32, "tsg")
    tus = [P.sb([128, TB], F32, f"tu{i}") for i in range(2)]
    sel = P.sb([NE, NE, 128], BF16, "sel")

    A = [P.ps([128, 512], F32, f"A{i}") for i in range(8)]
    T = [A[6 + i].view(A[6 + i].t[:].bitcast(BF16).rearrange("p (j n) -> p j n", n=128)) for i in range(2)]
    for e in range(NE):
        P.I("dve", "tensor_copy", out=sel[:, e, :], in_=C.ident_bf[0:NE, e:e + 1].to_broadcast([NE, 128]), R=[C.ident_bf], W=[sel])

    P.dma("sp", bgu[:], bgu_d[:], R=bgu_d, W=bgu)
    P.dma("sp", rbt[:], rb_d.t[0, :].partition_broadcast(128), R=rb_d, W=rbt)
    P.dma("pool", rws[:], rw_d.t.rearrange("(p c) n -> p c n", c=KC), R=rw_d, W=rws)

    def wview(wd2):
        return wd2.rearrange("(c p) n -> p c n", p=128)

    def fview(wd2):
        return wd2.rearrange("(p c) n -> p c n", c=KC)

    P.dma("pool", hT[:, :, 0:256], fview(memT_d.t), R=memT_d, W=hT)
    wkv_v = fview(wkv_d.t)
    for hd in range(4):
        pieces = []
        for g in range(4):
            r = next_piece()
            P.dma("pool", r[:, :, 0:128], wkv_v[:, g * 8:(g + 1) * 8, hd * 128:(hd + 1) * 128], R=wkv_d, W=r)
            pieces.append(r)
        for c in range(KC):
            r = pieces[c // 8]
            P.I("pe", "matmul", A[0][:, 0:256], r[:, c % 8, 0:128], hT[:, c, 0:256], start=(c == 0), stop=(c == KC - 1),
                R=[r, hT], W=[A[0]])
        C.copy("act", kT[:, hd, :], A[0][:, 0:256], R=[A[0]], W=[kT])
    for mt in range(2):
        for g in range(4):
            r = next_piece()
            P.dma("pool", r[:], wkv_v[:, g * 8:(g + 1) * 8, 512:1024], R=wkv_d, W=r)
            for j in range(8):
                c = g * 8 + j
                P.I("pe", "matmul", A[1][:], hT[:, c, mt * 128:(mt + 1) * 128], r[:, j, :], start=(c == 0), stop=(c == KC - 1),
                    R=[r, hT], W=[A[1]])
        C.copy("act", vv[:, mt, :], A[1][:], R=[A[1]], W=[vv])

    def load_ln(i):
        P.dma("sp", lnG[:], lnp_d.t[2 * i, :].partition_broadcast(128), R=lnp_d, W=lnG)
        P.dma("sp", lnB[:], lnp_d.t[2 * i + 1, :].partition_broadcast(128), R=lnp_d, W=lnB)

    def proj_residual(w_v, nk, src_T):
        for db in range(8):
            pieces = []
            if nk >= 8:
                for g in range(nk // 8):
                    r = next_piece()
                    P.dma("pool", r[:], w_v[:, g * 8:(g + 1) * 8, db * 512:(db + 1) * 512], R=dr["w_out"], W=r)
                    pieces.append(r)
            else:
                r = next_piece()
                P.dma("pool", r[:, 0:nk, :], w_v[:, 0:nk, db * 512:(db + 1) * 512], R=dr["xa_wo"], W=r)
                pieces.append(r)
            for tt in range(4):
                for c in range(nk):
                    r = pieces[c // 8]
                    P.I("pe", "matmul", A[tt][:], src_T[:, c, tt * 128:(tt + 1) * 128], r[:, c % 8, :],
                        start=(c == 0), stop=(c == nk - 1), R=[r, src_T], W=[A[tt]])
            for tt in range(4):
                P.I("dve", "scalar_tensor_tensor", out=hres[tt][:, db * 512:(db + 1) * 512],
                    in0=hres[tt][:, db * 512:(db + 1) * 512], scalar=DN_ALPHA, in1=A[tt][:],
                    op0=ALU.mult, op1=ALU.add, R=[hres[tt], A[tt]], W=[hres[tt]])

    def ln_and_transpose(i):
        load_ln(i)
        for tt in range(4):
            emit_layernorm(P, C, hres[tt], lnG, lnB, stats, mv, hb)
            emit_transpose_to_hT(P, C, hb, hT, tt, T)

    for blk in range(nblk):
        t0 = blk * TB
        P.dma("sp", hT[:], wview(yT_d.t)[:, :, t0:t0 + TB], R=yT_d, W=hT)
        for tt in range(4):
            P.dma("sp", hres[tt][:], hin_d.t[t0 + tt * 128:t0 + (tt + 1) * 128, :], R=hin_d, W=hres[tt])
        proj_residual(wview(wout_d.t), KC, hT)
        ln_and_transpose(0)
        wq_v = fview(wq_d.t)
        pieces = []
        for g in range(4):
            r = next_piece()
            P.dma("pool", r[:], wq_v[:, g * 8:(g + 1) * 8, :], R=wq_d, W=r)
            pieces.append(r)
        for hd in range(4):
            for c in range(KC):
                r = pieces[c // 8]
                P.I("pe", "matmul", A[hd][:], r[:, c % 8, hd * 128:(hd + 1) * 128], hT[:, c, :],
                    start=(c == 0), stop=(c == KC - 1), R=[r, hT], W=[A[hd]])
        for hd in range(4):
            C.copy(C.evac_engine(), qT[:, hd, :], A[hd][:], R=[A[hd]], W=[qT])
        for hd in range(4):
            for tt in range(4):
                sp_ = A[4 + (tt % 2)]
                P.I("pe", "matmul", sp_[:, 0:256], qT[:, hd, tt * 128:(tt + 1) * 128], kT[:, hd, :], start=True, stop=True,
                    R=[qT, kT], W=[sp_])
                P.I("dve", "reduce_max", out=red[:, 0:1], in_=sp_[:, 0:256], axis=AX.X, R=[sp_], W=[red])
                P.I("dve", "tensor_scalar", out=red[:, 1:2], in0=red[:, 0:1], scalar1=-(128.0 ** -0.5), scalar2=None,
                    op0=ALU.mult, R=[red], W=[red])
                P.I("act", "activation", out=sc[:], in_=sp_[:, 0:256], func=AF.Exp, bias=red[:, 1:2], scale=128.0 ** -0.5,
                    accum_out=red[:, 2:3], R=[sp_, red], W=[sc, red])
                P.I("dve", "reciprocal", out=red[:, 3:4], in_=red[:, 2:3], R=[red], W=[red])
                P.I("dve", "tensor_scalar", out=pb[:], in0=sc[:], scalar1=red[:, 3:4], scalar2=None, op0=ALU.mult,
                    R=[sc, red], W=[pb])
                tp = T[tt % 2]
                for mc in range(2):
                    P.I("pe", "transpose", out=tp[:, mc, :], in_=pb[:, mc * 128:(mc + 1) * 128], identity=C.ident_bf[:],
                        R=[pb, C.ident_bf], W=[tp])
                C.copy("act", PT[:, :, tt * 128:(tt + 1) * 128], tp[:, 0:2, :], R=[tp], W=[PT])
            for mc in range(2):
                P.I("pe", "matmul", A[hd][:], vv[:, mc, hd * 128:(hd + 1) * 128], PT[:, mc, :], start=(mc == 0), stop=(mc == 1),
                    R=[vv, PT], W=[A[hd]])
            C.copy("act", oT[:, hd, :], A[hd][:], R=[A[hd]], W=[oT])
        proj_residual(wview(wo_d.t), 4, oT)
        ln_and_transpose(1)
        for tt in range(4):
            lp = A[4 + (tt % 2)]
            for c in range(KC):
                P.I("pe", "matmul", lp[:, 0:NE], hT[:, c, tt * 128:(tt + 1) * 128], rws[:, c, :], start=(c == 0), stop=(c == KC - 1),
                    R=[hT, rws], W=[lp])
            P.I("dve", "tensor_tensor", out=lg[:], in0=lp[:, 0:NE], in1=rbt[:], op=ALU.add, R=[lp, rbt], W=[lg])
            P.I("dve", "max", out=top8[:], in_=lg[:], R=[lg], W=[top8])
            P.I("dve", "tensor_scalar", out=red[:, 4:5], in0=top8[:, 0:1], scalar1=-1.0, scalar2=None, op0=ALU.mult,
                R=[top8], W=[red])
            P.I("act", "activation", out=ex[:], in_=lg[:], func=AF.Exp, bias=red[:, 4:5], scale=1.0, R=[lg, red], W=[ex])
            P.I("dve", "tensor_scalar", out=msk[:], in0=lg[:], scalar1=top8[:, 3:4], scalar2=None, op0=ALU.is_ge,
                R=[lg, top8], W=[msk])
            P.I("dve", "tensor_tensor", out=ex[:], in0=ex[:], in1=msk[:], op=ALU.mult, R=[ex, msk], W=[ex])
            P.I("dve", "reduce_sum", out=red[:, 5:6], in_=ex[:], axis=AX.X, R=[ex], W=[red])
            P.I("dve", "reciprocal", out=red[:, 6:7], in_=red[:, 5:6], R=[red], W=[red])
            P.I("dve", "tensor_scalar", out=gate[tt][:], in0=ex[:], scalar1=red[:, 6:7], scalar2=None, op0=ALU.mult,
                R=[ex, red], W=[gate[tt]])
            P.I("pe", "transpose", out=lp[0:NE, 128:256], in_=gate[tt][:], identity=C.ident_f[:], R=[gate[tt], C.ident_f], W=[lp])
            C.copy("act", gateT[:, tt * 128:(tt + 1) * 128], lp[0:NE, 128:256], R=[lp], W=[gateT])
        for db in range(8):
            r = next_piece()
            P.dma("pool", r[0:NE, 0, :], bd_d.t[:, db * 512:(db + 1) * 512], R=bd_d, W=r)
            for tt in range(4):
                yp = A[4 + (tt % 2)]
                P.I("pe", "matmul", yp[:], gateT[:, tt * 128:(tt + 1) * 128], r[0:NE, 0, :], start=True, stop=True,
                    R=[gateT, r], W=[yp])
                P.I("dve", "scalar_tensor_tensor", out=hres[tt][:, db * 512:(db + 1) * 512],
                    in0=hres[tt][:, db * 512:(db + 1) * 512], scalar=DN_ALPHA, in1=yp[:],
                    op0=ALU.mult, op1=ALU.add, R=[hres[tt], yp], W=[hres[tt]])
        for half in range(2):
            e0 = half * 16
            nE = min(16, max(0, n_exp - e0))
            if nE == 0:
                continue
            for el in range(nE):
                e = e0 + el
                wg_v = fview(wg_d.t[e])
                wu_v = fview(wu_d.t[e])
                for g in range(4):
                    r = next_piece()
                    rv = r[:].rearrange("p j n -> p (j n)").rearrange("p (h j n) -> p h j n", h=2, j=8)
                    P.dma("pool", rv[:, 0], wg_v[:, g * 8:(g + 1) * 8, :], R=wg_d, W=r)
                    P.dma("pool", rv[:, 1], wu_v[:, g * 8:(g + 1) * 8, :], R=wu_d, W=r)
                    for j in range(8):
                        c = g * 8 + j
                        for m in range(4):
                            P.I("pe", "matmul", A[m][:], rv[:, m // 2, j, (m % 2) * 128:(m % 2 + 1) * 128], hT[:, c, :],
                                start=(c == 0), stop=(c == KC - 1), R=[r, hT], W=[A[m]])
                gb = A[4 + e % 2]
                P.I("pe", "matmul", gb[:], sel[:, e, :], gateT[:], start=True, stop=True, R=[sel, gateT], W=[gb])
                for fc in range(2):
                    P.I("dve", "tensor_scalar", out=tgs[fc][:], in0=A[fc][:], scalar1=bgu[:, e * 4 + fc:e * 4 + fc + 1], scalar2=7.0,
                        op0=ALU.add, op1=ALU.min, R=[A[fc], bgu], W=[tgs[fc]])
                    P.I("act", "activation", out=tus[fc][:], in_=A[2 + fc][:], func=AF.Identity, bias=bgu[:, e * 4 + 2 + fc:e * 4 + 3 + fc],
                        scale=1.0, R=[A[2 + fc], bgu], W=[tus[fc]])
                for fc in range(2):
                    tg, tu = tgs[fc], tus[fc]
                    P.I("act", "activation", out=tsg[:], in_=tg[:], func=AF.Sigmoid, scale=1.702, R=[tg], W=[tsg])
                    P.I("dve", "tensor_scalar", out=tu[:], in0=tu[:], scalar1=7.0, scalar2=-7.0, op0=ALU.min, op1=ALU.max,
                        R=[tu], W=[tu])
                    P.I("dve", "tensor_tensor", out=tg[:], in0=tg[:], in1=tsg[:], op=ALU.mult, R=[tg, tsg], W=[tg])
                    P.I("dve", "tensor_tensor", out=tg[:], in0=tg[:], in1=gb[:], op=ALU.mult, R=[tg, gb], W=[tg])
                    P.I("dve", "scalar_tensor_tensor", out=actH[:, el, fc, :], in0=tu[:], scalar=1.0, in1=tg[:], op0=ALU.add, op1=ALU.mult,
                        R=[tu, tg], W=[actH])
            npc = (nE + 3) // 4
            for db in range(8):
                pcs = []
                for q in range(npc):
                    r = next_piece()
                    ne_q = min(4, nE - q * 4)
                    src = wd_d.t[e0 + q * 4:e0 + q * 4 + ne_q].rearrange("e (c p) n -> p (e c) n", p=128)[:, :, db * 512:(db + 1) * 512]
                    P.dma("pool", r[:, 0:2 * ne_q, :], src, R=wd_d, W=r)
                    pcs.append((r, ne_q))
                for q, (r, ne_q) in enumerate(pcs):
                    for tt in range(4):
                        yp = A[(db % 2) * 4 + tt]
                        for i in range(2 * ne_q):
                            el, fc = q * 4 + i // 2, i % 2
                            first = (q == 0 and i == 0)
                            last = (q == npc - 1 and i == 2 * ne_q - 1)
                            P.I("pe", "matmul", yp[:], actH[:, el, fc, tt * 128:(tt + 1) * 128], r[:, i, :], start=first, stop=last,
                                R=[actH, r], W=[yp])
                for tt in range(4):
                    yp = A[(db % 2) * 4 + tt]
                    P.I("dve", "tensor_tensor", out=hres[tt][:, db * 512:(db + 1) * 512], in0=yp[:],
                        in1=hres[tt][:, db * 512:(db + 1) * 512], op=ALU.add, R=[yp, hres[tt]], W=[hres[tt]])
        load_ln(2)
        for tt in range(4):
            emit_layernorm(P, C, hres[tt], lnG, lnB, stats, mv, hb)
            P.dma("sp", hout_d.t[t0 + tt * 128:t0 + (tt + 1) * 128, :], hres[tt][:], R=hres[tt], W=hout_d)
            emit_transpose_to_hT(P, C, hb, hT, tt, T)
        P.dma("sp", hTout_d.t[blk], hT[:].rearrange("p c t -> p (c t)"), R=hT, W=hTout_d)
    if emb:
        P.end_phase()
        return P
    P.finish([hout_d, hTout_d])
    P.close()
    return P


T_SEQ = 2048


class ACtx:
    pass


def a_setup(P, n_units, ncols, extra_in, io=None, pre="", emb=False, nW=1):
    io = io or {}
    X = ACtx()
    X.P = P

    def din(name, shape, dt):
        return io[name] if name in io else P.dram(pre + name, shape, dt, "ExternalInput")
    X.hT_d = din("hT", [T_SEQ // 512, 128, KC * 512], BF16)
    X.w_d = din("w_units", [n_units, D, ncols], F32)
    idb_d = din("ident_bf", [128, 128], BF16)
    idf_d = din("ident_f", [128, 128], F32)
    X.ex = {}
    for name, shape, dt in extra_in:
        X.ex[name] = din(name, shape, dt)
    X.yT_d = io["yT_out"] if "yT_out" in io else P.dram("yT_out", [n_units * 128, T_SEQ], BF16, "ExternalOutput")
    if emb:
        P.begin_phase()
    X.C = Common(P, idb_d, idf_d)
    X.hTb = [P.sb([128, KC, 512], BF16, f"hTb{i}") for i in range(2)]
    X.Ws = [P.sb([128, KC, ncols], BF16, f"Wunit{i}") for i in range(nW)]
    X.nunit = 0
    X.nload = 0
    return X


def a_inproj(X, u, ncols, sinks, A):
    P = X.P
    wv = X.w_d.t[u].rearrange("(p c) n -> p c n", c=KC)
    Wb = X.Ws[X.nunit % len(X.Ws)]
    X.nunit += 1
    for g in range(4):
        P.dma("pool", Wb[:, g * 8:(g + 1) * 8, :], wv[:, g * 8:(g + 1) * 8, :], R=X.w_d, W=Wb)
    nb = ncols // 128
    for blk in range(4):
        hb_ = X.hTb[X.nload % 2]
        X.nload += 1
        P.dma("sp", hb_[:].rearrange("p c t -> p (c t)"), X.hT_d.t[blk], R=X.hT_d, W=hb_)
        for j in range(nb):
            ps = A[j % len(A)]
            for c in range(KC):
                P.I("pe", "matmul", ps[:], Wb[:, c, j * 128:(j + 1) * 128], hb_[:, c, :], start=(c == 0), stop=(c == KC - 1),
                    R=[Wb, hb_], W=[ps])
            sinks[j](blk, ps)


def a_to_tokmajor(X, srcT, dst, T):
    P = X.P
    for g in range(2):
        tp = T[g % len(T)]
        for j in range(8):
            t = g * 8 + j
            P.I("pe", "transpose", out=tp[:, j, :], in_=srcT[:, t * 128:(t + 1) * 128], identity=X.C.ident_bf[:],
                R=[srcT, X.C.ident_bf], W=[tp])
        X.C.copy(X.C.evac_engine(), dst[:, g * 8:(g + 1) * 8, :], tp[:], R=[tp], W=[dst])


def build_A_odd(nh=8, P=None, io=None, pre=""):
    emb = P is not None
    if not emb:
        P = Prog()
    n_units = 2 * nh
    X = a_setup(P, n_units, 384, [("bias_ext", [nh, 64, 704], F32), ("tril", [128, 128], F32)], io, pre, emb, nW=2)
    C = X.C
    scale = 128.0 ** -0.5
    A = [P.ps([128, 512], F32, f"A{i}") for i in range(4)]
    S2 = P.ps([128, 1024], F32, "S2")
    T = [P.ps([128, 8, 128], BF16, f"T{i}") for i in range(2)]

    qT = P.sb([128, T_SEQ], BF16, "qT")
    kT = P.sb([128, T_SEQ], BF16, "kT")
    vT = P.sb([128, T_SEQ], BF16, "vT")
    v = P.sb([128, 16, 128], BF16, "v")
    yT = [P.sb([128, T_SEQ], BF16, f"yT{i}") for i in range(2)]
    tril = P.sb([128, 128], F32, "tril")
    P.dma("sp", tril[:], X.ex["tril"][:], R=X.ex["tril"], W=tril)
    one = P.sb([128, 1], F32, "one")
    P.I("dve", "memset", one[:], 1.0, W=[one])
    red = P.sb([128, 8], F32, "red")

    def mk_sinks():
        def sq(blk, ps):
            P.I("act", "activation", out=qT[:, blk * 512:(blk + 1) * 512], in_=ps[:], func=AF.Copy, scale=scale, R=[ps], W=[qT])

        def sk(blk, ps):
            P.I("dve", "tensor_copy", out=kT[:, blk * 512:(blk + 1) * 512], in_=ps[:], R=[ps], W=[kT])

        def sv(blk, ps):
            P.I("act", "activation", out=vT[:, blk * 512:(blk + 1) * 512], in_=ps[:], func=AF.Copy, R=[ps], W=[vT])
        return [sq, sk, sv]

    bias = P.sb([64, 704], F32, "bias")
    s_sb = P.sb([64, 640], F32, "s_sb")
    Pb = [P.sb([64, 640], BF16, f"Pb{i}") for i in range(2)]
    pe_ = P.sb([64, 640], F32, "pexp")
    PTb = P.sb([128, 5, 64], BF16, "PTb")
    for i in range(2):
        P.I("dve", "memset", Pb[i][:], 0.0, W=[Pb[i]])
    for hl in range(nh):
        u = hl
        a_inproj(X, u, 384, mk_sinks(), A[0:3])
        a_to_tokmajor(X, vT, v, T)
        P.dma("sp", bias[:], X.ex["bias_ext"].t[hl], R=X.ex["bias_ext"], W=bias)
        yo = yT[u % 2]
        for c in range(32):
            par = c % 2
            wb0 = (c - 8) // 2
            lo_band, hi_band = (0, 576) if par == 0 else (64, 640)
            lo = max(lo_band, max(0, -wb0) * 128)
            hi = hi_band
            k0 = wb0 * 128
            for (a, b_) in ((lo, min(hi, 512)), (max(lo, 512), hi)):
                if b_ > a:
                    P.I("pe", "matmul", S2[0:64, a:b_], qT[:, c * 64:(c + 1) * 64], kT[:, k0 + a:k0 + b_], start=True, stop=True,
                        R=[qT, kT], W=[S2])
            boff = 64 if par == 0 else 0
            P.I("dve", "tensor_tensor", out=s_sb[:, lo:hi], in0=S2[0:64, lo:hi], in1=bias[:, boff + lo:boff + hi], op=ALU.add,
                R=[S2, bias], W=[s_sb])
            P.I("dve", "reduce_max", out=red[0:64, 0:1], in_=s_sb[:, lo:hi], axis=AX.X, R=[s_sb], W=[red])
            P.I("dve", "tensor_scalar", out=red[0:64, 1:2], in0=red[0:64, 0:1], scalar1=-1.0, scalar2=None, op0=ALU.mult, R=[red], W=[red])
            P.I("act", "activation", out=pe_[:, lo:hi], in_=s_sb[:, lo:hi], func=AF.Exp, bias=red[0:64, 1:2], scale=1.0,
                accum_out=red[0:64, 2:3], R=[s_sb, red], W=[pe_, red])
            P.I("dve", "reciprocal", out=red[0:64, 3:4], in_=red[0:64, 2:3], R=[red], W=[red])
            pbuf = Pb[par]
            P.I("dve", "tensor_scalar", out=pbuf[:, lo:hi], in0=pe_[:, lo:hi], scalar1=red[0:64, 3:4], scalar2=None, op0=ALU.mult,
                R=[pe_, red], W=[pbuf])
            wbs = [w for w in range(5) if wb0 + w >= 0]
            tp = T[c % 2]
            for w in wbs:
                P.I("pe", "transpose", out=tp[:, w, 0:64], in_=pbuf[:, w * 128:(w + 1) * 128], identity=C.ident_bf[0:64, 0:64],
                    R=[pbuf, C.ident_bf], W=[tp])
            C.copy("act", PTb[:, wbs[0]:5, :], tp[:, wbs[0]:5, 0:64], R=[tp], W=[PTb])
            op_ = A[3]
            for i, w in enumerate(wbs):
                P.I("pe", "matmul", op_[:, 0:64], v[:, wb0 + w, :], PTb[:, w, :], start=(i == 0), stop=(i == len(wbs) - 1),
                    R=[v, PTb], W=[op_])
            C.copy("dve", yo[:, c * 64:(c + 1) * 64], op_[:, 0:64], R=[op_], W=[yo])
        P.dma("sp", X.yT_d.t[u * 128:(u + 1) * 128, :], yo[:], R=yo, W=X.yT_d)

    E = P.sb([128, 512], F32, "E")
    SP = P.sb([128, 512], F32, "SP")
    Cc = P.sb([128, 512], F32, "Cc")
    tt_ = P.sb([128, T_SEQ], F32, "tt")
    Ab = P.sb([128, 512], BF16, "Ab")
    AT = P.sb([128, 4, 128], BF16, "AT")
    carry = P.sb([128, 2], F32, "carry")
    onesrow = P.sb([128, 512], F32, "onesrow")
    P.I("dve", "memset", onesrow[:], 1.0, W=[onesrow])
    for hl in range(nh):
        u = nh + hl
        a_inproj(X, u, 384, mk_sinks(), A[0:3])
        a_to_tokmajor(X, vT, v, T)
        yo = yT[u % 2]
        for qi in range(16):
            nkb = qi + 1
            nch = (nkb + 3) // 4
            for ch in range(nch):
                kb0 = ch * 4
                nb_ = min(4, nkb - kb0)
                w_ = nb_ * 128
                Z = A[ch % 2]
                P.I("pe", "matmul", Z[:, 0:w_], qT[:, qi * 128:(qi + 1) * 128], kT[:, kb0 * 128:kb0 * 128 + w_], start=True, stop=True,
                    R=[qT, kT], W=[Z])
                P.I("act", "activation", out=E[:, 0:w_], in_=Z[:, 0:w_], func=AF.Exp, R=[Z], W=[E])
                P.I("act", "activation", out=SP[:, 0:w_], in_=E[:, 0:w_], func=AF.Ln, bias=one[:], scale=1.0, R=[E, one], W=[SP])
                last = (ch == nch - 1)
                if last:
                    d0 = (nb_ - 1) * 128
                    P.I("dve", "tensor_tensor", out=SP[:, d0:d0 + 128], in0=SP[:, d0:d0 + 128], in1=tril[:], op=ALU.mult,
                        R=[SP, tril], W=[SP])
                init = 0.0 if ch == 0 else carry[:, 0:1]
                rd_ = [onesrow, SP] + ([] if ch == 0 else [carry])
                P.I("dve", "tensor_tensor_scan", out=Cc[:, 0:w_], data0=onesrow[:, 0:w_], data1=SP[:, 0:w_], initial=init,
                    op0=ALU.mult, op1=ALU.add, R=rd_, W=[Cc])
                P.I("dve", "tensor_copy", out=carry[:, 0:1], in_=Cc[:, w_ - 1:w_], R=[Cc], W=[carry])
                P.I("dve", "tensor_tensor", out=E[:, 0:w_], in0=Z[:, 0:w_], in1=SP[:, 0:w_], op=ALU.subtract, R=[Z, SP], W=[E])
                P.I("dve", "tensor_tensor", out=tt_[:, kb0 * 128:kb0 * 128 + w_], in0=E[:, 0:w_], in1=Cc[:, 0:w_], op=ALU.add,
                    R=[E, Cc], W=[tt_])
            P.I("dve", "tensor_scalar", out=carry[:, 1:2], in0=carry[:, 0:1], scalar1=-1.0, scalar2=None, op0=ALU.mult, R=[carry], W=[carry])
            op_ = A[2 + (qi % 2)]
            for ch in range(nch):
                kb0 = ch * 4
                nb_ = min(4, nkb - kb0)
                w_ = nb_ * 128
                P.I("act", "activation", out=Ab[:, 0:w_], in_=tt_[:, kb0 * 128:kb0 * 128 + w_], func=AF.Exp, bias=carry[:, 1:2], scale=1.0,
                    R=[tt_, carry], W=[Ab])
                if ch == nch - 1:
                    d0 = (nb_ - 1) * 128
                    P.I("dve", "tensor_tensor", out=Ab[:, d0:d0 + 128], in0=Ab[:, d0:d0 + 128], in1=tril[:], op=ALU.mult,
                        R=[Ab, tril], W=[Ab])
                tp = T[ch % 2]
                for j in range(nb_):
                    P.I("pe", "transpose", out=tp[:, j, :], in_=Ab[:, j * 128:(j + 1) * 128], identity=C.ident_bf[:],
                        R=[Ab, C.ident_bf], W=[tp])
                C.copy("act", AT[:, 0:nb_, :], tp[:, 0:nb_, :], R=[tp], W=[AT])
                for j in range(nb_):
                    kb = kb0 + j
                    P.I("pe", "matmul", op_[:, 0:128], v[:, kb, :], AT[:, j, :], start=(kb == 0), stop=(kb == nkb - 1),
                        R=[v, AT], W=[op_])
            C.copy("dve", yo[:, qi * 128:(qi + 1) * 128], op_[:, 0:128], R=[op_], W=[yo])
        P.dma("sp", X.yT_d.t[u * 128:(u + 1) * 128, :], yo[:], R=yo, W=X.yT_d)
    if emb:
        P.end_phase()
        return P
    P.finish([X.yT_d])
    P.close()
    return P


def build_A_even(nh=8, P=None, io=None, pre=""):
    emb = P is not None
    if not emb:
        P = Prog()
    n_units = 2 * nh
    extra = [("ropec", [128, T_SEQ], F32), ("ropes", [128, T_SEQ], F32), ("perm", [128, 128], BF16),
             ("DT", [nh, 128, 128], F32), ("qdec4", [nh, 512], F32), ("kdec", [128, nh], F32), ("cdec", [128, nh], F32),
             ("retg", [128, nh], F32), ("mlg", [128, nh], F32), ("convw", [128, nh * 5], F32),
             ("wqk", [nh, 2, 128, 128], F32), ("wgate", [2, D, nh], F32), ("gateb", [nh, 2], F32),
             ("triuT", [128, 128], F32), ("e127", [128, 128], F32)]
    X = a_setup(P, n_units, 512, extra, io, pre, emb)
    C = X.C
    scale = 128.0 ** -0.5
    A = [P.ps([128, 512], F32, f"A{i}") for i in range(6)]
    T = [P.ps([128, 8, 128], BF16, f"T{i}") for i in range(2)]

    def ld(name, shape, dt=F32, src=None, q="sp"):
        b = P.sb(shape, dt, name)
        P.dma(q, b[:], (src if src is not None else X.ex[name][:]), R=X.ex[name], W=b)
        return b

    ropec = ld("ropec", [128, T_SEQ])
    ropes = ld("ropes", [128, T_SEQ])
    perm = ld("perm", [128, 128], BF16)
    kdec = ld("kdec", [128, nh])
    cdec = ld("cdec", [128, nh])
    retg = ld("retg", [128, nh])
    mlg = ld("mlg", [128, nh])
    convw = ld("convw", [128, nh * 5])
    triuT = ld("triuT", [128, 128])
    e127 = ld("e127", [128, 128])
    gateb = ld("gateb", [nh, 2])
    DT = P.sb([128, 128], F32, "DT")
    qdec = P.sb([128, 512], F32, "qdec")

    z0 = P.sb([128, T_SEQ], BF16, "z0")
    z1 = P.sb([128, T_SEQ], BF16, "z1")
    z2 = P.sb([128, T_SEQ], BF16, "z2")
    z3 = P.sb([128, T_SEQ], BF16, "z3")
    qd = P.sb([128, T_SEQ], BF16, "qd")
    xf = P.sb([128, T_SEQ + 4], F32, "xf")
    acc = P.sb([128, T_SEQ], F32, "acc")
    vtok = P.sb([128, 16, 132], BF16, "vtok")
    ktok = P.sb([128, 16, 128], BF16, "ktok")
    gtok = P.sb([128, 16, 128], BF16, "gtok")
    yT = [P.sb([128, T_SEQ], BF16, f"yT{i}") for i in range(2)]
    S = P.sb([128, 132], F32, "S")
    Sb = P.sb([128, 132], BF16, "Sb")
    PTt = P.sb([128, 128], BF16, "PTt")
    ot = P.sb([128, 128], F32, "ot")
    onb = P.sb([128, 128], BF16, "onb")
    stats = P.sb([128, 6], F32, "stats")
    mv = P.sb([128, 8], F32, "mv")
    t1 = P.sb([128, 512], F32, "t1")
    t2 = P.sb([128, 512], F32, "t2")
    P.I("dve", "memset", vtok[:], 1.0, W=[vtok])
    P.I("dve", "memset", xf[:], 0.0, W=[xf])

    def rope_sink(dst, sc_):
        def f(blk, ps):
            sl = slice(blk * 512, (blk + 1) * 512)
            P.I("act", "activation", out=z3[:, sl], in_=ps[:], func=AF.Copy, scale=sc_, R=[ps], W=[z3])
            sw = A[4 + blk % 2]
            P.I("pe", "matmul", sw[:], perm[:], z3[:, sl], start=True, stop=True, R=[perm, z3], W=[sw])
            P.I("dve", "tensor_tensor", out=t1[:], in0=z3[:, sl], in1=ropec[:, sl], op=ALU.mult, R=[z3, ropec], W=[t1])
            P.I("dve", "tensor_tensor", out=t2[:], in0=sw[:], in1=ropes[:, sl], op=ALU.mult, R=[sw, ropes], W=[t2])
            P.I("dve", "tensor_tensor", out=dst[:, sl], in0=t1[:], in1=t2[:], op=ALU.add, R=[t1, t2], W=[dst])
        return f

    def plain_sink(dst, func=AF.Copy):
        def f(blk, ps):
            P.I("act", "activation", out=dst[:, blk * 512:(blk + 1) * 512], in_=ps[:], func=func, R=[ps], W=[dst])
        return f

    def head_norm_out(src_ps_or_sb, R_, gcol, yo, tok0):
        P.I("dve", "bn_stats", out=stats[:], in_=src_ps_or_sb, R=R_, W=[stats])
        P.I("dve", "bn_aggr", out=mv[:, 0:2], in_=stats[:], R=[stats], W=[mv])
        P.I("act", "activation", out=mv[:, 2:3], in_=mv[:, 1:2], func=AF.Sqrt, bias=C.eps_hn[:], scale=1.0, R=[mv, C.eps_hn], W=[mv])
        P.I("dve", "reciprocal", out=mv[:, 3:4], in_=mv[:, 2:3], R=[mv], W=[mv])
        P.I("dve", "tensor_scalar", out=onb[:], in0=src_ps_or_sb, scalar1=mv[:, 0:1], scalar2=mv[:, 3:4], op0=ALU.subtract, op1=ALU.mult,
            R=list(R_) + [mv], W=[onb])
        tp = T[0]
        P.I("pe", "transpose", out=tp[:, 0, :], in_=onb[:], identity=C.ident_bf[:], R=[onb, C.ident_bf], W=[tp])
        return tp

    for hl in range(nh):
        u = hl
        a_inproj(X, u, 512, [rope_sink(z0, scale), rope_sink(z1, 1.0), plain_sink(z2), plain_sink(acc, AF.Silu)], A[0:4])
        qT_, kT_, vT_ = z0, z1, z2
        P.dma("sp", DT[:], X.ex["DT"].t[hl], R=X.ex["DT"], W=DT)
        P.dma("sp", qdec[:], X.ex["qdec4"].t[hl, :].partition_broadcast(128), R=X.ex["qdec4"], W=qdec)
        for blk in range(4):
            sl = slice(blk * 512, (blk + 1) * 512)
            P.I("dve", "tensor_tensor", out=qd[:, sl], in0=qT_[:, sl], in1=qdec[:], op=ALU.mult, R=[qT_, qdec], W=[qd])
        for g in range(2):
            tp = T[g % 2]
            for j in range(8):
                t = g * 8 + j
                P.I("pe", "transpose", out=tp[:, j, :], in_=vT_[:, t * 128:(t + 1) * 128], identity=C.ident_bf[:], R=[vT_, C.ident_bf], W=[tp])
            C.copy("dve", vtok[:, g * 8:(g + 1) * 8, 0:128], tp[:], R=[tp], W=[vtok])
        for g in range(2):
            tp = T[g % 2]
            for j in range(8):
                t = g * 8 + j
                P.I("pe", "transpose", out=tp[:, j, :], in_=kT_[:, t * 128:(t + 1) * 128], identity=C.ident_bf[:], R=[kT_, C.ident_bf], W=[tp])
            P.I("act", "activation", out=ktok[:, g * 8:(g + 1) * 8, :], in_=tp[:], func=AF.Copy, scale=kdec[:, hl:hl + 1], R=[tp, kdec], W=[ktok])
        P.I("dve", "memset", S[:], 0.0, W=[S])
        P.I("dve", "memset", Sb[:], 0.0, W=[Sb])
        yo = yT[u % 2]
        for c in range(16):
            cs = slice(c * 128, (c + 1) * 128)
            sp_ = A[c % 2]
            P.I("pe", "matmul", sp_[:, 0:128], kT_[:, cs], qT_[:, cs], start=True, stop=True, R=[kT_, qT_], W=[sp_])
            P.I("dve", "tensor_tensor", out=PTt[:], in0=sp_[:, 0:128], in1=DT[:], op=ALU.mult, R=[sp_, DT], W=[PTt])
            op_ = A[2 + c % 2]
            P.I("pe", "matmul", op_[:, 0:128], PTt[:], vtok[:, c, 0:128], start=True, stop=False, R=[PTt, vtok], W=[op_])
            P.I("pe", "matmul", op_[:, 0:128], qd[:, cs], Sb[:, 0:128], start=False, stop=True, R=[qd, Sb], W=[op_])
            stp = A[4 + c % 2]
            P.I("pe", "matmul", stp[:, 0:128], ktok[:, c, :], vtok[:, c, 0:128], start=True, stop=True, R=[ktok, vtok], W=[stp])
            P.I("dve", "scalar_tensor_tensor", out=S[:, 0:128], in0=S[:, 0:128], scalar=cdec[:, hl:hl + 1], in1=stp[:, 0:128],
                op0=ALU.mult, op1=ALU.add, R=[S, cdec, stp], W=[S])
            P.I("act", "activation", out=Sb[:, 0:128], in_=S[:, 0:128], func=AF.Copy, R=[S], W=[Sb])
            tp = head_norm_out(op_[:, 0:128], [op_], None, yo, c * 128)
            P.I("dve", "scalar_tensor_tensor", out=yo[:, cs], in0=tp[:, 0, :], scalar=retg[:, hl:hl + 1], in1=acc[:, cs],
                op0=ALU.mult, op1=ALU.mult, R=[tp, retg, acc], W=[yo])
        P.dma("sp", X.yT_d.t[u * 128:(u + 1) * 128, :], yo[:], R=yo, W=X.yT_d)

    class V:
        def __init__(self, buf):
            self.buf = buf

        def __getitem__(self, idx):
            if not isinstance(idx, tuple):
                idx = (idx,)
            return self.buf.t[(slice(0, nh),) + tuple(idx[1:])]
    IG, FG, Rf, ones8 = V(ropec), V(ropes), V(xf), V(acc)
    Gm = P.sb([nh, T_SEQ], F32, "Gm")
    one8 = P.sb([nh, 1], F32, "one8")
    ngb = P.sb([nh, 2], F32, "ngb")
    wgs = P.sb([128, 2, KC, nh], BF16, "wgs")
    AL = slice(0, T_SEQ)
    P.I("dve", "memset", ones8[:, AL], 1.0, W=[acc])
    P.I("dve", "memset", one8[:], 1.0, W=[one8])
    P.I("dve", "tensor_scalar", out=ngb[:], in0=gateb[:], scalar1=-1.0, scalar2=None, op0=ALU.mult, R=[gateb], W=[ngb])
    for gi in range(2):
        P.dma("pool", wgs[:, gi, :, :], X.ex["wgate"].t[gi].rearrange("(p c) n -> p c n", c=KC), R=X.ex["wgate"], W=wgs)
    for blk in range(4):
        hb_ = X.hTb[X.nload % 2]
        X.nload += 1
        P.dma("sp", hb_[:].rearrange("p c t -> p (c t)"), X.hT_d.t[blk], R=X.hT_d, W=hb_)
        sl = slice(blk * 512, (blk + 1) * 512)
        for gi in range(2):
            ps = A[gi]
            for c in range(KC):
                P.I("pe", "matmul", ps[0:nh, :], wgs[:, gi, c, :], hb_[:, c, :], start=(c == 0), stop=(c == KC - 1), R=[wgs, hb_], W=[ps])
        P.I("act", "activation", out=IG[:, sl], in_=A[0][0:nh, :], func=AF.Identity, bias=gateb[:, 0:1], scale=1.0, R=[A[0], gateb], W=[ropec])
        P.I("act", "activation", out=FG[:, sl], in_=A[1][0:nh, :], func=AF.Exp, bias=ngb[:, 1:2], scale=-1.0, R=[A[1], ngb], W=[ropes])
    P.I("act", "activation", out=FG[:, AL], in_=FG[:, AL], func=AF.Ln, bias=one8[:], scale=1.0, R=[ropes, one8], W=[ropes])
    P.I("dve", "tensor_tensor_scan", out=FG[:, AL], data0=ones8[:, AL], data1=FG[:, AL], initial=0.0, op0=ALU.mult, op1=ALU.add,
        R=[acc, ropes], W=[ropes])
    P.I("dve", "tensor_tensor", out=IG[:, AL], in0=IG[:, AL], in1=FG[:, AL], op=ALU.add, R=[ropec, ropes], W=[ropec])
    P.I("dve", "tensor_tensor_scan", out=Gm[:], data0=IG[:, AL], data1=IG[:, AL], initial=0.0, op0=ALU.max, op1=ALU.max, R=[ropec], W=[Gm])
    G3 = Gm[:].rearrange("p (c l) -> p c l", l=128)
    R3 = Rf[:, AL].rearrange("p (c l) -> p c l", l=128)
    P.I("dve", "memset", Rf[:, 0:128], 0.0, W=[xf])
    P.I("dve", "tensor_copy", out=R3[:, 1:16, :], in_=G3[:, 0:15, 127:128].to_broadcast([nh, 15, 128]), R=[Gm], W=[xf])
    P.I("dve", "tensor_tensor", out=IG[:, AL], in0=IG[:, AL], in1=Rf[:, AL], op=ALU.subtract, R=[ropec, xf], W=[ropec])
    P.I("act", "activation", out=IG[:, AL], in_=IG[:, AL], func=AF.Exp, R=[ropec], W=[ropec])
    P.I("dve", "tensor_tensor", out=Rf[:, AL], in0=Rf[:, AL], in1=Gm[:], op=ALU.subtract, R=[xf, Gm], W=[xf])
    P.I("act", "activation", out=Rf[:, AL], in_=Rf[:, AL], func=AF.Exp, R=[xf], W=[xf])
    P.I("dve", "tensor_tensor", out=FG[:, AL], in0=FG[:, AL], in1=Gm[:], op=ALU.subtract, R=[ropes, Gm], W=[ropes])
    P.I("act", "activation", out=FG[:, AL], in_=FG[:, AL], func=AF.Exp, R=[ropes], W=[ropes])
    qa, qr, qe = IG, Rf, FG
    qpar = {id(IG): ropec, id(Rf): xf, id(FG): ropes}
    qtok = [P.sb([128, 16, nh], F32, f"qtok{i}") for i in range(3)]
    for qi_, src in enumerate((qa, qr, qe)):
        ps = A[2 + qi_ % 2]
        for c in range(16):
            P.I("pe", "transpose", out=ps[:, c * nh:(c + 1) * nh], in_=src[:, c * 128:(c + 1) * 128], identity=C.ident_f[0:nh, 0:nh],
                R=[qpar[id(src)], C.ident_f], W=[ps])
        C.copy("dve", qtok[qi_][:], ps[:, 0:16 * nh].rearrange("p (c h) -> p c h", h=nh), R=[ps], W=[qtok[qi_]])
    a_tok, r_tok, e_tok = qtok
    decb = P.sb([128, 16, nh], F32, "decb")
    P.I("dve", "memset", xf[:], 0.0, R=[], W=[xf])
    P.I("pe", "matmul", A[4][:, 0:16 * nh], e127[:], r_tok[:].rearrange("p c h -> p (c h)"), start=True, stop=True, R=[e127, r_tok], W=[A[4]])
    C.copy("dve", decb[:], A[4][:, 0:16 * nh].rearrange("p (c h) -> p c h", h=nh), R=[A[4]], W=[decb])

    wq_s = P.sb([128, 128], BF16, "wq_s")
    wk_s = P.sb([128, 128], BF16, "wk_s")
    ka = ktok
    den = P.sb([128, 8], F32, "den")
    for hl in range(nh):
        u = nh + hl
        cw = convw[:, hl * 5:(hl + 1) * 5]

        def mu_sink(blk, ps):
            P.I("act", "activation", out=xf[:, 3 + blk * 512:3 + (blk + 1) * 512], in_=ps[:], func=AF.Copy, R=[ps], W=[xf])
        a_inproj(X, u, 384, [mu_sink, plain_sink(z2), plain_sink(z3, AF.Sigmoid)], A[0:3])
        P.dma("pool", wq_s[:], X.ex["wqk"].t[hl, 0], R=X.ex["wqk"], W=wq_s)
        P.dma("pool", wk_s[:], X.ex["wqk"].t[hl, 1], R=X.ex["wqk"], W=wk_s)
        P.I("dve", "tensor_scalar", out=acc[:], in0=xf[:, 3:3 + T_SEQ], scalar1=cw[:, 3:4], scalar2=cw[:, 4:5], op0=ALU.mult, op1=ALU.add,
            R=[xf, convw], W=[acc])
        for i in range(3):
            P.I("dve", "scalar_tensor_tensor", out=acc[:], in0=xf[:, i:i + T_SEQ], scalar=cw[:, i:i + 1], in1=acc[:], op0=ALU.mult, op1=ALU.add,
                R=[xf, convw, acc], W=[acc])
        P.I("act", "activation", out=z0[:], in_=acc[:], func=AF.Silu, R=[acc], W=[z0])
        uT = z0
        for blk in range(4):
            sl = slice(blk * 512, (blk + 1) * 512)
            P.I("pe", "matmul", A[0][:], wq_s[:], uT[:, sl], start=True, stop=True, R=[wq_s, uT], W=[A[0]])
            C.copy("act", z1[:, sl], A[0][:], R=[A[0]], W=[z1])
            P.I("pe", "matmul", A[1][:], wk_s[:], uT[:, sl], start=True, stop=True, R=[wk_s, uT], W=[A[1]])
            P.I("act", "activation", out=qd[:, sl], in_=A[1][:], func=AF.Copy, scale=scale, R=[A[1]], W=[qd])
        qmT, kmT = z1, qd
        for t in range(16):
            ps = A[2 + t % 2]
            P.I("pe", "matmul", ps[:, 0:128], uT[:, t * 128:(t + 1) * 128], wk_s[:], start=True, stop=True, R=[uT, wk_s], W=[ps])
            P.I("dve", "tensor_scalar", out=ka[:, t, :], in0=ps[:, 0:128], scalar1=a_tok[:, t, hl:hl + 1], scalar2=scale, op0=ALU.mult, op1=ALU.mult,
                R=[ps, a_tok], W=[ka])
        for src, dst in ((z2, vtok), (z3, gtok)):
            for g in range(2):
                tp = T[g % 2]
                for j in range(8):
                    t = g * 8 + j
                    P.I("pe", "transpose", out=tp[:, j, :], in_=src[:, t * 128:(t + 1) * 128], identity=C.ident_bf[:], R=[src, C.ident_bf], W=[tp])
                C.copy("dve" if dst is vtok else "act", dst[:, g * 8:(g + 1) * 8, 0:128], tp[:], R=[tp], W=[dst])
        P.I("dve", "memset", S[:], 0.0, W=[S])
        P.I("dve", "memset", Sb[:], 0.0, W=[Sb])
        yo = yT[u % 2]
        for c in range(16):
            cs = slice(c * 128, (c + 1) * 128)
            sp_ = A[c % 2]
            P.I("pe", "matmul", sp_[:, 0:128], kmT[:, cs], qmT[:, cs], start=True, stop=True, R=[kmT, qmT], W=[sp_])
            P.I("dve", "scalar_tensor_tensor", out=PTt[:], in0=sp_[:, 0:128], scalar=a_tok[:, c, hl:hl + 1], in1=triuT[:], op0=ALU.mult, op1=ALU.mult,
                R=[sp_, a_tok, triuT], W=[PTt])
            op_ = A[2 + c % 2]
            P.I("pe", "matmul", op_[:, 0:129], PTt[:], vtok[:, c, 0:129], start=True, stop=False, R=[PTt, vtok], W=[op_])
            P.I("pe", "matmul", op_[:, 0:129], qmT[:, cs], Sb[:, 0:129], start=False, stop=True, R=[qmT, Sb], W=[op_])
            stp = A[4 + c % 2]
            P.I("pe", "matmul", stp[:, 0:129], ka[:, c, :], vtok[:, c, 0:129], start=True, stop=True, R=[ka, vtok], W=[stp])
            P.I("dve", "tensor_tensor", out=S[:, 0:129], in0=S[:, 0:129], in1=stp[:, 0:129], op=ALU.add, R=[S, stp], W=[S])
            P.I("dve", "tensor_scalar", out=S[:, 0:129], in0=S[:, 0:129], scalar1=decb[:, c, hl:hl + 1], scalar2=None, op0=ALU.mult, R=[S, decb], W=[S])
            P.I("act", "activation", out=Sb[:, 0:129], in_=S[:, 0:129], func=AF.Copy, R=[S], W=[Sb])
            P.I("dve", "tensor_scalar", out=den[:, 0:1], in0=op_[:, 128:129], scalar1=r_tok[:, c, hl:hl + 1], scalar2=None, op0=ALU.mult,
                R=[op_, r_tok], W=[den])
            P.I("dve", "tensor_scalar", out=den[:, 4:5], in0=den[:, 0:1], scalar1=-1.0, scalar2=None, op0=ALU.mult, R=[den], W=[den])
            P.I("dve", "tensor_tensor", out=den[:, 0:1], in0=den[:, 0:1], in1=den[:, 4:5], op=ALU.max, R=[den], W=[den])
            P.I("dve", "tensor_tensor", out=den[:, 1:2], in0=den[:, 0:1], in1=e_tok[:, c, hl:hl + 1], op=ALU.max, R=[den, e_tok], W=[den])
            P.I("dve", "reciprocal", out=den[:, 2:3], in_=den[:, 1:2], R=[den], W=[den])
            P.I("dve", "tensor_tensor", out=den[:, 3:4], in0=den[:, 2:3], in1=r_tok[:, c, hl:hl + 1], op=ALU.mult, R=[den, r_tok], W=[den])
            P.I("dve", "scalar_tensor_tensor", out=ot[:], in0=op_[:, 0:128], scalar=den[:, 3:4], in1=gtok[:, c, :], op0=ALU.mult, op1=ALU.mult,
                R=[op_, den, gtok], W=[ot])
            tp = head_norm_out(ot[:], [ot], None, yo, c * 128)
            P.I("act", "activation", out=yo[:, cs], in_=tp[:, 0, :], func=AF.Copy, scale=mlg[:, hl:hl + 1], R=[tp, mlg], W=[yo])
        P.dma("sp", X.yT_d.t[u * 128:(u + 1) * 128, :], yo[:], R=yo, W=X.yT_d)
    if emb:
        P.end_phase()
        return P
    P.finish([X.yT_d])
    P.close()
    return P


def build_P0(ntok=1024, P=None, io=None):
    emb = P is not None
    if not emb:
        P = Prog()
    io = io or {}
    x_d = io["x"] if "x" in io else P.dram("x", [ntok, D], F32, "ExternalInput")
    idb_d = io["ident_bf"] if "ident_bf" in io else P.dram("ident_bf", [128, 128], BF16, "ExternalInput")
    idf_d = io["ident_f"] if "ident_f" in io else P.dram("ident_f", [128, 128], F32, "ExternalInput")
    hT_d = io["hT_out"] if "hT_out" in io else P.dram("hT_out", [ntok // 512, 128, KC * 512], BF16, "ExternalOutput")
    if emb:
        P.begin_phase()
    C = Common(P, idb_d, idf_d)
    xs = [P.sb([128, D], F32, f"xs{i}") for i in range(2)]
    hb = [P.sb([128, D], BF16, f"hb{i}") for i in range(2)]
    hT = P.sb([128, KC, 512], BF16, "hT")
    T = [P.ps([128, 8, 128], BF16, f"T{i}") for i in range(2)]
    for blk in range(ntok // 512):
        for tt in range(4):
            t = blk * 4 + tt
            P.dma("sp", xs[t % 2][:], x_d.t[t * 128:(t + 1) * 128, :], R=x_d, W=xs[t % 2])
            P.I("act", "activation", out=hb[t % 2][:], in_=xs[t % 2][:], func=AF.Copy, R=[xs[t % 2]], W=[hb[t % 2]])
            emit_transpose_to_hT(P, C, hb[t % 2], hT, tt, T)
        P.dma("sp", hT_d.t[blk], hT[:].rearrange("p c t -> p (c t)"), R=hT, W=hT_d)
    if emb:
        P.end_phase()
        return P
    P.finish([hT_d])
    P.close()
    return P


def build_fused(depth=DEPTH, n_exp=NE):
    P = Prog()
    x_d = P.dram("x", [T_SEQ, D], F32, "ExternalInput")
    memT_d = P.dram("memT", [D, 256], F32, "ExternalInput")
    idb_d = P.dram("ident_bf", [128, 128], BF16, "ExternalInput")
    idf_d = P.dram("ident_f", [128, 128], F32, "ExternalInput")
    out_d = P.dram("out", [T_SEQ, D], F32, "ExternalOutput")
    hT_s = P.dram("hT_s", [T_SEQ // 512, 128, KC * 512], BF16, "Internal")
    yT_s = P.dram("yT_s", [D, T_SEQ], BF16, "Internal")
    h_s = P.dram("h_s", [T_SEQ, D], F32, "Internal")
    ids = dict(ident_bf=idb_d, ident_f=idf_d)
    build_P0(T_SEQ, P=P, io=dict(x=x_d, hT_out=hT_s, **ids))
    for layer in range(depth):
        ioA = dict(hT=hT_s, yT_out=yT_s, **ids)
        if layer % 2 == 0:
            build_A_even(16, P=P, io=ioA, pre=f"L{layer}_")
        else:
            build_A_odd(16, P=P, io=ioA, pre=f"L{layer}_")
        ioB = dict(yT=yT_s, h_in=(x_d if layer == 0 else h_s), memT=memT_d,
                   h_out=(out_d if layer == depth - 1 else h_s), hT_out=hT_s, **ids)
        build_B(T_SEQ, n_exp, P=P, io=ioB, pre=f"L{layer}_")
    P.finish([out_d])
    P.close()
    return P


_bf = ml_dtypes.bfloat16
GW = 2048


def _consts():
    return dict(ident_bf=np.eye(128, dtype=np.float32).astype(_bf), ident_f=np.eye(128, dtype=np.float32))


def prep_A_even(inp, i, heads):
    nh = len(heads)
    w_in = inp["ab_w_in"][i]
    units = []
    for h in heads:
        units.append(np.concatenate([w_in[:, j * GW + h * 128:j * GW + (h + 1) * 128] for j in range(4)], axis=1))
    for h in heads:
        u = np.zeros((D, 512), np.float32)
        for jj, j in enumerate((4, 5, 6)):
            u[:, jj * 128:(jj + 1) * 128] = w_in[:, j * GW + h * 128:j * GW + (h + 1) * 128]
        units.append(u)
    pos = np.arange(T_SEQ, dtype=np.float64)
    inv = 10000.0 ** (-np.arange(0, 128, 2, dtype=np.float64) / 128)
    ang = (pos[:, None] * inv[None, :]).astype(np.float32).astype(np.float64)
    cos, sin = np.cos(ang).T, np.sin(ang).T
    ropec = np.concatenate([cos, cos], 0).astype(np.float32)
    ropes = np.concatenate([-sin, sin], 0).astype(np.float32)
    perm = np.zeros((128, 128), np.float32)
    for m in range(128):
        perm[(m + 64) % 128, m] = 1.0
    gam = np.array([1.0 - 2.0 ** (-(5.0 + h)) for h in heads], np.float64)
    ii = np.arange(128)
    DTm = np.zeros((nh, 128, 128), np.float32)
    for a, g in enumerate(gam):
        d = ii[None, :] - ii[:, None]
        DTm[a] = np.where(d >= 0, g ** np.maximum(d, 0), 0.0)
    qdec4 = np.stack([np.tile(g ** (ii + 1.0), 4) for g in gam]).astype(np.float32)
    kdec = np.stack([g ** (127.0 - ii) for g in gam], 1).astype(np.float32)
    cdec = np.tile((gam ** 128.0)[None, :], (128, 1)).astype(np.float32)
    retg = np.stack([inp["ab_ret_norm_g"][i][h * 128:(h + 1) * 128] for h in heads], 1)
    mlg = np.stack([inp["ab_mlstm_norm_g"][i][h * 128:(h + 1) * 128] for h in heads], 1)
    convw = np.zeros((128, nh * 5), np.float32)
    for a, h in enumerate(heads):
        convw[:, a * 5:a * 5 + 4] = inp["ab_conv_w"][i][:, h * 128:(h + 1) * 128].T
        convw[:, a * 5 + 4] = inp["ab_conv_b"][i][h * 128:(h + 1) * 128]
    wqk = np.stack([np.stack([inp["ab_wq"][i][h], inp["ab_wk"][i][h]]) for h in heads])
    wgate = np.stack([np.stack([w_in[:, 7 * GW + h] for h in heads], 1), np.stack([w_in[:, 7 * GW + 16 + h] for h in heads], 1)])
    gateb = np.stack([np.array([inp["ab_gate_b"][i][h], inp["ab_gate_b"][i][16 + h]]) for h in heads]).astype(np.float32)
    triuT = np.triu(np.ones((128, 128), np.float32))
    e127 = np.zeros((128, 128), np.float32)
    e127[127, :] = 1.0
    d = dict(w_units=np.ascontiguousarray(np.stack(units)), ropec=ropec, ropes=ropes, perm=perm.astype(_bf), DT=DTm, qdec4=qdec4,
             kdec=kdec, cdec=cdec, retg=np.ascontiguousarray(retg), mlg=np.ascontiguousarray(mlg), convw=convw,
             wqk=np.ascontiguousarray(wqk), wgate=np.ascontiguousarray(wgate), gateb=gateb, triuT=triuT, e127=e127)
    d.update(_consts())
    return d


def prep_A_odd(inp, i, heads):
    w_in = inp["cd_w_in"][i]
    rel = inp["cd_rel_bias"][i]

    def unit(g, h):
        return np.concatenate([w_in[:, (3 * g + j) * GW + h * 128:(3 * g + j) * GW + (h + 1) * 128] for j in range(3)], axis=1)
    w_units = np.stack([unit(0, h) for h in heads] + [unit(1, h) for h in heads])
    r = np.arange(64)[:, None]
    m = np.arange(704)[None, :]
    idx = np.clip(r + 512 - (m - 64), -63, 128) + 63
    bias_ext = np.stack([rel[h][idx] for h in heads]).astype(np.float32)
    d = dict(w_units=np.ascontiguousarray(w_units), bias_ext=bias_ext, tril=np.tril(np.ones((128, 128), np.float32), -1))
    d.update(_consts())
    return d


def prep_B(inp, layer):
    i = layer // 2
    w_out = inp["ab_w_out"][i] if layer % 2 == 0 else inp["cd_w_out"][i]
    lnp = np.stack([inp["mix_ln_g"][layer], inp["mix_ln_b"][layer], inp["xa_ln_g"][layer], inp["xa_ln_b"][layer],
                    inp["moe_ln_g"][layer], inp["moe_ln_b"][layer]])
    bg, bu = inp["moe_b_gate"][layer], inp["moe_b_up"][layer]
    bgu = np.zeros((128, NE * 4), np.float32)
    for c in range(2):
        bgu[:, c::4] = bg[:, c * 128:(c + 1) * 128].T
        bgu[:, 2 + c::4] = bu[:, c * 128:(c + 1) * 128].T
    d = dict(w_out=np.ascontiguousarray(w_out), lnp=lnp, xa_wq=np.ascontiguousarray(inp["xa_wq"][layer]),
             xa_wkv=np.ascontiguousarray(inp["xa_wkv"][layer]), xa_wo=np.ascontiguousarray(inp["xa_wo"][layer]),
             rw=np.ascontiguousarray(inp["moe_router_w"][layer]), rb=np.ascontiguousarray(inp["moe_router_b"][layer][None, :]),
             wg=np.ascontiguousarray(inp["moe_w_gate"][layer]), wu=np.ascontiguousarray(inp["moe_w_up"][layer]),
             wd=np.ascontiguousarray(inp["moe_w_down"][layer]), bgu=bgu, bd=np.ascontiguousarray(inp["moe_b_down"][layer]))
    d.update(_consts())
    return d


_PROGS = {}


def _prog(name):
    if name not in _PROGS:
        _PROGS[name] = {"P0": build_P0, "A_even": lambda: build_A_even(8), "A_odd": lambda: build_A_odd(8), "B": build_B,
                        "fused": build_fused}[name]()
    return _PROGS[name]


def fused_weights(inp, depth=DEPTH):
    w = {}
    heads = list(range(16))
    for layer in range(depth):
        i = layer // 2
        d = prep_A_even(inp, i, heads) if layer % 2 == 0 else prep_A_odd(inp, i, heads)
        d.update(prep_B(inp, layer))
        for k, v in d.items():
            if k in ("ident_bf", "ident_f"):
                continue
            w[f"L{layer}_{k}"] = v
    return w


def kernel(**inp):
    inp = {k: np.asarray(v) for k, v in inp.items()}
    x = inp["x"]
    mem = inp["mem"]
    w = fused_weights(inp)
    cst = _consts()
    maps = []
    for c in range(NCORES):
        b = c % 4
        d = dict(w)
        d.update(cst)
        d["x"] = np.ascontiguousarray(x[b])
        d["memT"] = np.ascontiguousarray(mem[b].T)
        maps.append(d)
    res = run_bass_kernel_spmd(_prog("fused").nc, maps, core_ids=list(range(NCORES))).results
    return np.stack([res[b]["out"] for b in range(4)]).astype(np.float32)
```
